# Optimizing a Trainium2 kernel written in Bass

```python
import math
import jax
import jax.numpy as jnp
from jax import lax
import numpy as np

D_MODEL = 2048
BATCH = 4
SEQ = 2048
DEPTH = 4

GRID_W = 64
CTX_LEN = 256
EPS = 1e-6
ROPE_BASE = 10000.0
QBLOCK = 128

A_HEADS = 8
A_HEAD_DIM = 64
A_WIDTH = A_HEADS * 2 * A_HEAD_DIM
POOL_WINDOWS = (2, 4, 8, 16)
POOL_GROUP = 256
B_WIDTH = len(POOL_WINDOWS) * POOL_GROUP
EVEN_IN = 3 * A_WIDTH + B_WIDTH

C_HEADS = 12
Q_LORA = 512
KV_LORA = 512
NOPE_DIM = 128
ROPE_DIM = 64
V_DIM = 128
QK_DIM = NOPE_DIM + ROPE_DIM
C_WIDTH = C_HEADS * V_DIM
F_GROUPS = 4
F_GROUP = 128
D_WIDTH = F_GROUPS * F_GROUP
C_KV_OFF = Q_LORA
C_PE_OFF = Q_LORA + KV_LORA
D_OFF = C_PE_OFF + ROPE_DIM
ODD_IN = D_OFF + D_WIDTH

N_EXPERTS = 32
TOP_K = 4
D_EXPERT = 768
SWIGLU_LIMIT = 7.0
SWIGLU_ALPHA = 1.702
MOE_BLOCK = 128

N_EVEN = (DEPTH + 1) // 2
N_ODD = DEPTH // 2

kernel_name = 'hybrid_diffattn_pool_mla_fourier_moe_dit'


def rmsnorm(x, g):
    xf = x.astype(jnp.float32)
    y = xf * lax.rsqrt(jnp.mean(xf * xf, axis=-1, keepdims=True) + EPS)
    return y.astype(x.dtype) * g


def modulate(h, shift, scale):
    return h * (1 + scale) + shift


def axial_rope(n_tokens, rot_dim, dtype):
    rows = n_tokens // GRID_W
    row = jnp.repeat(jnp.arange(rows, dtype=jnp.float32), GRID_W)
    col = jnp.tile(jnp.arange(GRID_W, dtype=jnp.float32), rows)
    axis_dim = rot_dim // 2
    inv = ROPE_BASE ** (-jnp.arange(0, axis_dim, 2, dtype=jnp.float32) / axis_dim)
    ang = jnp.concatenate([row[:, None] * inv, col[:, None] * inv], axis=-1)
    return jnp.cos(ang).astype(dtype), jnp.sin(ang).astype(dtype)


def apply_rope(x, cos, sin):
    shape = (1, cos.shape[0]) + (1,) * (x.ndim - 3) + (cos.shape[1],)
    cos = cos.reshape(shape)
    sin = sin.reshape(shape)
    x1, x2 = jnp.split(x, 2, axis=-1)
    return jnp.concatenate([x1 * cos - x2 * sin, x2 * cos + x1 * sin], axis=-1)


def sweep_query_blocks(fn, q):
    b, n = q.shape[:2]
    nb = n // QBLOCK
    qb = jnp.moveaxis(q.reshape((b, nb, QBLOCK) + q.shape[2:]), 1, 0)
    out = jnp.moveaxis(lax.map(fn, qb), 0, 1)
    return out.reshape((b, n) + out.shape[3:])


def diff_attend(qb, k, v, lam):
    s = jnp.einsum('bqhmd,bkhmd->bhmqk', qb, k).astype(jnp.float32) * (A_HEAD_DIM ** -0.5)
    p = jax.nn.softmax(s, axis=-1)
    a = p[:, :, 0] - lam * p[:, :, 1]
    return jnp.einsum('bhqk,bkhe->bqhe', a.astype(v.dtype), v)


def softmax_attend(qb, k, v):
    s = jnp.einsum('bqhd,bkhd->bhqk', qb, k).astype(jnp.float32) * (QK_DIM ** -0.5)
    p = jax.nn.softmax(s, axis=-1)
    return jnp.einsum('bhqk,bkhe->bqhe', p.astype(v.dtype), v)


def multiscale_pool(u, w_pool, s_pool):
    b, n, g, cg = u.shape
    uf = u.astype(jnp.float32)
    cs = jnp.concatenate([jnp.zeros((b, 1, g, cg), jnp.float32), jnp.cumsum(uf, axis=1)], axis=1)
    t = jnp.arange(n)
    means = []
    for gi, w in enumerate(POOL_WINDOWS):
        lo = jnp.clip(t - w // 2, 0, n)
        hi = jnp.clip(t + w - w // 2, 0, n)
        win_sum = cs[:, hi, gi] - cs[:, lo, gi]
        means.append(win_sum / (hi - lo).astype(jnp.float32)[None, :, None])
    pooled = (jnp.stack(means, axis=2) - uf).astype(u.dtype)
    mixed = jnp.einsum('bngc,gce->bnge', pooled, w_pool)
    return mixed.reshape(b, n, g * cg) * s_pool


def fourier_mix(u, w_f):
    f = jnp.fft.fft2(u.astype(jnp.float32), axes=(1, 3), norm='ortho').real.astype(u.dtype)
    return jnp.einsum('bngc,gce->bnge', f, w_f).reshape(u.shape[0], u.shape[1], D_WIDTH)


def mixer_ab(p_lat, p_ctx, rope, g_aq, g_ak, lam_p, g_subln, w_pool, s_pool, lam_init, ctx_out):
    cos, sin = rope
    lam_f = lam_p.astype(jnp.float32)
    lam = jnp.exp(jnp.sum(lam_f[0] * lam_f[1])) - jnp.exp(jnp.sum(lam_f[2] * lam_f[3])) + lam_init

    def heads(p, off, g):
        b, n = p.shape[:2]
        return rmsnorm(p[..., off:off + A_WIDTH].reshape(b, n, A_HEADS, 2, A_HEAD_DIM), g)

    def values(p):
        b, n = p.shape[:2]
        return p[..., 2 * A_WIDTH:3 * A_WIDTH].reshape(b, n, A_HEADS, 2 * A_HEAD_DIM)

    def pool_in(p):
        b, n = p.shape[:2]
        return p[..., 3 * A_WIDTH:].reshape(b, n, len(POOL_WINDOWS), POOL_GROUP)

    def finish(o):
        b, n = o.shape[:2]
        return (rmsnorm(o, g_subln) * (1.0 - lam_init)).reshape(b, n, A_WIDTH)

    k_ctx = heads(p_ctx, A_WIDTH, g_ak)
    v_ctx = values(p_ctx)
    q_lat = apply_rope(heads(p_lat, 0, g_aq), cos, sin)
    k_all = jnp.concatenate([apply_rope(heads(p_lat, A_WIDTH, g_ak), cos, sin), k_ctx], axis=1)
    v_all = jnp.concatenate([values(p_lat), v_ctx], axis=1)
    o_lat = sweep_query_blocks(lambda qb: diff_attend(qb, k_all, v_all, lam), q_lat)
    y_lat = jnp.concatenate([finish(o_lat), multiscale_pool(pool_in(p_lat), w_pool, s_pool)], axis=-1)
    if not ctx_out:
        return y_lat, None
    o_ctx = sweep_query_blocks(lambda qb: diff_attend(qb, k_ctx, v_ctx, lam), heads(p_ctx, 0, g_aq))
    y_ctx = jnp.concatenate([finish(o_ctx), multiscale_pool(pool_in(p_ctx), w_pool, s_pool)], axis=-1)
    return y_lat, y_ctx


def rope_tail(x, cos, sin):
    return jnp.concatenate([x[..., :NOPE_DIM], apply_rope(x[..., NOPE_DIM:], cos, sin)], axis=-1)


def mla_q(p, g_qa, w_qb, g_mq, rope):
    b, n = p.shape[:2]
    q = (rmsnorm(p[..., :Q_LORA], g_qa) @ w_qb).reshape(b, n, C_HEADS, QK_DIM)
    q = rmsnorm(q, g_mq)
    return q if rope is None else rope_tail(q, rope[0], rope[1])


def mla_kv(p, g_kva, w_kvb, g_mk, rope):
    b, n = p.shape[:2]
    kv = (rmsnorm(p[..., C_KV_OFF:C_PE_OFF], g_kva) @ w_kvb).reshape(b, n, C_HEADS, NOPE_DIM + V_DIM)
    k_pe = jnp.broadcast_to(p[..., C_PE_OFF:D_OFF][:, :, None, :], (b, n, C_HEADS, ROPE_DIM))
    k = rmsnorm(jnp.concatenate([kv[..., :NOPE_DIM], k_pe], axis=-1), g_mk)
    if rope is not None:
        k = rope_tail(k, rope[0], rope[1])
    return k, kv[..., NOPE_DIM:]


def mixer_cd(p_lat, p_ctx, rope, g_qa, g_kva, w_qb, w_kvb, g_mq, g_mk, w_f, ctx_out):
    b, n = p_lat.shape[:2]
    k_ctx, v_ctx = mla_kv(p_ctx, g_kva, w_kvb, g_mk, None)
    k_lat, v_lat = mla_kv(p_lat, g_kva, w_kvb, g_mk, rope)
    k_all = jnp.concatenate([k_lat, k_ctx], axis=1)
    v_all = jnp.concatenate([v_lat, v_ctx], axis=1)
    q_lat = mla_q(p_lat, g_qa, w_qb, g_mq, rope)
    o_lat = sweep_query_blocks(lambda qb: softmax_attend(qb, k_all, v_all), q_lat).reshape(b, n, C_WIDTH)
    u_lat = p_lat[..., D_OFF:].reshape(b, n, F_GROUPS, F_GROUP)
    y_lat = jnp.concatenate([o_lat, fourier_mix(u_lat, w_f)], axis=-1)
    if not ctx_out:
        return y_lat, None
    nc = p_ctx.shape[1]
    q_ctx = mla_q(p_ctx, g_qa, w_qb, g_mq, None)
    o_ctx = sweep_query_blocks(lambda qb: softmax_attend(qb, k_ctx, v_ctx), q_ctx).reshape(b, nc, C_WIDTH)
    u_ctx = p_ctx[..., D_OFF:].reshape(b, nc, F_GROUPS, F_GROUP)
    y_ctx = jnp.concatenate([o_ctx, fourier_mix(u_ctx, w_f)], axis=-1)
    return y_lat, y_ctx


def moe_ffn(h, w_router, b_router, w_gu, b_gu, w_down, b_down):
    n_tok, d = h.shape
    n_assign = n_tok * TOP_K
    logits = (h @ w_router).astype(jnp.float32) + b_router.astype(jnp.float32)
    top_logit, top_e = lax.top_k(logits, TOP_K)
    gate = jax.nn.softmax(top_logit, axis=-1)
    e_flat = top_e.reshape(-1)
    order = jnp.argsort(e_flat)
    e_sorted = e_flat[order]
    counts = jnp.bincount(e_flat, length=N_EXPERTS)
    padded = (counts + MOE_BLOCK - 1) // MOE_BLOCK * MOE_BLOCK
    pad_end = jnp.cumsum(padded)
    rank = jnp.arange(n_assign) - (jnp.cumsum(counts) - counts)[e_sorted]
    slot = (pad_end - padded)[e_sorted] + rank
    n_blocks = (n_assign + N_EXPERTS * (MOE_BLOCK - 1) + MOE_BLOCK - 1) // MOE_BLOCK
    n_rows = n_blocks * MOE_BLOCK
    tok = jnp.zeros((n_rows,), jnp.int32).at[slot].set((order // TOP_K).astype(jnp.int32))
    wgt = jnp.zeros((n_rows,), jnp.float32).at[slot].set(gate.reshape(-1)[order])
    blk_e = jnp.minimum(jnp.searchsorted(pad_end, jnp.arange(n_blocks) * MOE_BLOCK, side='right'), N_EXPERTS - 1)
    xb = h[tok].reshape(n_blocks, MOE_BLOCK, d)

    def expert_block(args):
        xe, e = args
        gu = xe @ w_gu[e] + b_gu[e]
        g_ = jnp.minimum(gu[..., :D_EXPERT], SWIGLU_LIMIT)
        up = jnp.clip(gu[..., D_EXPERT:], -SWIGLU_LIMIT, SWIGLU_LIMIT)
        act = (up + 1) * (g_ * jax.nn.sigmoid(SWIGLU_ALPHA * g_))
        return act @ w_down[e] + b_down[e]

    yb = lax.map(expert_block, (xb, blk_e)).reshape(n_rows, d)
    return jnp.zeros_like(h).at[tok].add(yb * wgt[:, None].astype(h.dtype))


def setup_inputs(seed: int = 0) -> dict:
    key = jax.random.key(seed)
    ks = iter(jax.random.split(key, 32))
    D = D_MODEL

    def nrm(shape, scale):
        return jax.random.normal(next(ks), shape, jnp.float32) * scale

    def gain(shape):
        return 1.0 + nrm(shape, 0.05)

    return {
        'x': nrm((BATCH, SEQ, D), 1.0),
        'c': nrm((BATCH, D), 1.0),
        'ctx': nrm((BATCH, CTX_LEN, D), 1.0),
        'c_ctx': nrm((D,), 1.0),
        'w_mod': nrm((DEPTH, D, 6 * D), 0.5 * D ** -0.5),
        'b_mod': nrm((DEPTH, 6 * D), 0.02),
        'g_mix': gain((DEPTH, D)),
        'g_ffn': gain((DEPTH, D)),
        'w_out': nrm((DEPTH, D, D), D ** -0.5),
        'w_in_ab': nrm((N_EVEN, D, EVEN_IN), D ** -0.5),
        'g_aq': gain((N_EVEN, A_HEAD_DIM)),
        'g_ak': gain((N_EVEN, A_HEAD_DIM)),
        'lam': nrm((N_EVEN, 4, A_HEAD_DIM), 0.1),
        'g_subln': gain((N_EVEN, 2 * A_HEAD_DIM)),
        'w_pool': nrm((N_EVEN, len(POOL_WINDOWS), POOL_GROUP, POOL_GROUP), POOL_GROUP ** -0.5),
        's_pool': gain((N_EVEN, B_WIDTH)),
        'w_in_cd': nrm((N_ODD, D, ODD_IN), D ** -0.5),
        'g_qa': gain((N_ODD, Q_LORA)),
        'g_kva': gain((N_ODD, KV_LORA)),
        'w_qb': nrm((N_ODD, Q_LORA, C_HEADS * QK_DIM), Q_LORA ** -0.5),
        'w_kvb': nrm((N_ODD, KV_LORA, C_HEADS * (NOPE_DIM + V_DIM)), KV_LORA ** -0.5),
        'g_mq': gain((N_ODD, QK_DIM)),
        'g_mk': gain((N_ODD, QK_DIM)),
        'w_fourier': nrm((N_ODD, F_GROUPS, F_GROUP, F_GROUP), F_GROUP ** -0.5),
        'w_router': nrm((DEPTH, D, N_EXPERTS), D ** -0.5),
        'b_router': nrm((DEPTH, N_EXPERTS), 0.01),
        'w_gu': nrm((DEPTH, N_EXPERTS, D, 2 * D_EXPERT), D ** -0.5),
        'b_gu': nrm((DEPTH, N_EXPERTS, 2 * D_EXPERT), 0.01),
        'w_down': nrm((DEPTH, N_EXPERTS, D_EXPERT, D), D_EXPERT ** -0.5),
        'b_down': nrm((DEPTH, N_EXPERTS, D), 0.01),
    }


def reference(x, c, ctx, c_ctx, w_mod, b_mod, g_mix, g_ffn, w_out, w_in_ab, g_aq, g_ak, lam, g_subln, w_pool, s_pool, w_in_cd, g_qa, g_kva, w_qb, w_kvb, g_mq, g_mk, w_fourier, w_router, b_router, w_gu, b_gu, w_down, b_down):
    b, n, d = x.shape
    n_ctx = ctx.shape[1]
    rope_a = axial_rope(n, A_HEAD_DIM, x.dtype)
    rope_c = axial_rope(n, ROPE_DIM, x.dtype)
    s_lat = jax.nn.silu(c)
    s_ctx = jax.nn.silu(c_ctx)
    xl, xc = x, ctx
    for l in range(DEPTH):
        ctx_out = l < DEPTH - 1
        i = l // 2
        ml = jnp.split((s_lat @ w_mod[l] + b_mod[l])[:, None, :], 6, axis=-1)
        mc = jnp.split(s_ctx @ w_mod[l] + b_mod[l], 6, axis=-1)
        hl = modulate(rmsnorm(xl, g_mix[l]), ml[0], ml[1])
        hc = modulate(rmsnorm(xc, g_mix[l]), mc[0], mc[1])
        if l % 2 == 0:
            lam_init = 0.8 - 0.6 * math.exp(-0.3 * l)
            yl, yc = mixer_ab(hl @ w_in_ab[i], hc @ w_in_ab[i], rope_a, g_aq[i], g_ak[i], lam[i],
                              g_subln[i], w_pool[i], s_pool[i], lam_init, ctx_out)
        else:
            yl, yc = mixer_cd(hl @ w_in_cd[i], hc @ w_in_cd[i], rope_c, g_qa[i], g_kva[i], w_qb[i],
                              w_kvb[i], g_mq[i], g_mk[i], w_fourier[i], ctx_out)
        xl = xl + ml[2] * (yl @ w_out[l])
        fl = modulate(rmsnorm(xl, g_ffn[l]), ml[3], ml[4]).reshape(b * n, d)
        if ctx_out:
            xc = xc + mc[2] * (yc @ w_out[l])
            fc = modulate(rmsnorm(xc, g_ffn[l]), mc[3], mc[4]).reshape(b * n_ctx, d)
            f = moe_ffn(jnp.concatenate([fl, fc], axis=0), w_router[l], b_router[l], w_gu[l], b_gu[l], w_down[l], b_down[l])
            xl = xl + ml[5] * f[:b * n].reshape(b, n, d)
            xc = xc + mc[5] * f[b * n:].reshape(b, n_ctx, d)
        else:
            f = moe_ffn(fl, w_router[l], b_router[l], w_gu[l], b_gu[l], w_down[l], b_down[l])
            xl = xl + ml[5] * f.reshape(b, n, d)
    return xl
```

```python
import math
import numpy as np
import ml_dtypes
import concourse.bass as bass
import concourse.mybir as mybir
from concourse.bass_utils import run_bass_kernel_spmd

F32 = mybir.dt.float32
BF16 = mybir.dt.bfloat16
ALU = mybir.AluOpType
AF = mybir.ActivationFunctionType
AX = mybir.AxisListType
NPBF = ml_dtypes.bfloat16

D = 2048
KC = 16
DEPTH = 4
NCORE = 8
TL = 1024
TCX = 128
T = TL + TCX
NT = T // 128
EPS = 1e-6
NEXP = 32
DEXP = 768
NTILES_N = [(0, 512), (512, 512), (1024, 128)]


class Buf:
    __slots__ = ("w", "r")

    def __init__(self):
        self.w = None
        self.r = {}


class Sched:
    ENGS = ("pe", "act", "dve", "pool", "sp")

    def __init__(self, nc, n_dma_sems=8):
        self.nc = nc
        self.ops = {e: [] for e in self.ENGS}
        self.sems = {}
        self.cnt = {e: 0 for e in self.ENGS}
        self.seen = {e: {} for e in self.ENGS}
        self._ctx = []
        for e in self.ENGS:
            self.sems[e] = self._sem("s_" + e)
        self.dq = {}
        for q in ("sp", "act", "pool"):
            lst = []
            for i in range(n_dma_sems):
                k = "d_%s_%d" % (q, i)
                self.sems[k] = self._sem(k)
                lst.append(k)
            self.dq[q] = {"keys": lst, "n": 0}

    def _sem(self, name):
        cm = self.nc.semaphore(name)
        h = cm.__enter__()
        self._ctx.append(cm)
        return h

    def _need(self, eng, reads, writes):
        need = {}

        def add(ev):
            if ev is None:
                return
            k, v = ev
            if need.get(k, 0) < v:
                need[k] = v
        for b in reads:
            add(b.w)
        for b in writes:
            add(b.w)
            for k, v in b.r.items():
                add((k, v))
        out = []
        for k, v in need.items():
            if k == "pe" and eng == "pe":
                continue
            if self.seen[eng].get(k, 0) < v:
                self.seen[eng][k] = v
                out.append((k, v))
        return out

    def _emit_waits(self, eng, waits):
        for k, v in waits:
            h = self.sems[k]
            self.ops[eng].append(lambda e, h=h, v=v: e.wait_ge(h, v))

    def _mark(self, ev, reads, writes):
        k, v = ev
        for b in reads:
            if b.r.get(k, 0) < v:
                b.r[k] = v
        for b in writes:
            b.w = ev
            b.r = {}

    def op(self, eng, fn, reads=(), writes=()):
        waits = self._need(eng, reads, writes)
        self._emit_waits(eng, waits)
        self.cnt[eng] += 1
        v = self.cnt[eng]
        h = self.sems[eng]
        self.ops[eng].append(lambda e, fn=fn, h=h: fn(e).then_inc(h, 1))
        self._mark((eng, v), reads, writes)

    def _slot(self, q, waits):
        d = self.dq[q]
        i = d["n"]
        d["n"] += 1
        n = len(d["keys"])
        k = d["keys"][i % n]
        val = 16 * (i // n + 1)
        if i >= n:
            prev = 16 * (i // n)
            if self.seen[q].get(k, 0) < prev:
                self.seen[q][k] = prev
                waits.append((k, prev))
        return k, val

    def dma(self, q, out, in_, reads=(), writes=(), **kw):
        waits = self._need(q, reads, writes)
        k, val = self._slot(q, waits)
        self._emit_waits(q, waits)
        h = self.sems[k]
        self.ops[q].append(lambda e, out=out, in_=in_, h=h, kw=kw: e.dma_start(out=out, in_=in_, **kw).then_inc(h, 16))
        self._mark((k, val), reads, writes)

    def wait_all(self, eng, bufs):
        self._emit_waits(eng, self._need(eng, bufs, bufs))

    def emit(self):
        ops = self.ops
        with self.nc.Block() as block:
            @block.tensor
            def _(e):
                for f in ops["pe"]:
                    f(e)

            @block.scalar
            def _(e):
                for f in ops["act"]:
                    f(e)

            @block.vector
            def _(e):
                for f in ops["dve"]:
                    f(e)

            @block.gpsimd
            def _(e):
                for f in ops["pool"]:
                    f(e)

            @block.sync
            def _(e):
                for f in ops["sp"]:
                    f(e)

    def close(self):
        for cm in reversed(self._ctx):
            cm.__exit__(None, None, None)
        self._ctx = []


class Mem:
    def __init__(self, nc):
        self.nc = nc
        self._ctx = []

    def sb(self, name, shape, dtype):
        cm = self.nc.sbuf_tensor(name, list(shape), dtype)
        t = cm.__enter__()
        self._ctx.append(cm)
        return t

    def ps(self, name, shape, dtype):
        cm = self.nc.psum_tensor(name, list(shape), dtype)
        t = cm.__enter__()
        self._ctx.append(cm)
        return t

    def close(self):
        for cm in reversed(self._ctx):
            cm.__exit__(None, None, None)
        self._ctx = []


class Prog:
    def __init__(self):
        self.nc = bass.Bass("TRN2", target_bir_lowering=False)
        self.S = Sched(self.nc)
        self.M = Mem(self.nc)
        self.pb = [self.M.ps("pb%d" % i, [128, 512], F32) for i in range(8)]
        self.pbb = [Buf() for _ in range(8)]
        self.ins = {}
        self.outs = {}
        self._rr = {}

    def din(self, name, shape, dtype=F32):
        t = self.nc.dram_tensor(name, list(shape), dtype, kind="ExternalInput").ap()
        self.ins[name] = t
        return t

    def dout(self, name, shape, dtype=F32):
        t = self.nc.dram_tensor(name, list(shape), dtype, kind="ExternalOutput").ap()
        self.outs[name] = t
        return t

    def rr(self, key, n):
        v = self._rr.get(key, 0)
        self._rr[key] = v + 1
        return v % n

    def consts(self):
        S, M = self.S, self.M
        self.identb = M.sb("identb", [128, 128], BF16)
        self.b_identb = Buf()
        self.identf = M.sb("identf", [128, 128], F32)
        self.b_identf = Buf()
        self.epsc = M.sb("epsc", [128, 1], F32)
        self.b_eps = Buf()
        S.op("pool", lambda e: e.memset(self.identf[:], 1.0), writes=[self.b_identf])
        S.op("pool", lambda e: e.affine_select(out=self.identf[:], in_=self.identf[:], pattern=[[-1, 128]],
                                                compare_op=ALU.is_equal, fill=0.0, base=0, channel_multiplier=1),
             reads=[self.b_identf], writes=[self.b_identf])
        S.op("pool", lambda e: e.tensor_copy(out=self.identb[:], in_=self.identf[:]), reads=[self.b_identf],
             writes=[self.b_identb])
        S.op("pool", lambda e: e.memset(self.epsc[:], EPS), writes=[self.b_eps])

    def finish(self, out_bufs):
        self.S.wait_all("sp", out_bufs)
        self.S.emit()
        self.M.close()
        self.S.close()
        return self.nc


def mm_group(P, out_ap, pairs, reads, writes):
    n = len(pairs)

    def fn(e):
        ins = None
        for i, (l, r) in enumerate(pairs):
            ins = e.matmul(out_ap, lhsT=l, rhs=r, start=(i == 0), stop=(i == n - 1))
        return ins
    P.S.op("pe", fn, reads=reads, writes=writes)


def build_M():
    P = Prog()
    S, M = P.S, P.M
    cT5 = P.din("cT5", [128, KC, 5])
    wm = P.din("wmod_sh", [DEPTH, D, 1536])
    bmT = P.din("bmodT", [128, DEPTH, 12])
    out = P.dout("mod_sh", [128, DEPTH * 12 * 5])
    ct = M.sb("ct", [128, KC, 5], F32)
    st = M.sb("st", [128, KC, 5], F32)
    bt = M.sb("bt", [128, DEPTH, 12], F32)
    res = M.sb("res", [128, DEPTH * 12 * 5], F32)
    wbuf = [M.sb("wm%d" % i, [128, KC, 768], F32) for i in range(2)]
    b_ct, b_st, b_bt, b_res = Buf(), Buf(), Buf(), Buf()
    b_w = [Buf(), Buf()]
    S.dma("sp", ct[:], cT5, writes=[b_ct])
    S.dma("sp", bt[:], bmT, writes=[b_bt])
    S.op("act", lambda e: e.activation(out=st[:], in_=ct[:], func=AF.Silu), reads=[b_ct], writes=[b_st])
    it = 0
    for l in range(DEPTH):
        for hh in range(2):
            wb, bw = wbuf[it % 2], b_w[it % 2]
            it += 1
            S.dma("sp" if hh == 0 else "act", wb[:],
                  wm[l, :, hh * 768:(hh + 1) * 768].rearrange("(k p) c -> p k c", p=128), writes=[bw])
            for q6 in range(6):
                q = hh * 6 + q6
                bank = P.rr("m", 4)
                ps = P.pb[bank][:, 0:5]
                mm_group(P, ps, [(wb[:, k, q6 * 128:(q6 + 1) * 128], st[:, k, :]) for k in range(KC)],
                         reads=[bw, b_st], writes=[P.pbb[bank]])
                o = (l * 12 + q) * 5
                S.op("dve", lambda e, ps=ps, o=o, l=l, q=q: e.tensor_scalar(
                    out=res[:, o:o + 5], in0=ps, scalar1=bt[:, l, q:q + 1], scalar2=None, op0=ALU.add),
                    reads=[P.pbb[bank], b_bt], writes=[b_res])
    bo = Buf()
    S.dma("sp", out, res[:], reads=[b_res], writes=[bo])
    return P.finish([bo]), P


def stream_x(P, x_in):
    P.Xs = P.M.sb("Xs", [128, 2, D], F32)
    P.bXs = [Buf(), Buf()]
    P.x_src = x_in


def xtile(P, t):
    if hasattr(P, "x_src"):
        i = t % 2
        P.S.dma("sp" if i == 0 else "act", P.Xs[:, i, :], P.x_src[t * 128:(t + 1) * 128, :], writes=[P.bXs[i]])
        return P.Xs[:, i, :], P.bXs[i]
    return P.X[:, t, :], P.bX[t]


def load_x(P, x_in):
    P.X = P.M.sb("X", [128, NT, D], F32)
    P.bX = [Buf() for _ in range(NT)]
    for t in range(NT):
        P.S.dma("sp" if t % 2 == 0 else "act", P.X[:, t, :], x_in[t * 128:(t + 1) * 128, :], writes=[P.bX[t]])


def prenorm(P, A, B, bAB, name):
    S, M = P.S, P.M
    if not hasattr(P, "actT"):
        P.actT = M.sb("actT", [128, KC, T], BF16)
        P.bact = [Buf() for _ in range(NT)]
        P.ss = M.sb("ss", [128, NT], F32)
        P.bss = [Buf() for _ in range(NT)]
        P.xn = [M.sb("xn%d" % i, [128, D], BF16) for i in range(1)]
        P.bxn = [Buf()]
    for t in range(NT):
        typ = 0 if t < 8 else 1
        ss = P.ss[:, t:t + 1]
        xt_, bxt_ = xtile(P, t)
        S.op("act", lambda e, xt_=xt_, ss=ss: e.activation(out=P.xn[0][:], in_=xt_, func=AF.Square, accum_out=ss),
             reads=[bxt_], writes=[P.bxn[0], P.bss[t]])
        S.op("act", lambda e, ss=ss: e.activation(out=ss, in_=ss, func=AF.Sqrt, bias=P.epsc[:, 0:1], scale=1.0 / D),
             reads=[P.bss[t], P.b_eps], writes=[P.bss[t]])
        S.op("dve", lambda e, ss=ss: e.reciprocal(out=ss, in_=ss), reads=[P.bss[t]], writes=[P.bss[t]])
        xi = 0
        xn, bxn = P.xn[xi], P.bxn[xi]
        S.op("dve", lambda e, xt_=xt_, xn=xn, ss=ss: e.tensor_scalar(out=xn[:], in0=xt_, scalar1=ss, scalar2=None,
                                                                op0=ALU.mult),
             reads=[bxt_, P.bss[t]], writes=[bxn])
        for g in range(2):
            bank = P.rr("tp", 2)
            pv = P.pb[bank][:, :].bitcast(BF16)

            def tr(e, g=g, pv=pv, xn=xn):
                ins = None
                for j in range(8):
                    k = g * 8 + j
                    ins = e.transpose(pv[:, j * 128:(j + 1) * 128], xn[:, k * 128:(k + 1) * 128], P.identb[:])
                return ins
            S.op("pe", tr, reads=[bxn, P.b_identb], writes=[P.pbb[bank]])
            for j in range(8):
                k = g * 8 + j
                eng = "act" if j % 2 == 0 else "dve"
                dst = P.actT[:, k, t * 128:(t + 1) * 128]
                src = pv[:, j * 128:(j + 1) * 128]
                if eng == "act":
                    S.op("act", lambda e, dst=dst, src=src, k=k, typ=typ: e.activation(
                        out=dst, in_=src, func=AF.Identity, bias=B[:, typ, k:k + 1], scale=A[:, typ, k:k + 1]),
                        reads=[P.pbb[bank], bAB], writes=[P.bact[t]])
                else:
                    S.op("dve", lambda e, dst=dst, src=src, k=k, typ=typ: e.tensor_scalar(
                        out=dst, in0=src, scalar1=A[:, typ, k:k + 1], scalar2=B[:, typ, k:k + 1],
                        op0=ALU.mult, op1=ALU.add),
                        reads=[P.pbb[bank], bAB], writes=[P.bact[t]])


def mod_vectors(P, modl, g_in, part_scale, part_shift, name):
    S, M = P.S, P.M
    A = M.sb("A_" + name, [128, 2, KC], F32)
    B = M.sb("B_" + name, [128, 2, KC], F32)
    bAB = Buf()
    for typ in range(2):
        S.op("dve", lambda e, typ=typ: e.tensor_scalar(
            out=A[:, typ, :], in0=modl[:, typ, part_scale * 16:(part_scale + 1) * 16], scalar1=1.0, scalar2=None,
            op0=ALU.add), reads=[P.b_modl], writes=[bAB])
        S.op("dve", lambda e, typ=typ: e.tensor_tensor(out=A[:, typ, :], in0=A[:, typ, :], in1=g_in, op=ALU.mult),
             reads=[bAB, P.b_g], writes=[bAB])
        S.op("dve", lambda e, typ=typ: e.tensor_copy(
            out=B[:, typ, :], in_=modl[:, typ, part_shift * 16:(part_shift + 1) * 16]), reads=[P.b_modl], writes=[bAB])
    return A, B, bAB


def bcast_rows(P, vec, bvec, name):
    S, M = P.S, P.M
    if not hasattr(P, "ones128"):
        P.ones128 = M.sb("ones128", [128, 128], F32)
        P.b_ones = Buf()
        S.op("pool", lambda e: e.memset(P.ones128[:], 1.0), writes=[P.b_ones])
        P.diag = [M.sb("diag%d" % i, [128, 512], F32) for i in range(2)]
        P.bdiag = [Buf(), Buf()]
    out = M.sb("bc_" + name, [128, 2, D], BF16)
    bout = Buf()
    for typ in range(2):
        for c4 in range(4):
            di = P.rr("diag", 2)
            dg, bdg = P.diag[di], P.bdiag[di]
            for j in range(4):
                k = c4 * 4 + j
                S.op("dve", lambda e, dg=dg, j=j, k=k, typ=typ: e.tensor_scalar(
                    out=dg[:, j * 128:(j + 1) * 128], in0=P.identf[:], scalar1=vec[:, typ, k:k + 1], scalar2=None,
                    op0=ALU.mult), reads=[P.b_identf, bvec], writes=[bdg])
            bank = P.rr("bc", 2) + 2
            mm_group(P, P.pb[bank][:, :], [(P.ones128[:], dg[:])], reads=[bdg, P.b_ones], writes=[P.pbb[bank]])
            S.op("act", lambda e, bank=bank, typ=typ, c4=c4: e.copy(out=out[:, typ, c4 * 512:(c4 + 1) * 512],
                                                                   in_=P.pb[bank][:, :]),
                 reads=[P.pbb[bank]], writes=[bout])
    return out, bout


def load_small(P, name, shape, dtype=F32, q="sp"):
    ap = P.din(name, shape, dtype)
    t = P.M.sb("s_" + name, shape, dtype)
    b = Buf()
    P.S.dma(q, t[:], ap, writes=[b])
    return t, b


def stream_slabs(P, w_ap, ncols, cgs, name):
    raise NotImplementedError


def build_A_even():
    P = Prog()
    S, M = P.S, P.M
    P.consts()
    x_in = P.din("x_in", [T, D])
    w_in = P.din("w_in", [D, 4096])
    q_out = P.dout("q_out", [T, 1024], BF16)
    kvu_out = P.dout("kvu_out", [T, 3072], BF16)
    modl, P.b_modl = load_small(P, "modl", [128, 2, 96])
    gmix, P.b_g = load_small(P, "gmix", [128, KC])
    gqk, b_gqk = load_small(P, "gqk", [128, 2, 64])
    rope, b_rope = load_small(P, "rope", [128, NT, 2, 32])
    stream_x(P, x_in)
    A, B, bAB = mod_vectors(P, modl, gmix[:], 1, 0, "mix")
    prenorm(P, A, B, bAB, "mix")
    slabs = [M.sb("slab%d" % i, [128, KC, 512], BF16) for i in range(2)]
    bslab = [Buf(), Buf()]
    stg = [M.sb("stg%d" % i, [128, 512], BF16) for i in range(3)]
    bstg = [Buf() for _ in range(3)]
    sqb = [M.sb("sqb%d" % i, [128, 512], F32) for i in range(2)]
    bsq = [Buf(), Buf()]
    qn = [M.sb("qn%d" % i, [128, 512], F32) for i in range(2)]
    bqn = [Buf(), Buf()]
    ra = [M.sb("ra%d" % i, [128, 8, 32], F32) for i in range(4)]
    bra = [Buf() for _ in range(4)]
    rs = M.sb("rs", [128, 2, 8], F32)
    brs = [Buf(), Buf()]
    bo = Buf()
    for cg in range(8):
        sl, bsl = slabs[cg % 2], bslab[cg % 2]
        S.dma("pool", sl[:], w_in[:, cg * 512:(cg + 1) * 512].rearrange("(k p) c -> p k c", p=128), writes=[bsl])
        for t in range(NT):
            bank = 4 + P.rr("ip", 4)
            ps = P.pb[bank]
            mm_group(P, ps[:, :], [(P.actT[:, k, t * 128:(t + 1) * 128], sl[:, k, :]) for k in range(KC)],
                     reads=[P.bact[t], bsl], writes=[P.pbb[bank]])
            si = P.rr("stg", 3)
            sg, bsg = stg[si], bstg[si]
            rows = slice(t * 128, (t + 1) * 128)
            if cg < 4:
                which = 0 if cg < 2 else 1
                i2 = P.rr("sq", 2)
                sq, bq, q_, bq_, rsv, brsv = sqb[i2], bsq[i2], qn[i2], bqn[i2], rs[:, i2, :], brs[i2]
                S.op("act", lambda e, sq=sq, ps=ps: e.activation(out=sq[:], in_=ps[:, :], func=AF.Square),
                     reads=[P.pbb[bank]], writes=[bq])
                S.op("dve", lambda e, sq=sq, rsv=rsv: e.tensor_reduce(
                    out=rsv, in_=sq[:].rearrange("p (g d) -> p g d", d=64), axis=AX.X, op=ALU.add),
                    reads=[bq], writes=[brsv])
                S.op("act", lambda e, rsv=rsv: e.activation(out=rsv, in_=rsv, func=AF.Sqrt, bias=P.epsc[:, 0:1],
                                                         scale=1.0 / 64), reads=[brsv, P.b_eps], writes=[brsv])
                S.op("dve", lambda e, rsv=rsv: e.reciprocal(out=rsv, in_=rsv), reads=[brsv], writes=[brsv])
                S.op("dve", lambda e, q_=q_, ps=ps, rsv=rsv: e.tensor_tensor(
                    out=q_[:].rearrange("p (g d) -> p g d", d=64), in0=ps[:, :].rearrange("p (g d) -> p g d", d=64),
                    in1=rsv.unsqueeze(2).to_broadcast([128, 8, 64]), op=ALU.mult),
                    reads=[P.pbb[bank], brsv], writes=[bq_])
                S.op("pool", lambda e, q_=q_, which=which: e.tensor_tensor(
                    out=q_[:].rearrange("p (g d) -> p g d", d=64), in0=q_[:].rearrange("p (g d) -> p g d", d=64),
                    in1=gqk[:, which, :].unsqueeze(1).to_broadcast([128, 8, 64]), op=ALU.mult),
                    reads=[bq_, b_gqk], writes=[bq_])
                q4 = q_[:].rearrange("p (g h d) -> p g h d", h=2, d=32)
                x1, x2 = q4[:, :, 0, :], q4[:, :, 1, :]
                cosb = rope[:, t, 0, :].unsqueeze(1).to_broadcast([128, 8, 32])
                sinb = rope[:, t, 1, :].unsqueeze(1).to_broadcast([128, 8, 32])
                r4 = [P.rr("ra", 4) for _ in range(4)]
                tmp = [(ra[i], bra[i]) for i in r4]
                S.op("dve", lambda e, o=tmp[0][0], x1=x1, cosb=cosb: e.tensor_tensor(out=o[:], in0=x1, in1=cosb, op=ALU.mult),
                     reads=[bq_, b_rope], writes=[tmp[0][1]])
                S.op("pool", lambda e, o=tmp[1][0], x2=x2, sinb=sinb: e.tensor_tensor(out=o[:], in0=x2, in1=sinb, op=ALU.mult),
                     reads=[bq_, b_rope], writes=[tmp[1][1]])
                S.op("dve", lambda e, o=tmp[2][0], x2=x2, cosb=cosb: e.tensor_tensor(out=o[:], in0=x2, in1=cosb, op=ALU.mult),
                     reads=[bq_, b_rope], writes=[tmp[2][1]])
                S.op("pool", lambda e, o=tmp[3][0], x1=x1, sinb=sinb: e.tensor_tensor(out=o[:], in0=x1, in1=sinb, op=ALU.mult),
                     reads=[bq_, b_rope], writes=[tmp[3][1]])
                s4 = sg[:].rearrange("p (g h d) -> p g h d", h=2, d=32)
                S.op("dve", lambda e, s4=s4, a=tmp[0][0], b=tmp[1][0]: e.tensor_tensor(
                    out=s4[:, :, 0, :], in0=a[:], in1=b[:], op=ALU.subtract),
                    reads=[tmp[0][1], tmp[1][1]], writes=[bsg])
                S.op("pool", lambda e, s4=s4, a=tmp[2][0], b=tmp[3][0]: e.tensor_tensor(
                    out=s4[:, :, 1, :], in0=a[:], in1=b[:], op=ALU.add),
                    reads=[tmp[2][1], tmp[3][1]], writes=[bsg])
                if cg < 2:
                    dst = q_out[rows, cg * 512:(cg + 1) * 512]
                else:
                    dst = kvu_out[rows, (cg - 2) * 512:(cg - 1) * 512]
            else:
                S.op("act", lambda e, sg=sg, ps=ps: e.copy(out=sg[:], in_=ps[:, :]), reads=[P.pbb[bank]], writes=[bsg])
                dst = kvu_out[rows, 1024 + (cg - 4) * 512:1024 + (cg - 3) * 512]
            S.dma("sp", dst, sg[:], reads=[bsg], writes=[bo])
    return P.finish([bo]), P


def out_proj(P, w_out, gate_bc, b_gate):
    S, M = P.S, P.M
    CW = 256
    slabs = [M.sb("oslab%d" % i, [128, KC, CW], BF16) for i in range(2)]
    bslab = [Buf(), Buf()]
    tmp = [M.sb("otmp%d" % i, [128, CW], F32) for i in range(2)]
    btmp = [Buf(), Buf()]
    for cg in range(D // CW):
        sl, bsl = slabs[cg % 2], bslab[cg % 2]
        S.dma("pool", sl[:], w_out[:, cg * CW:(cg + 1) * CW].rearrange("(k p) c -> p k c", p=128), writes=[bsl])
        for t in range(NT):
            typ = 0 if t < 8 else 1
            bank = 6 + P.rr("op", 2)
            ps = P.pb[bank][:, 0:CW]
            mm_group(P, ps, [(P.actT[:, k, t * 128:(t + 1) * 128], sl[:, k, :]) for k in range(KC)],
                     reads=[P.bact[t], bsl], writes=[P.pbb[bank]])
            ti = P.rr("otmp", 2)
            tm, btm = tmp[ti], btmp[ti]
            S.op("dve", lambda e, tm=tm, ps=ps, typ=typ, cg=cg: e.tensor_tensor(
                out=tm[:], in0=ps, in1=gate_bc[:, typ, cg * CW:(cg + 1) * CW], op=ALU.mult),
                reads=[P.pbb[bank], b_gate], writes=[btm])
            xs = P.X[:, t, cg * CW:(cg + 1) * CW]
            S.op("pool", lambda e, xs=xs, tm=tm: e.tensor_tensor(out=xs, in0=xs, in1=tm[:], op=ALU.add),
                 reads=[btm, P.bX[t]], writes=[P.bX[t]])


def moe(P, w_router, b_router_bc, w_gu_r, b_guT, w_down, b_down, gate_bc, b_gate, n_exp=NEXP):
    S, M = P.S, P.M
    wr = M.sb("wr", [128, KC, NEXP], BF16)
    bwr = Buf()
    S.dma("pool", wr[:], w_router, writes=[bwr])
    brt, bbrt = b_router_bc
    G = M.sb("G", [128, NT, NEXP], F32)
    bG = [Buf() for _ in range(NT)]
    lg = M.sb("lg", [128, NT, NEXP], F32)
    mx = M.sb("mx", [128, NT, 8], F32)
    den = M.sb("den", [128, NT, 2], F32)
    for t in range(NT):
        bank = P.rr("rt", 2)
        ps = P.pb[bank][:, 0:NEXP]
        mm_group(P, ps, [(P.actT[:, k, t * 128:(t + 1) * 128], wr[:, k, :]) for k in range(KC)],
                 reads=[P.bact[t], bwr], writes=[P.pbb[bank]])
        l_, m_, g_, d_ = lg[:, t, :], mx[:, t, :], G[:, t, :], den[:, t, :]
        S.op("dve", lambda e, l_=l_, ps=ps: e.tensor_tensor(out=l_, in0=ps, in1=brt[:], op=ALU.add),
             reads=[P.pbb[bank], bbrt], writes=[bG[t]])
        S.op("dve", lambda e, l_=l_, m_=m_: e.max(out=m_, in_=l_), reads=[bG[t]], writes=[bG[t]])
        S.op("dve", lambda e, m_=m_, d_=d_: e.tensor_scalar(out=d_[:, 0:1], in0=m_[:, 0:1], scalar1=-1.0, scalar2=None,
                                                         op0=ALU.mult), reads=[bG[t]], writes=[bG[t]])
        S.op("act", lambda e, g_=g_, l_=l_, d_=d_: e.activation(out=g_, in_=l_, func=AF.Exp, bias=d_[:, 0:1], scale=1.0),
             reads=[bG[t]], writes=[bG[t]])
        S.op("dve", lambda e, l_=l_, m_=m_: e.tensor_scalar(out=l_, in0=l_, scalar1=m_[:, 3:4], scalar2=None, op0=ALU.is_ge),
             reads=[bG[t]], writes=[bG[t]])
        S.op("dve", lambda e, g_=g_, l_=l_: e.tensor_tensor(out=g_, in0=g_, in1=l_, op=ALU.mult), reads=[bG[t]], writes=[bG[t]])
        S.op("dve", lambda e, g_=g_, d_=d_: e.tensor_reduce(out=d_[:, 1:2], in_=g_, axis=AX.X, op=ALU.add),
             reads=[bG[t]], writes=[bG[t]])
        S.op("dve", lambda e, d_=d_: e.reciprocal(out=d_[:, 1:2], in_=d_[:, 1:2]), reads=[bG[t]], writes=[bG[t]])
        S.op("dve", lambda e, g_=g_, d_=d_: e.tensor_scalar(out=g_, in0=g_, scalar1=d_[:, 1:2], scalar2=None, op0=ALU.mult),
             reads=[bG[t]], writes=[bG[t]])
    bgu = M.sb("bgu", [128, NEXP, 12], F32)
    bbgu = Buf()
    S.dma("sp", bgu[:], b_guT, writes=[bbgu])
    bdn = M.sb("bdn", [NEXP, D], BF16)
    bbdn = Buf()
    S.dma("pool", bdn[:], b_down, writes=[bbdn])
    GT = M.sb("GT", [NEXP, T], BF16)
    bGT = [Buf() for _ in range(NT)]
    tmp = [M.sb("mtmp%d" % i, [128, 512], F32) for i in range(2)]
    btmp = [Buf(), Buf()]
    for t in range(NT):
        typ = 0 if t < 8 else 1
        bank = P.rr("rt", 2)
        pt = P.pb[bank][0:NEXP, 0:128]
        S.op("pe", lambda e, pt=pt, t=t: e.transpose(pt, G[:, t, :], P.identf[:]), reads=[bG[t], P.b_identf],
             writes=[P.pbb[bank]])
        S.op("act", lambda e, pt=pt, t=t: e.copy(out=GT[:, t * 128:(t + 1) * 128], in_=pt), reads=[P.pbb[bank]],
             writes=[bGT[t]])
        for nb in range(4):
            bank = 4 + nb
            ps = P.pb[bank]
            mm_group(P, ps[:, :], [(GT[:, t * 128:(t + 1) * 128], bdn[:, nb * 512:(nb + 1) * 512])],
                     reads=[bGT[t], bbdn], writes=[P.pbb[bank]])
            ti = P.rr("mtmp", 2)
            tm, btm = tmp[ti], btmp[ti]
            S.op("dve", lambda e, tm=tm, ps=ps, typ=typ, nb=nb: e.tensor_tensor(
                out=tm[:], in0=ps[:, :], in1=gate_bc[:, typ, nb * 512:(nb + 1) * 512], op=ALU.mult),
                reads=[P.pbb[bank], b_gate], writes=[btm])
            xs = P.X[:, t, nb * 512:(nb + 1) * 512]
            S.op("pool", lambda e, xs=xs, tm=tm: e.tensor_tensor(out=xs, in0=xs, in1=tm[:], op=ALU.add),
                 reads=[btm, P.bX[t]], writes=[P.bX[t]])
    slabs = [M.sb("gslab%d" % i, [128, KC, 256], BF16) for i in range(2)]
    bslab = [Buf(), Buf()]
    wd = M.sb("wd", [128, 6, D], BF16)
    bwd = Buf()
    gact = M.sb("gact", [128, 6, T], BF16)
    bga = [Buf() for _ in range(3)]
    gc = [M.sb("gc%d" % i, [128, 512], F32) for i in range(2)]
    sg = [M.sb("sgm%d" % i, [128, 512], BF16) for i in range(2)]
    uc = [M.sb("uc%d" % i, [128, 512], BF16) for i in range(2)]
    bgc, bsg, buc = [Buf(), Buf()], [Buf(), Buf()], [Buf(), Buf()]
    si = 0
    for e_ in range(n_exp):
        S.dma("pool", wd[:], w_down[e_].rearrange("(k p) c -> p k c", p=128), writes=[bwd])
        for i in range(6):
            sl, bsl = slabs[si % 2], bslab[si % 2]
            si += 1
            S.dma("pool", sl[:], w_gu_r[e_, i].rearrange("(k p) c -> p k c", p=128), writes=[bsl])
            for jj in range(1):
                for ng, (n0, nsz) in enumerate(NTILES_N):
                    tl = [n0 // 128 + x for x in range(nsz // 128)]
                    rd = [P.bact[t] for t in tl]
                    bkg, bku = P.rr("gu", 2) * 2, None
                    bku = bkg + 1
                    pg, pu = P.pb[bkg][:, 0:nsz], P.pb[bku][:, 0:nsz]
                    mm_group(P, pg, [(sl[:, k, 0:128], P.actT[:, k, n0:n0 + nsz]) for k in range(KC)],
                             reads=rd + [bsl], writes=[P.pbb[bkg]])
                    mm_group(P, pu, [(sl[:, k, 128:256], P.actT[:, k, n0:n0 + nsz]) for k in range(KC)],
                             reads=rd + [bsl], writes=[P.pbb[bku]])
                    x = P.rr("sw", 2)
                    gcv, sgv, ucv = gc[x][:, 0:nsz], sg[x][:, 0:nsz], uc[x][:, 0:nsz]
                    S.op("dve", lambda e, gcv=gcv, pg=pg, e_=e_, i=i: e.tensor_scalar(
                        out=gcv, in0=pg, scalar1=bgu[:, e_, i:i + 1], scalar2=7.0, op0=ALU.add, op1=ALU.min),
                        reads=[P.pbb[bkg], bbgu], writes=[bgc[x]])
                    S.op("act", lambda e, sgv=sgv, gcv=gcv: e.activation(out=sgv, in_=gcv, func=AF.Sigmoid, scale=1.702),
                         reads=[bgc[x]], writes=[bsg[x]])
                    S.op("dve", lambda e, ucv=ucv, pu=pu, e_=e_, i=i: e.tensor_scalar(
                        out=ucv, in0=pu, scalar1=bgu[:, e_, 6 + i:7 + i], scalar2=7.0, op0=ALU.add, op1=ALU.min),
                        reads=[P.pbb[bku], bbgu], writes=[buc[x]])
                    S.op("pool", lambda e, ucv=ucv: e.tensor_scalar(out=ucv, in0=ucv, scalar1=-7.0, scalar2=1.0,
                                                                   op0=ALU.max, op1=ALU.add), reads=[buc[x]], writes=[buc[x]])
                    S.op("pool", lambda e, gcv=gcv, sgv=sgv: e.tensor_tensor(out=gcv, in0=gcv, in1=sgv, op=ALU.mult),
                         reads=[bgc[x], bsg[x]], writes=[bgc[x]])
                    S.op("pool", lambda e, gcv=gcv, ucv=ucv, i=i, n0=n0, nsz=nsz: e.tensor_tensor(
                        out=gact[:, i, n0:n0 + nsz], in0=gcv, in1=ucv, op=ALU.mult),
                        reads=[bgc[x], buc[x]], writes=[bga[ng]])
        for t in range(NT):
            typ = 0 if t < 8 else 1
            ng = 0 if t < 4 else (1 if t < 8 else 2)
            for nb in range(4):
                bank = 4 + nb
                ps = P.pb[bank]
                pairs = [(gact[:, i, t * 128:(t + 1) * 128], wd[:, i, nb * 512:(nb + 1) * 512]) for i in range(6)]
                mm_group(P, ps[:, :], pairs, reads=[bga[ng], bwd], writes=[P.pbb[bank]])
                ti = P.rr("mtmp", 2)
                tm, btm = tmp[ti], btmp[ti]
                S.op("dve", lambda e, tm=tm, ps=ps, t=t, e_=e_, typ=typ, nb=nb: e.scalar_tensor_tensor(
                    out=tm[:], in0=ps[:, :], scalar=G[:, t, e_:e_ + 1], in1=gate_bc[:, typ, nb * 512:(nb + 1) * 512],
                    op0=ALU.mult, op1=ALU.mult), reads=[P.pbb[bank], bG[t], b_gate], writes=[btm])
                xs = P.X[:, t, nb * 512:(nb + 1) * 512]
                S.op("pool", lambda e, xs=xs, tm=tm: e.tensor_tensor(out=xs, in0=xs, in1=tm[:], op=ALU.add),
                     reads=[btm, P.bX[t]], writes=[P.bX[t]])


def store_x(P, x_out, rows=NT):
    bo = Buf()
    for t in range(rows):
        P.S.dma("sp", x_out[t * 128:(t + 1) * 128, :], P.X[:, t, :], reads=[P.bX[t]], writes=[bo])
    return bo


def moe_inputs(P):
    w_router = P.din("w_router", [128, KC, NEXP])
    brt = load_small(P, "b_router", [128, NEXP])
    w_gu_r = P.din("w_gu_r", [NEXP, 6, D, 256])
    b_guT = P.din("b_guT", [128, NEXP, 12])
    w_down = P.din("w_down", [NEXP, DEXP, D])
    b_down = P.din("b_down", [NEXP, D])
    return w_router, brt, w_gu_r, b_guT, w_down, b_down


def build_moe_test(n_exp=NEXP):
    P = Prog()
    P.consts()
    x_in = P.din("x_in", [T, D])
    x_out = P.dout("x_out", [T, D])
    modl, P.b_modl = load_small(P, "modl", [128, 2, 96])
    gffn, P.b_g = load_small(P, "gffn", [128, KC])
    mi = moe_inputs(P)
    load_x(P, x_in)
    A, B, bAB = mod_vectors(P, modl, gffn[:], 4, 3, "ffn")
    prenorm(P, A, B, bAB, "ffn")
    g2 = P.M.sb("g2v", [128, 2, KC], F32)
    bg2 = Buf()
    P.S.op("dve", lambda e: e.tensor_copy(out=g2[:], in_=modl[:, :, 80:96]), reads=[P.b_modl], writes=[bg2])
    gate_bc, b_gate = bcast_rows(P, g2, bg2, "g2")
    moe(P, *mi, gate_bc, b_gate, n_exp=n_exp)
    bo = store_x(P, x_out)
    return P.finish([bo]), P


QGROUPS = [(0, 512), (512, 512), (1024, 128)]


def build_B1_even(lam_init):
    P = Prog()
    S, M = P.S, P.M
    P.consts()
    x_in = P.din("x_in", [T, D])
    q_in = P.din("q_in", [T, 1024], BF16)
    kvu = P.din("kvu_in", [2304, 3072], BF16)
    poolA = P.din("poolA", [4, NT, 3, 128, 128], BF16)
    w_pool = P.din("w_pool", [4, 256, 256])
    w_out = P.din("w_out", [D, D])
    x_out = P.dout("x_out", [T, D])
    modl, P.b_modl = load_small(P, "modl", [128, 2, 96])
    spool, b_spool = load_small(P, "s_poolT", [128, 8])
    lamr, b_lamr = load_small(P, "lam", [128, 256])
    gsub, b_gsub = load_small(P, "g_subln", [128, 128])
    load_x(P, x_in)
    P.actT = M.sb("actT", [128, KC, T], BF16)
    P.bact = [Buf() for _ in range(NT)]
    sc = M.sb("sc", [128, 8], F32)
    bsc = Buf()
    lp = M.sb("lp", [128, 128], F32)
    S.op("dve", lambda e: e.tensor_tensor(out=lp[:, 0:64], in0=lamr[:, 0:64], in1=lamr[:, 64:128], op=ALU.mult),
         reads=[b_lamr], writes=[bsc])
    S.op("dve", lambda e: e.tensor_tensor(out=lp[:, 64:128], in0=lamr[:, 128:192], in1=lamr[:, 192:256], op=ALU.mult),
         reads=[b_lamr, bsc], writes=[bsc])
    S.op("dve", lambda e: e.tensor_reduce(out=sc[:, 0:2], in_=lp[:].rearrange("p (a d) -> p a d", d=64), axis=AX.X, op=ALU.add),
         reads=[bsc], writes=[bsc])
    S.op("act", lambda e: e.activation(out=sc[:, 2:4], in_=sc[:, 0:2], func=AF.Exp), reads=[bsc], writes=[bsc])
    S.op("dve", lambda e: e.tensor_tensor(out=sc[:, 4:5], in0=sc[:, 3:4], in1=sc[:, 2:3], op=ALU.subtract), reads=[bsc], writes=[bsc])
    S.op("dve", lambda e: e.tensor_scalar(out=sc[:, 4:5], in0=sc[:, 4:5], scalar1=-float(lam_init), scalar2=None, op0=ALU.add),
         reads=[bsc], writes=[bsc])
    neglam = sc[:, 4:5]
    S.op("dve", lambda e: e.tensor_scalar(out=gsub[:], in0=gsub[:], scalar1=float(1.0 - lam_init), scalar2=None, op0=ALU.mult),
         reads=[b_gsub], writes=[b_gsub])
    Kl = M.sb("Kl", [128, 18, 128], BF16)
    Va = M.sb("Va", [128, 18, 129], BF16)
    Ql = M.sb("Ql", [128, NT, 128], BF16)
    KT = M.sb("KT", [128, 2304], BF16)
    QT = M.sb("QT", [128, T], BF16)
    PT = [M.sb("PT%d" % i, [128, 18, 512], BF16) for i in range(1)]
    o0 = M.sb("o0", [128, NT, 128], F32)
    ob = M.sb("ob", [128, 128], F32)
    ytok = M.sb("ytok", [128, 128], BF16)
    sm = M.sb("sm", [128, 8], F32)
    bKl, bVa, bQl, bKT, bQT, bo0, bob, byt, bsm = [Buf() for _ in range(9)]
    bPT = [Buf()]
    S.op("pool", lambda e: e.memset(Va[:, :, 128:129], 1.0), writes=[bVa])
    for h in range(8):
        S.dma("sp", Kl[:], kvu[:, h * 128:(h + 1) * 128].rearrange("(t p) c -> p t c", p=128), writes=[bKl])
        S.dma("act", Va[:, :, 0:128], kvu[:, 1024 + h * 128:1024 + (h + 1) * 128].rearrange("(t p) c -> p t c", p=128),
              writes=[bVa])
        S.dma("sp", Ql[:], q_in[:, h * 128:(h + 1) * 128].rearrange("(t p) c -> p t c", p=128), writes=[bQl])
        for (src, bsrc, ntl, dst, bdst) in ((Kl, bKl, 18, KT, bKT), (Ql, bQl, NT, QT, bQT)):
            for g0 in range(0, ntl, 8):
                n = min(8, ntl - g0)
                bank = P.rr("tp", 2)
                pv = P.pb[bank][:, :].bitcast(BF16)

                def tr(e, src=src, g0=g0, n=n, pv=pv):
                    ins = None
                    for j in range(n):
                        ins = e.transpose(pv[:, j * 128:(j + 1) * 128], src[:, g0 + j, :], P.identb[:])
                    return ins
                S.op("pe", tr, reads=[bsrc, P.b_identb], writes=[P.pbb[bank]])
                eng = "act" if (g0 // 8) % 2 == 0 else "dve"
                if eng == "act":
                    S.op("act", lambda e, dst=dst, g0=g0, n=n, pv=pv: e.copy(out=dst[:, g0 * 128:(g0 + n) * 128], in_=pv[:, 0:n * 128]),
                         reads=[P.pbb[bank]], writes=[bdst])
                else:
                    S.op("dve", lambda e, dst=dst, g0=g0, n=n, pv=pv: e.tensor_copy(out=dst[:, g0 * 128:(g0 + n) * 128], in_=pv[:, 0:n * 128]),
                         reads=[P.pbb[bank]], writes=[bdst])
        for m in range(2):
            ms = slice(m * 64, (m + 1) * 64)
            for (q0, qn_) in QGROUPS:
                ktiles = list(range(18)) if q0 < 1024 else [16, 17]
                pi = 0
                pt, bpt = PT[pi], bPT[pi]
                for kt in ktiles:
                    bank = 2 + P.rr("sc", 2)
                    ps = P.pb[bank][:, 0:qn_]
                    mm_group(P, ps, [(KT[ms, kt * 128:(kt + 1) * 128], QT[ms, q0:q0 + qn_])], reads=[bKT, bQT],
                             writes=[P.pbb[bank]])
                    S.op("act", lambda e, pt=pt, kt=kt, ps=ps, qn_=qn_: e.activation(
                        out=pt[:, kt, 0:qn_], in_=ps, func=AF.Exp, scale=0.125), reads=[P.pbb[bank]], writes=[bpt])
                for qq in range(qn_ // 128):
                    t = q0 // 128 + qq
                    bank = 4 + P.rr("av", 2)
                    po = P.pb[bank][:, 0:129]
                    mm_group(P, po, [(pt[:, kt, qq * 128:(qq + 1) * 128], Va[:, kt, :]) for kt in ktiles],
                             reads=[bpt, bVa], writes=[P.pbb[bank]])
                    S.op("dve", lambda e, po=po: e.reciprocal(out=sm[:, 0:1], in_=po[:, 128:129]), reads=[P.pbb[bank]],
                         writes=[bsm])
                    if m == 0:
                        S.op("dve", lambda e, po=po, t=t: e.tensor_scalar(out=o0[:, t, :], in0=po[:, 0:128], scalar1=sm[:, 0:1],
                                                                        scalar2=None, op0=ALU.mult),
                             reads=[P.pbb[bank], bsm], writes=[bo0])
                    else:
                        S.op("dve", lambda e: e.tensor_tensor(out=sm[:, 1:2], in0=sm[:, 0:1], in1=neglam, op=ALU.mult),
                             reads=[bsm, bsc], writes=[bsm])
                        S.op("dve", lambda e, po=po, t=t: e.scalar_tensor_tensor(
                            out=ob[:], in0=po[:, 0:128], scalar=sm[:, 1:2], in1=o0[:, t, :], op0=ALU.mult, op1=ALU.add),
                            reads=[P.pbb[bank], bsm, bo0], writes=[bob])
                        S.op("act", lambda e: e.activation(out=ytok[:], in_=ob[:], func=AF.Square, accum_out=sm[:, 2:3]),
                             reads=[bob], writes=[byt, bsm])
                        S.op("act", lambda e: e.activation(out=sm[:, 2:3], in_=sm[:, 2:3], func=AF.Sqrt, bias=P.epsc[:, 0:1],
                                                           scale=1.0 / 128), reads=[bsm, P.b_eps], writes=[bsm])
                        S.op("dve", lambda e: e.reciprocal(out=sm[:, 2:3], in_=sm[:, 2:3]), reads=[bsm], writes=[bsm])
                        S.op("dve", lambda e: e.scalar_tensor_tensor(out=ytok[:], in0=ob[:], scalar=sm[:, 2:3], in1=gsub[:],
                                                                     op0=ALU.mult, op1=ALU.mult),
                             reads=[bob, bsm, b_gsub], writes=[byt])
                        bk2 = P.rr("tp", 2)
                        pv = P.pb[bk2][:, :].bitcast(BF16)
                        S.op("pe", lambda e, pv=pv: e.transpose(pv[:, 0:128], ytok[:], P.identb[:]), reads=[byt, P.b_identb],
                             writes=[P.pbb[bk2]])
                        S.op("act", lambda e, pv=pv, h=h, t=t: e.copy(out=P.actT[:, h, t * 128:(t + 1) * 128], in_=pv[:, 0:128]),
                             reads=[P.pbb[bk2]], writes=[P.bact[t]])
    Ug = M.sb("Ug", [128, 18, 256], BF16)
    PA = M.sb("PA", [128, NT * 3, 128], BF16)
    pooledT = M.sb("pooledT", [128, 2, T], BF16)
    wp = M.sb("wp", [128, 4, 2, 256], BF16)
    bUg, bPA, bpl, bwp = Buf(), Buf(), Buf(), Buf()
    S.dma("pool", wp[:], w_pool.rearrange("g (c p) e -> p g c e", p=128), writes=[bwp])
    for g in range(4):
        S.dma("sp", Ug[:], kvu[:, 2048 + g * 256:2048 + (g + 1) * 256].rearrange("(t p) c -> p t c", p=128), writes=[bUg])
        S.dma("act", PA[:], poolA[g].rearrange("j k p t -> p (j k) t"), writes=[bPA])
        for j in range(NT):
            if j < 8:
                kts = [(j - 1) if j > 0 else 15, j, (j + 1) if j < 7 else 8]
            else:
                kts = [17, 16, 17]
            bank = 6 + P.rr("op", 2)
            for cc in range(2):
                ps = P.pb[bank][:, cc * 128:(cc + 1) * 128]
                mm_group(P, ps, [(Ug[:, kts[kk], cc * 128:(cc + 1) * 128], PA[:, j * 3 + kk, :]) for kk in range(3)],
                         reads=[bUg, bPA] + ([P.pbb[bank]] if cc else []), writes=[P.pbb[bank]])
            S.op("act", lambda e, bank=bank, j=j: e.copy(
                out=pooledT[:, :, j * 128:(j + 1) * 128], in_=P.pb[bank][:, 0:256].rearrange("p (c t) -> p c t", c=2)),
                reads=[P.pbb[bank]], writes=[bpl])
        for ec in range(2):
            for (n0, nsz) in QGROUPS:
                bank = 6 + P.rr("op", 2)
                ps = P.pb[bank][:, 0:nsz]
                mm_group(P, ps, [(wp[:, g, cc, ec * 128:(ec + 1) * 128], pooledT[:, cc, n0:n0 + nsz]) for cc in range(2)],
                         reads=[bwp, bpl], writes=[P.pbb[bank]])
                ch = 8 + g * 2 + ec
                tl = [P.bact[n0 // 128 + x] for x in range(nsz // 128)]
                S.op("act", lambda e, ps=ps, ch=ch, n0=n0, nsz=nsz, g=g, ec=ec: e.activation(
                    out=P.actT[:, ch, n0:n0 + nsz], in_=ps, func=AF.Identity, scale=spool[:, g * 2 + ec:g * 2 + ec + 1]),
                    reads=[P.pbb[bank], b_spool], writes=tl)
    g1 = M.sb("g1v", [128, 2, KC], F32)
    bg1 = Buf()
    S.op("dve", lambda e: e.tensor_copy(out=g1[:], in_=modl[:, :, 32:48]), reads=[P.b_modl], writes=[bg1])
    gate_bc, b_gate = bcast_rows(P, g1, bg1, "g1")
    out_proj(P, w_out, gate_bc, b_gate)
    bo = store_x(P, x_out)
    return P.finish([bo]), P


def rms_rstd(P, sq_ap, out_ap, n, reads, bout):
    S = P.S
    S.op("act", lambda e: e.activation(out=out_ap, in_=out_ap, func=AF.Sqrt, bias=P.epsc[:, 0:1], scale=1.0 / n),
         reads=[bout, P.b_eps] + reads, writes=[bout])
    S.op("dve", lambda e: e.reciprocal(out=out_ap, in_=out_ap), reads=[bout], writes=[bout])


def build_A_odd():
    P = Prog()
    S, M = P.S, P.M
    P.consts()
    x_in = P.din("x_in", [T, D])
    w_in = P.din("w_in", [D, 1600])
    w_qb = P.din("w_qb", [512, 2304])
    w_kvb = P.din("w_kvb", [512, 3072])
    q_out = P.dout("q_out", [T, 2304], BF16)
    kvu_out = P.dout("kvu_out", [T, 4352], BF16)
    modl, P.b_modl = load_small(P, "modl", [128, 2, 96])
    gmix, P.b_g = load_small(P, "gmix", [128, KC])
    gqa, b_gqa = load_small(P, "g_qa", [128, 512])
    gkva, b_gkva = load_small(P, "g_kva", [128, 512])
    gmq, b_gmq = load_small(P, "g_mq", [128, 192])
    gmk, b_gmk = load_small(P, "g_mk", [128, 192])
    rope, b_rope = load_small(P, "rope", [128, NT, 2, 32])
    stream_x(P, x_in)
    A, B, bAB = mod_vectors(P, modl, gmix[:], 1, 0, "mix")
    prenorm(P, A, B, bAB, "mix")
    wq = M.sb("wq", [128, 4, 2304], BF16)
    wkv = M.sb("wkv", [128, 4, 3072], BF16)
    bwq, bwkv = Buf(), Buf()
    S.dma("pool", wq[:], w_qb.rearrange("(k p) c -> p k c", p=128), writes=[bwq])
    S.dma("pool", wkv[:], w_kvb.rearrange("(k p) c -> p k c", p=128), writes=[bwkv])
    slabs = [M.sb("slab%d" % i, [128, KC, 512], BF16) for i in range(2)]
    bslab = [Buf(), Buf()]
    cT = [M.sb("cqT", [128, 4, T], BF16), M.sb("ckvT", [128, 4, T], BF16)]
    bcT = [[Buf() for _ in range(NT)] for _ in range(2)]
    kpe = M.sb("kpe", [128, NT, 64], F32)
    bkpe = [Buf() for _ in range(NT)]
    sspe = M.sb("sspe", [128, NT], F32)
    stg = [M.sb("stg%d" % i, [128, 640], BF16) for i in range(2)]
    bstg = [Buf(), Buf()]
    sqb = M.sb("sqb", [128, 512], F32)
    bsq = Buf()
    cn = M.sb("cn", [128, 512], BF16)
    bcn = Buf()
    qn = M.sb("qn", [128, 512], F32)
    bqn = Buf()
    ra = [M.sb("ra%d" % i, [128, 2, 32], F32) for i in range(4)]
    bra = [Buf() for _ in range(4)]
    rs = M.sb("rs", [128, 4], F32)
    brs = Buf()
    bo = Buf()
    cgs = [(0, 512), (512, 512), (1024, 512), (1536, 64)]
    for cg, (c0, cw) in enumerate(cgs):
        sl, bsl = slabs[cg % 2], bslab[cg % 2]
        S.dma("pool", sl[:, :, 0:cw], w_in[:, c0:c0 + cw].rearrange("(k p) c -> p k c", p=128), writes=[bsl])
        for t in range(NT):
            bank = 4 + P.rr("ip", 4)
            ps = P.pb[bank][:, 0:cw]
            mm_group(P, ps, [(P.actT[:, k, t * 128:(t + 1) * 128], sl[:, k, 0:cw]) for k in range(KC)],
                     reads=[P.bact[t], bsl], writes=[P.pbb[bank]])
            rows = slice(t * 128, (t + 1) * 128)
            if cg < 2:
                gvec, bg_ = (gqa, b_gqa) if cg == 0 else (gkva, b_gkva)
                S.op("act", lambda e, ps=ps: e.activation(out=sqb[:], in_=ps, func=AF.Square, accum_out=rs[:, 0:1]),
                     reads=[P.pbb[bank]], writes=[bsq, brs])
                rms_rstd(P, None, rs[:, 0:1], 512, [], brs)
                S.op("dve", lambda e, ps=ps, gvec=gvec: e.scalar_tensor_tensor(
                    out=cn[:], in0=ps, scalar=rs[:, 0:1], in1=gvec[:], op0=ALU.mult, op1=ALU.mult),
                    reads=[P.pbb[bank], brs, bg_], writes=[bcn])
                bk2 = P.rr("tp", 2)
                pv = P.pb[bk2][:, :].bitcast(BF16)

                def tr(e, pv=pv):
                    ins = None
                    for j in range(4):
                        ins = e.transpose(pv[:, j * 128:(j + 1) * 128], cn[:, j * 128:(j + 1) * 128], P.identb[:])
                    return ins
                S.op("pe", tr, reads=[bcn, P.b_identb], writes=[P.pbb[bk2]])
                S.op("act", lambda e, pv=pv, cg=cg, t=t: e.copy(
                    out=cT[cg][:, :, t * 128:(t + 1) * 128], in_=pv[:, 0:512].rearrange("p (c t) -> p c t", c=4)),
                    reads=[P.pbb[bk2]], writes=[bcT[cg][t]])
            elif cg == 2:
                si = P.rr("stg", 2)
                S.op("act", lambda e, si=si, ps=ps: e.copy(out=stg[si][:, 0:512], in_=ps), reads=[P.pbb[bank]], writes=[bstg[si]])
                S.dma("sp", kvu_out[rows, 3840:4352], stg[si][:, 0:512], reads=[bstg[si]], writes=[bo])
            else:
                S.op("act", lambda e, ps=ps, t=t: e.copy(out=kpe[:, t, :], in_=ps), reads=[P.pbb[bank]], writes=[bkpe[t]])
                S.op("act", lambda e, t=t: e.activation(out=sqb[:, 0:64], in_=kpe[:, t, :], func=AF.Square,
                                                      accum_out=sspe[:, t:t + 1]), reads=[bkpe[t]], writes=[bsq, bkpe[t]])

    def rope2(src4, dst4, t, breads, bdst):
        x1, x2 = src4[:, :, 0, :], src4[:, :, 1, :]
        cosb = rope[:, t, 0, :].unsqueeze(1).to_broadcast([128, 2, 32])
        sinb = rope[:, t, 1, :].unsqueeze(1).to_broadcast([128, 2, 32])
        ids = [P.rr("ra", 4) for _ in range(4)]
        tm = [(ra[i], bra[i]) for i in ids]
        S.op("dve", lambda e: e.tensor_tensor(out=tm[0][0][:], in0=x1, in1=cosb, op=ALU.mult), reads=breads + [b_rope], writes=[tm[0][1]])
        S.op("pool", lambda e: e.tensor_tensor(out=tm[1][0][:], in0=x2, in1=sinb, op=ALU.mult), reads=breads + [b_rope], writes=[tm[1][1]])
        S.op("dve", lambda e: e.tensor_tensor(out=tm[2][0][:], in0=x2, in1=cosb, op=ALU.mult), reads=breads + [b_rope], writes=[tm[2][1]])
        S.op("pool", lambda e: e.tensor_tensor(out=tm[3][0][:], in0=x1, in1=sinb, op=ALU.mult), reads=breads + [b_rope], writes=[tm[3][1]])
        S.op("dve", lambda e: e.tensor_tensor(out=dst4[:, :, 0, :], in0=tm[0][0][:], in1=tm[1][0][:], op=ALU.subtract),
             reads=[tm[0][1], tm[1][1]], writes=[bdst])
        S.op("pool", lambda e: e.tensor_tensor(out=dst4[:, :, 1, :], in0=tm[2][0][:], in1=tm[3][0][:], op=ALU.add),
             reads=[tm[2][1], tm[3][1]], writes=[bdst])

    for g6 in range(6):
        for t in range(NT):
            bank = 4 + P.rr("ip", 4)
            ps = P.pb[bank][:, 0:384]
            mm_group(P, ps, [(cT[0][:, c, t * 128:(t + 1) * 128], wq[:, c, g6 * 384:(g6 + 1) * 384]) for c in range(4)],
                     reads=[bcT[0][t], bwq], writes=[P.pbb[bank]])
            S.op("act", lambda e, ps=ps: e.activation(out=sqb[:, 0:384], in_=ps, func=AF.Square), reads=[P.pbb[bank]], writes=[bsq])
            S.op("dve", lambda e: e.tensor_reduce(out=rs[:, 0:2], in_=sqb[:, 0:384].rearrange("p (h d) -> p h d", d=192),
                                                  axis=AX.X, op=ALU.add), reads=[bsq], writes=[brs])
            rms_rstd(P, None, rs[:, 0:2], 192, [], brs)
            q3 = qn[:, 0:384].rearrange("p (h d) -> p h d", d=192)
            S.op("dve", lambda e, ps=ps, q3=q3: e.tensor_tensor(out=q3, in0=ps.rearrange("p (h d) -> p h d", d=192),
                                                              in1=rs[:, 0:2].unsqueeze(2).to_broadcast([128, 2, 192]), op=ALU.mult),
                 reads=[P.pbb[bank], brs], writes=[bqn])
            S.op("pool", lambda e, q3=q3: e.tensor_tensor(out=q3, in0=q3, in1=gmq[:].unsqueeze(1).to_broadcast([128, 2, 192]),
                                                         op=ALU.mult), reads=[bqn, b_gmq], writes=[bqn])
            si = P.rr("stg", 2)
            s3 = stg[si][:, 0:384].rearrange("p (h d) -> p h d", d=192)
            S.op("act", lambda e, s3=s3, q3=q3: e.copy(out=s3[:, :, 0:128], in_=q3[:, :, 0:128]), reads=[bqn], writes=[bstg[si]])
            rope2(q3[:, :, 128:192].rearrange("p h (a d) -> p h a d", a=2), s3[:, :, 128:192].rearrange("p h (a d) -> p h a d", a=2),
                  t, [bqn], bstg[si])
            S.dma("sp", q_out[t * 128:(t + 1) * 128, g6 * 384:(g6 + 1) * 384], stg[si][:, 0:384], reads=[bstg[si]], writes=[bo])
    for g6 in range(6):
        for t in range(NT):
            bank = 4 + P.rr("ip", 4)
            ps = P.pb[bank][:, 0:512]
            mm_group(P, ps, [(cT[1][:, c, t * 128:(t + 1) * 128], wkv[:, c, g6 * 512:(g6 + 1) * 512]) for c in range(4)],
                     reads=[bcT[1][t], bwkv], writes=[P.pbb[bank]])
            p4 = ps.rearrange("p (h a d) -> p h a d", h=2, a=2)
            S.op("act", lambda e, p4=p4: e.activation(out=sqb[:, 0:256].rearrange("p (h d) -> p h d", d=128), in_=p4[:, :, 0, :],
                                                    func=AF.Square), reads=[P.pbb[bank]], writes=[bsq])
            S.op("dve", lambda e: e.tensor_reduce(out=rs[:, 0:2], in_=sqb[:, 0:256].rearrange("p (h d) -> p h d", d=128),
                                                  axis=AX.X, op=ALU.add), reads=[bsq], writes=[brs])
            S.op("dve", lambda e, t=t: e.tensor_scalar(out=rs[:, 0:2], in0=rs[:, 0:2], scalar1=sspe[:, t:t + 1], scalar2=None,
                                                      op0=ALU.add), reads=[brs, bkpe[t]], writes=[brs])
            rms_rstd(P, None, rs[:, 0:2], 192, [], brs)
            si = P.rr("stg", 2)
            s3 = stg[si][:, 0:640].rearrange("p (h d) -> p h d", d=320)
            k3 = qn[:, 0:256].rearrange("p (h d) -> p h d", d=128)
            S.op("dve", lambda e, p4=p4, k3=k3: e.tensor_tensor(out=k3, in0=p4[:, :, 0, :],
                                                              in1=rs[:, 0:2].unsqueeze(2).to_broadcast([128, 2, 128]), op=ALU.mult),
                 reads=[P.pbb[bank], brs], writes=[bqn])
            S.op("pool", lambda e, k3=k3, s3=s3: e.tensor_tensor(out=s3[:, :, 0:128], in0=k3,
                                                                in1=gmk[:, 0:128].unsqueeze(1).to_broadcast([128, 2, 128]), op=ALU.mult),
                 reads=[bqn, b_gmk], writes=[bstg[si]])
            kp = qn[:, 256:384].rearrange("p (h d) -> p h d", d=64)
            S.op("dve", lambda e, kp=kp, t=t: e.tensor_tensor(out=kp, in0=kpe[:, t, :].unsqueeze(1).to_broadcast([128, 2, 64]),
                                                             in1=rs[:, 0:2].unsqueeze(2).to_broadcast([128, 2, 64]), op=ALU.mult),
                 reads=[bkpe[t], brs, bqn], writes=[bqn])
            S.op("pool", lambda e, kp=kp: e.tensor_tensor(out=kp, in0=kp, in1=gmk[:, 128:192].unsqueeze(1).to_broadcast([128, 2, 64]),
                                                         op=ALU.mult), reads=[bqn, b_gmk], writes=[bqn])
            rope2(kp.rearrange("p h (a d) -> p h a d", a=2), s3[:, :, 128:192].rearrange("p h (a d) -> p h a d", a=2), t, [bqn], bstg[si])
            S.op("act", lambda e, s3=s3, p4=p4: e.copy(out=s3[:, :, 192:320], in_=p4[:, :, 1, :]), reads=[P.pbb[bank]], writes=[bstg[si]])
            S.dma("sp", kvu_out[t * 128:(t + 1) * 128, g6 * 640:(g6 + 1) * 640], stg[si][:, 0:640], reads=[bstg[si]], writes=[bo])
    return P.finish([bo]), P


QG256 = [(0, 256), (256, 256), (512, 256), (768, 256), (1024, 128)]


def build_B1_odd():
    P = Prog()
    S, M = P.S, P.M
    P.consts()
    x_in = P.din("x_in", [T, D])
    q_in = P.din("q_in", [T, 2304], BF16)
    kvu = P.din("kvu_in", [2304, 4352], BF16)
    CN = P.din("dftC", [2048, 1024], BF16)
    SN = P.din("dftS", [2048, 1024], BF16)
    CNc = P.din("dftCc", [256, 128], BF16)
    SNc = P.din("dftSc", [256, 128], BF16)
    CCd = P.din("dftCC", [128, 128], BF16)
    SCd = P.din("dftSC", [128, 128], BF16)
    w_f = P.din("w_fourier", [4, 128, 128])
    w_out = P.din("w_out", [D, D])
    x_out = P.dout("x_out", [T, D])
    modl, P.b_modl = load_small(P, "modl", [128, 2, 96])
    load_x(P, x_in)
    P.actT = M.sb("actT", [128, KC, T], BF16)
    P.bact = [Buf() for _ in range(NT)]
    Kl = M.sb("Kl", [128, 18, 192], BF16)
    Va = M.sb("Va", [128, 18, 129], BF16)
    Ql = M.sb("Ql", [128, NT, 192], BF16)
    KT0 = M.sb("KT0", [128, 2304], BF16)
    KT1 = M.sb("KT1", [64, 2304], BF16)
    QT0 = M.sb("QT0", [128, T], BF16)
    QT1 = M.sb("QT1", [64, T], BF16)
    PT = M.sb("PT", [128, 18, 256], BF16)
    ytok = M.sb("ytok", [128, 128], BF16)
    sm = M.sb("sm", [128, 4], F32)
    bKl, bVa, bQl, bKT, bQT, bPT, byt, bsm = [Buf() for _ in range(8)]
    S.op("pool", lambda e: e.memset(Va[:, :, 128:129], 1.0), writes=[bVa])
    scale = 192 ** -0.5
    for h in range(12):
        S.dma("sp", Kl[:], kvu[:, h * 320:h * 320 + 192].rearrange("(t p) c -> p t c", p=128), writes=[bKl])
        S.dma("act", Va[:, :, 0:128], kvu[:, h * 320 + 192:(h + 1) * 320].rearrange("(t p) c -> p t c", p=128), writes=[bVa])
        S.dma("sp", Ql[:], q_in[:, h * 192:(h + 1) * 192].rearrange("(t p) c -> p t c", p=128), writes=[bQl])
        for (src, bsrc, ntl, d0, d1, bdst) in ((Kl, bKl, 18, KT0, KT1, bKT), (Ql, bQl, NT, QT0, QT1, bQT)):
            for g0 in range(0, ntl, 8):
                n = min(8, ntl - g0)
                for part in range(2):
                    bank = P.rr("tp", 2)
                    pv = P.pb[bank][:, :].bitcast(BF16)
                    np_ = 128 if part == 0 else 64
                    c0 = 0 if part == 0 else 128

                    def tr(e, src=src, g0=g0, n=n, pv=pv, np_=np_, c0=c0):
                        ins = None
                        for j in range(n):
                            ins = e.transpose(pv[0:np_, j * 128:(j + 1) * 128], src[:, g0 + j, c0:c0 + np_], P.identb[:])
                        return ins
                    S.op("pe", tr, reads=[bsrc, P.b_identb], writes=[P.pbb[bank]])
                    dst = d0 if part == 0 else d1
                    if part == 0:
                        S.op("act", lambda e, dst=dst, g0=g0, n=n, pv=pv, np_=np_: e.copy(
                            out=dst[0:np_, g0 * 128:(g0 + n) * 128], in_=pv[0:np_, 0:n * 128]), reads=[P.pbb[bank]], writes=[bdst])
                    else:
                        S.op("dve", lambda e, dst=dst, g0=g0, n=n, pv=pv, np_=np_: e.tensor_copy(
                            out=dst[0:np_, g0 * 128:(g0 + n) * 128], in_=pv[0:np_, 0:n * 128]), reads=[P.pbb[bank]], writes=[bdst])
        for (q0, qn_) in QG256:
            ktiles = list(range(18)) if q0 < 1024 else [16, 17]
            for kt in ktiles:
                bank = 2 + P.rr("sc", 2)
                ps = P.pb[bank][:, 0:qn_]
                mm_group(P, ps, [(KT0[:, kt * 128:(kt + 1) * 128], QT0[:, q0:q0 + qn_]),
                                 (KT1[:, kt * 128:(kt + 1) * 128], QT1[:, q0:q0 + qn_])], reads=[bKT, bQT], writes=[P.pbb[bank]])
                S.op("act", lambda e, kt=kt, ps=ps, qn_=qn_: e.activation(out=PT[:, kt, 0:qn_], in_=ps, func=AF.Exp, scale=scale),
                     reads=[P.pbb[bank]], writes=[bPT])
            for qq in range(qn_ // 128):
                t = q0 // 128 + qq
                bank = 4 + P.rr("av", 2)
                po = P.pb[bank][:, 0:129]
                mm_group(P, po, [(PT[:, kt, qq * 128:(qq + 1) * 128], Va[:, kt, :]) for kt in ktiles], reads=[bPT, bVa],
                         writes=[P.pbb[bank]])
                S.op("dve", lambda e, po=po: e.reciprocal(out=sm[:, 0:1], in_=po[:, 128:129]), reads=[P.pbb[bank]], writes=[bsm])
                S.op("dve", lambda e, po=po: e.tensor_scalar(out=ytok[:], in0=po[:, 0:128], scalar1=sm[:, 0:1], scalar2=None,
                                                            op0=ALU.mult), reads=[P.pbb[bank], bsm], writes=[byt])
                bk2 = P.rr("tp", 2)
                pv = P.pb[bk2][:, :].bitcast(BF16)
                S.op("pe", lambda e, pv=pv: e.transpose(pv[:, 0:128], ytok[:], P.identb[:]), reads=[byt, P.b_identb],
                     writes=[P.pbb[bk2]])
                S.op("act", lambda e, pv=pv, h=h, t=t: e.copy(out=P.actT[:, h, t * 128:(t + 1) * 128], in_=pv[:, 0:128]),
                     reads=[P.pbb[bk2]], writes=[P.bact[t]])
    Ug = M.sb("Ug", [128, 18, 128], BF16)
    Csl = [M.sb("Csl%d" % i, [128, 16, 256], BF16) for i in range(2)]
    Ssl = [M.sb("Ssl%d" % i, [128, 16, 256], BF16) for i in range(2)] if False else None
    Cc = M.sb("Cc", [128, 2, 2, 128], BF16)
    AB = M.sb("AB", [128, 2, T], BF16)
    W12 = M.sb("W12", [128, 2, 4, 128], BF16)
    CS = M.sb("CS", [128, 2, 128], BF16)
    wf = M.sb("wf", [128, 4, 128], BF16)
    bUg, bCc, bAB, bW, bCS, bwf = [Buf() for _ in range(6)]
    bCsl = [Buf(), Buf()]
    S.dma("sp", Cc[:, 0], CNc.rearrange("(t p) c -> p t c", p=128), writes=[bCc])
    S.dma("sp", Cc[:, 1], SNc.rearrange("(t p) c -> p t c", p=128), writes=[bCc])
    S.dma("sp", CS[:, 0, :], CCd, writes=[bCS])
    S.dma("sp", CS[:, 1, :], SCd, writes=[bCS])
    S.dma("pool", wf[:], w_f.rearrange("g c e -> c g e"), writes=[bwf])
    for g in range(4):
        for ab in range(2):
            bank = 6 + P.rr("op", 2)
            ps = P.pb[bank][:, 0:128]
            mm_group(P, ps, [(CS[:, ab, :], wf[:, g, :])], reads=[bCS, bwf], writes=[P.pbb[bank]])
            S.op("act", lambda e, ps=ps, ab=ab, g=g: e.activation(out=W12[:, ab, g, :], in_=ps, func=AF.Identity,
                                                                scale=(1.0 if ab == 0 else -1.0)), reads=[P.pbb[bank]], writes=[bW])
    for g in range(4):
        S.dma("sp", Ug[:], kvu[:, 3840 + g * 128:3840 + (g + 1) * 128].rearrange("(t p) c -> p t c", p=128), writes=[bUg])
        for ab, tab in enumerate((CN, SN)):
            for og in range(4):
                ci = P.rr("csl", 2)
                S.dma("act" if ci else "sp", Csl[ci][:], tab[:, og * 256:(og + 1) * 256].rearrange("(t p) c -> p t c", p=128),
                      writes=[bCsl[ci]])
                bank = 6 + P.rr("op", 2)
                ps = P.pb[bank][:, 0:256]
                mm_group(P, ps, [(Ug[:, kt, :], Csl[ci][:, kt, :]) for kt in range(16)], reads=[bUg, bCsl[ci]], writes=[P.pbb[bank]])
                S.op("act" if og % 2 else "dve", (lambda e, ps=ps, ab=ab, g=g, og=og: e.copy(out=AB[:, ab, og * 256:(og + 1) * 256], in_=ps))
                     if og % 2 else (lambda e, ps=ps, ab=ab, g=g, og=og: e.tensor_copy(out=AB[:, ab, og * 256:(og + 1) * 256], in_=ps)),
                     reads=[P.pbb[bank]], writes=[bAB])
            bank = 6 + P.rr("op", 2)
            ps = P.pb[bank][:, 0:128]
            mm_group(P, ps, [(Ug[:, 16 + kt, :], Cc[:, ab, kt, :]) for kt in range(2)], reads=[bUg, bCc], writes=[P.pbb[bank]])
            S.op("act", lambda e, ps=ps, ab=ab, g=g: e.copy(out=AB[:, ab, 1024:1152], in_=ps), reads=[P.pbb[bank]], writes=[bAB])
        for (n0, nsz) in QGROUPS:
            bank = 6 + P.rr("op", 2)
            ps = P.pb[bank][:, 0:nsz]
            mm_group(P, ps, [(W12[:, 0, g, :], AB[:, 0, n0:n0 + nsz]), (W12[:, 1, g, :], AB[:, 1, n0:n0 + nsz])],
                     reads=[bW, bAB], writes=[P.pbb[bank]])
            tl = [P.bact[n0 // 128 + x] for x in range(nsz // 128)]
            S.op("act", lambda e, ps=ps, g=g, n0=n0, nsz=nsz: e.copy(out=P.actT[:, 12 + g, n0:n0 + nsz], in_=ps),
                 reads=[P.pbb[bank]], writes=tl)
    g1 = M.sb("g1v", [128, 2, KC], F32)
    bg1 = Buf()
    S.op("dve", lambda e: e.tensor_copy(out=g1[:], in_=modl[:, :, 32:48]), reads=[P.b_modl], writes=[bg1])
    gate_bc, b_gate = bcast_rows(P, g1, bg1, "g1")
    out_proj(P, w_out, gate_bc, b_gate)
    bo = store_x(P, x_out)
    return P.finish([bo]), P


def _fm(v):
    return np.ascontiguousarray(np.asarray(v, np.float32).reshape(-1, 128).T)


def _bc(v):
    return np.ascontiguousarray(np.tile(np.asarray(v, np.float32).reshape(1, -1), (128, 1)))


def _rope_tab(rot_dim):
    rows = 2048 // 64
    row = np.repeat(np.arange(rows, dtype=np.float32), 64)
    col = np.tile(np.arange(64, dtype=np.float32), rows)
    axis_dim = rot_dim // 2
    inv = (10000.0 ** (-np.arange(0, axis_dim, 2, dtype=np.float32) / axis_dim)).astype(np.float32)
    ang = np.concatenate([row[:, None] * inv, col[:, None] * inv], -1)
    return np.cos(ang).astype(np.float32), np.sin(ang).astype(np.float32)


def _rope_core(rot_dim, half):
    c, s = _rope_tab(rot_dim)
    rope = np.zeros((T, 2, rot_dim // 2), np.float32)
    rope[:, 0] = 1.0
    rope[:TL, 0] = c[half * TL:(half + 1) * TL]
    rope[:TL, 1] = s[half * TL:(half + 1) * TL]
    return np.ascontiguousarray(rope.reshape(NT, 128, 2, rot_dim // 2).transpose(1, 0, 2, 3))


def _pool_mats(half):
    out = np.zeros((4, NT, 3, 128, 128), np.float32)
    for wi, w in enumerate((2, 4, 8, 16)):
        for j in range(NT):
            n = 2048 if j < 8 else 256
            gt = (half * 8 + j) if j < 8 else half
            t = gt * 128 + np.arange(128)
            lo = np.clip(t - w // 2, 0, n)
            hi = np.clip(t + w - w // 2, 0, n)
            cnt = (hi - lo).astype(np.float32)
            for kk in range(3):
                gi = gt + kk - 1
                if gi < 0 or gi >= n // 128:
                    continue
                tp = gi * 128 + np.arange(128)
                m = ((tp[:, None] >= lo[None, :]) & (tp[:, None] < hi[None, :])).astype(np.float32) / cnt[None, :]
                m -= (tp[:, None] == t[None, :]).astype(np.float32)
                out[wi, j, kk] = m
    return out.astype(NPBF)


def _dft_tabs(half):
    i = np.arange(2048)
    n_in = np.where(i < 1024, half * 1024 + i, (1 - half) * 1024 + (i - 1024)).astype(np.int64)
    n_out = (half * 1024 + np.arange(1024)).astype(np.int64)
    ang = 2 * np.pi * ((n_in[:, None] * n_out[None, :]) % 2048).astype(np.float64) / 2048
    nrm = 1.0 / np.sqrt(2048 * 128)
    d = {"dftC": (np.cos(ang) * nrm).astype(NPBF), "dftS": (np.sin(ang) * nrm).astype(NPBF)}
    i = np.arange(256)
    n_in = np.where(i < 128, half * 128 + i, (1 - half) * 128 + (i - 128)).astype(np.int64)
    n_out = (half * 128 + np.arange(128)).astype(np.int64)
    ang = 2 * np.pi * ((n_in[:, None] * n_out[None, :]) % 256).astype(np.float64) / 256
    nrm = 1.0 / np.sqrt(256 * 128)
    d["dftCc"] = (np.cos(ang) * nrm).astype(NPBF)
    d["dftSc"] = (np.sin(ang) * nrm).astype(NPBF)
    c = np.arange(128)
    ang = 2 * np.pi * ((c[:, None] * c[None, :]) % 128).astype(np.float64) / 128
    d["dftCC"] = np.cos(ang).astype(NPBF)
    d["dftSC"] = np.sin(ang).astype(NPBF)
    return d


def _modl_core(mod, l, b):
    m = np.zeros((128, 2, 96), np.float32)
    for j in range(6):
        m[:, 0, j * 16:(j + 1) * 16] = _fm(mod[l, b, j * 2048:(j + 1) * 2048])
        m[:, 1, j * 16:(j + 1) * 16] = _fm(mod[l, 4, j * 2048:(j + 1) * 2048])
    return m


def _kv_pair(own, partner):
    return np.ascontiguousarray(np.concatenate([own[:TL], partner[:TL], own[TL:], partner[TL:]], 0))


def _moe_host(inp, l):
    wg = inp["w_gu"][l]
    g = wg[:, :, :DEXP].reshape(NEXP, D, 6, 1, 128)
    u = wg[:, :, DEXP:].reshape(NEXP, D, 6, 1, 128)
    w_gu_r = np.ascontiguousarray(np.concatenate([g, u], 3).transpose(0, 2, 1, 3, 4).reshape(NEXP, 6, D, 256))
    b_guT = np.ascontiguousarray(inp["b_gu"][l].reshape(NEXP, 12, 128).transpose(2, 0, 1))
    return {"w_router": np.ascontiguousarray(inp["w_router"][l].reshape(KC, 128, NEXP).transpose(1, 0, 2)),
            "b_router": _bc(inp["b_router"][l]), "w_gu_r": w_gu_r, "b_guT": b_guT,
            "w_down": np.ascontiguousarray(inp["w_down"][l]), "b_down": np.ascontiguousarray(inp["b_down"][l])}


_PROGS = {}


def _prog(key, fn):
    if key not in _PROGS:
        _PROGS[key] = fn()[0]
    return _PROGS[key]


def _run(nc, in_maps):
    res = run_bass_kernel_spmd(nc, in_maps, core_ids=list(range(NCORE)))
    return res.results


def kernel(**inp):
    inp = {k: np.asarray(v) for k, v in inp.items()}
    x, c, ctx, c_ctx = inp["x"], inp["c"], inp["ctx"], inp["c_ctx"]
    cores = [(cid // 2, cid % 2) for cid in range(NCORE)]
    C5 = np.concatenate([c, c_ctx[None]], 0).astype(np.float32)
    cT5 = np.ascontiguousarray(C5.reshape(5, KC, 128).transpose(2, 1, 0))
    ims = []
    for cid in range(NCORE):
        sl = slice(cid * 1536, (cid + 1) * 1536)
        ims.append({"cT5": cT5, "wmod_sh": np.ascontiguousarray(inp["w_mod"][:, :, sl]),
                    "bmodT": np.ascontiguousarray(inp["b_mod"][:, sl].reshape(DEPTH, 12, 128).transpose(2, 0, 1))})
    rM = _run(_prog("M", build_M), ims)
    mod = np.zeros((DEPTH, 5, 6 * D), np.float32)
    for cid in range(NCORE):
        o = rM[cid]["mod_sh"].reshape(128, DEPTH, 12, 5)
        mod[:, :, cid * 1536:(cid + 1) * 1536] = o.transpose(1, 3, 2, 0).reshape(DEPTH, 5, 1536)
    X = [np.ascontiguousarray(np.concatenate([x[b, h * TL:(h + 1) * TL], ctx[b, h * TCX:(h + 1) * TCX]], 0)) for (b, h) in cores]
    ropes = [_rope_core(64, h) for h in range(2)]
    perm = np.concatenate([np.arange(0, 1024), np.arange(1088, 1600), np.arange(1024, 1088)])
    for l in range(DEPTH):
        i = l // 2
        modls = [_modl_core(mod, l, b) for b in range(4)]
        gmix = _fm(inp["g_mix"][l])
        w_out = np.ascontiguousarray(inp["w_out"][l])
        if l % 2 == 0:
            lam_init = 0.8 - 0.6 * math.exp(-0.3 * l)
            w_in = np.ascontiguousarray(inp["w_in_ab"][i])
            gqk = np.ascontiguousarray(np.stack([_bc(inp["g_aq"][i]), _bc(inp["g_ak"][i])], 1))
            ims = [{"x_in": X[cid], "w_in": w_in, "modl": modls[b], "gmix": gmix, "gqk": gqk, "rope": ropes[h]}
                   for cid, (b, h) in enumerate(cores)]
            rA = _run(_prog("Ae", build_A_even), ims)
            pm = [_pool_mats(h) for h in range(2)]
            w_pool = np.ascontiguousarray(inp["w_pool"][i])
            sp = _fm(inp["s_pool"][i])
            lamr = _bc(inp["lam"][i].reshape(-1))
            gs = _bc(inp["g_subln"][i])
            ims = [{"x_in": X[cid], "q_in": rA[cid]["q_out"], "kvu_in": _kv_pair(rA[cid]["kvu_out"], rA[cid ^ 1]["kvu_out"]),
                    "poolA": pm[h], "w_pool": w_pool, "w_out": w_out, "modl": modls[b], "s_poolT": sp, "lam": lamr, "g_subln": gs}
                   for cid, (b, h) in enumerate(cores)]
            rB = _run(_prog("Be%d" % l, lambda: build_B1_even(lam_init)), ims)
        else:
            w_in = np.ascontiguousarray(inp["w_in_cd"][i][:, perm])
            w_qb = np.ascontiguousarray(inp["w_qb"][i])
            w_kvb = np.ascontiguousarray(inp["w_kvb"][i])
            gq, gk, gmq, gmk = _bc(inp["g_qa"][i]), _bc(inp["g_kva"][i]), _bc(inp["g_mq"][i]), _bc(inp["g_mk"][i])
            ims = [{"x_in": X[cid], "w_in": w_in, "w_qb": w_qb, "w_kvb": w_kvb, "modl": modls[b], "gmix": gmix, "g_qa": gq,
                    "g_kva": gk, "g_mq": gmq, "g_mk": gmk, "rope": ropes[h]} for cid, (b, h) in enumerate(cores)]
            rA = _run(_prog("Ao", build_A_odd), ims)
            dft = [_dft_tabs(h) for h in range(2)]
            w_f = np.ascontiguousarray(inp["w_fourier"][i])
            ims = []
            for cid, (b, h) in enumerate(cores):
                d = {"x_in": X[cid], "q_in": rA[cid]["q_out"], "kvu_in": _kv_pair(rA[cid]["kvu_out"], rA[cid ^ 1]["kvu_out"]),
                     "w_fourier": w_f, "w_out": w_out, "modl": modls[b]}
                d.update(dft[h])
                ims.append(d)
            rB = _run(_prog("Bo", build_B1_odd), ims)
        X = [np.ascontiguousarray(rB[cid]["x_out"]) for cid in range(NCORE)]
        mh = _moe_host(inp, l)
        gffn = _fm(inp["g_ffn"][l])
        ims = []
        for cid, (b, h) in enumerate(cores):
            d = {"x_in": X[cid], "modl": modls[b], "gffn": gffn}
            d.update(mh)
            ims.append(d)
        rC = _run(_prog("C", build_moe_test), ims)
        X = [np.ascontiguousarray(rC[cid]["x_out"]) for cid in range(NCORE)]
    out = np.zeros((4, 2048, D), np.float32)
    for cid, (b, h) in enumerate(cores):
        out[b, h * TL:(h + 1) * TL] = X[cid][:TL]
    return out
```

```python
import math
import numpy as np
import ml_dtypes
import concourse.bass as bass
import concourse.mybir as mybir
from concourse.bass_utils import run_bass_kernel_spmd

F32 = mybir.dt.float32
BF16 = mybir.dt.bfloat16
ALU = mybir.AluOpType
AF = mybir.ActivationFunctionType
AX = mybir.AxisListType
NPBF = ml_dtypes.bfloat16

D = 2048
KC = 16
DEPTH = 4
NCORE = 8
TL = 1024
TCX = 128
T = TL + TCX
NT = T // 128
EPS = 1e-6
NEXP = 32
DEXP = 768
NTILES_N = [(0, 512), (512, 512), (1024, 128)]


class Buf:
    __slots__ = ("w", "r")

    def __init__(self):
        self.w = None
        self.r = {}


class Sched:
    ENGS = ("pe", "act", "dve", "pool", "sp")

    def __init__(self, nc, n_dma_sems=8):
        self.nc = nc
        self.ops = {e: [] for e in self.ENGS}
        self.sems = {}
        self.cnt = {e: 0 for e in self.ENGS}
        self.seen = {e: {} for e in self.ENGS}
        self._ctx = []
        for e in self.ENGS:
            self.sems[e] = self._sem("s_" + e)
        self.dq = {}
        for q in ("sp", "act", "pool"):
            lst = []
            for i in range(n_dma_sems):
                k = "d_%s_%d" % (q, i)
                self.sems[k] = self._sem(k)
                lst.append(k)
            self.dq[q] = {"keys": lst, "n": 0}

    def _sem(self, name):
        cm = self.nc.semaphore(name)
        h = cm.__enter__()
        self._ctx.append(cm)
        return h

    def _need(self, eng, reads, writes):
        need = {}

        def add(ev):
            if ev is None:
                return
            k, v = ev
            if need.get(k, 0) < v:
                need[k] = v
        for b in reads:
            add(b.w)
        for b in writes:
            add(b.w)
            for k, v in b.r.items():
                add((k, v))
        out = []
        for k, v in need.items():
            if k == "pe" and eng == "pe":
                continue
            if self.seen[eng].get(k, 0) < v:
                self.seen[eng][k] = v
                out.append((k, v))
        return out

    def _emit_waits(self, eng, waits):
        for k, v in waits:
            h = self.sems[k]
            self.ops[eng].append(lambda e, h=h, v=v: e.wait_ge(h, v))

    def _mark(self, ev, reads, writes):
        k, v = ev
        for b in reads:
            if b.r.get(k, 0) < v:
                b.r[k] = v
        for b in writes:
            b.w = ev
            b.r = {}

    def op(self, eng, fn, reads=(), writes=()):
        waits = self._need(eng, reads, writes)
        self._emit_waits(eng, waits)
        self.cnt[eng] += 1
        v = self.cnt[eng]
        h = self.sems[eng]
        self.ops[eng].append(lambda e, fn=fn, h=h: fn(e).then_inc(h, 1))
        self._mark((eng, v), reads, writes)

    def _slot(self, q, waits):
        d = self.dq[q]
        i = d["n"]
        d["n"] += 1
        n = len(d["keys"])
        k = d["keys"][i % n]
        val = 16 * (i // n + 1)
        if i >= n:
            prev = 16 * (i // n)
            if self.seen[q].get(k, 0) < prev:
                self.seen[q][k] = prev
                waits.append((k, prev))
        return k, val

    def dma(self, q, out, in_, reads=(), writes=(), **kw):
        waits = self._need(q, reads, writes)
        k, val = self._slot(q, waits)
        self._emit_waits(q, waits)
        h = self.sems[k]
        self.ops[q].append(lambda e, out=out, in_=in_, h=h, kw=kw: e.dma_start(out=out, in_=in_, **kw).then_inc(h, 16))
        self._mark((k, val), reads, writes)

    def custom(self, q, fn, reads=(), writes=(), inc=16):
        waits = self._need(q, reads, writes)
        k, val = self._slot(q, waits)
        self._emit_waits(q, waits)
        h = self.sems[k]

        def run(e, fn=fn, h=h, inc=inc):
            fn(e).then_inc(h, inc)
            if inc < 16:
                e.sem_inc(h, 16 - inc)
        self.ops[q].append(run)
        self._mark((k, val), reads, writes)

    def barrier(self):
        evs = [(e, self.cnt[e]) for e in self.ENGS if self.cnt[e] > 0]
        for q, d in self.dq.items():
            n = len(d["keys"])
            for j, k in enumerate(d["keys"]):
                cntk = (d["n"] - j + n - 1) // n if d["n"] > j else 0
                if cntk > 0:
                    evs.append((k, 16 * cntk))
        for eng in self.ENGS:
            waits = []
            for k, v in evs:
                if k == "pe" and eng == "pe":
                    continue
                if self.seen[eng].get(k, 0) < v:
                    self.seen[eng][k] = v
                    waits.append((k, v))
            self._emit_waits(eng, waits)

    def wait_all(self, eng, bufs):
        self._emit_waits(eng, self._need(eng, bufs, bufs))

    def emit(self):
        ops = self.ops
        with self.nc.Block() as block:
            @block.tensor
            def _(e):
                for f in ops["pe"]:
                    f(e)

            @block.scalar
            def _(e):
                for f in ops["act"]:
                    f(e)

            @block.vector
            def _(e):
                for f in ops["dve"]:
                    f(e)

            @block.gpsimd
            def _(e):
                for f in ops["pool"]:
                    f(e)

            @block.sync
            def _(e):
                for f in ops["sp"]:
                    f(e)

    def close(self):
        for cm in reversed(self._ctx):
            cm.__exit__(None, None, None)
        self._ctx = []


class Mem:
    def __init__(self, nc):
        self.nc = nc
        self._ctx = []

    def sb(self, name, shape, dtype):
        cm = self.nc.sbuf_tensor(name, list(shape), dtype)
        t = cm.__enter__()
        self._ctx.append(cm)
        return t

    def ps(self, name, shape, dtype):
        cm = self.nc.psum_tensor(name, list(shape), dtype)
        t = cm.__enter__()
        self._ctx.append(cm)
        return t

    def close(self):
        for cm in reversed(self._ctx):
            cm.__exit__(None, None, None)
        self._ctx = []


class ArenaMem:
    def __init__(self, arena, nbytes):
        self.arena = arena
        self.nbytes = nbytes
        self.off = 0

    def reset(self):
        self.off = 0

    def sb(self, name, shape, dtype):
        esz = 4 if dtype == F32 or str(dtype).endswith("int32") else 2
        n = 1
        for d in shape[1:]:
            n *= d
        nb = (n * esz + 63) // 64 * 64
        assert self.off + nb <= self.nbytes, ("arena overflow", name, self.off, nb, self.nbytes)
        v = self.arena[0:shape[0], self.off // 2:(self.off + n * esz) // 2]
        self.off += nb
        if esz == 4:
            v = v.bitcast(dtype)
        if len(shape) == 3:
            v = v.rearrange("p (a b) -> p a b", a=shape[1])
        elif len(shape) == 4:
            v = v.rearrange("p (a b c) -> p a b c", a=shape[1], b=shape[2])
        return v


class Prog:
    def __init__(self, fused=False, arena_bytes=150 * 1024):
        self.nc = bass.Bass("TRN2", target_bir_lowering=False)
        self.S = Sched(self.nc)
        self.MP = Mem(self.nc)
        self.fused = fused
        self.tag = ""
        self.bind = {}
        self.sb_bind = {}
        self.phase_id = 0
        self.M = self.MP
        self.pb = [self.MP.ps("pb%d" % i, [128, 512], F32) for i in range(8)]
        self.pbb = [Buf() for _ in range(8)]
        self.ins = {}
        self.outs = {}
        self._rr = {}

    def din(self, name, shape, dtype=F32):
        if name in self.bind:
            return self.bind[name]
        full = self.tag + name
        if full in self.ins:
            return self.ins[full]
        t = self.nc.dram_tensor(full, list(shape), dtype, kind="ExternalInput").ap()
        self.ins[full] = t
        return t

    def dout(self, name, shape, dtype=F32):
        if name in self.bind:
            return self.bind[name]
        if self.fused:
            t = self.nc.dram_tensor(self.tag + name, list(shape), dtype).ap()
            self.bind[name] = t
            return t
        t = self.nc.dram_tensor(name, list(shape), dtype, kind="ExternalOutput").ap()
        self.outs[name] = t
        return t

    def make_arena(self):
        nbytes = (int(self.nc.sbuf_bytes_remaining) - 512) // 64 * 64
        self.arena = self.MP.sb("arena", [128, nbytes // 2], BF16)
        self.M = ArenaMem(self.arena, nbytes)

    def new_phase(self, tag=None):
        self.S.barrier()
        self.phase_id += 1
        if self.fused:
            self.M.reset()
        if tag is not None:
            self.tag = tag

    def rr(self, key, n):
        v = self._rr.get(key, 0)
        self._rr[key] = v + 1
        return v % n

    def consts(self):
        S, M = self.S, self.MP
        self.ones128 = M.sb("ones128", [128, 128], F32)
        self.b_ones = Buf()
        S.op("pool", lambda e: e.memset(self.ones128[:], 1.0), writes=[self.b_ones])
        self.identb = M.sb("identb", [128, 128], BF16)
        self.b_identb = Buf()
        self.identf = M.sb("identf", [128, 128], F32)
        self.b_identf = Buf()
        self.epsc = M.sb("epsc", [128, 1], F32)
        self.b_eps = Buf()
        S.op("pool", lambda e: e.memset(self.identf[:], 1.0), writes=[self.b_identf])
        S.op("pool", lambda e: e.affine_select(out=self.identf[:], in_=self.identf[:], pattern=[[-1, 128]],
                                                compare_op=ALU.is_equal, fill=0.0, base=0, channel_multiplier=1),
             reads=[self.b_identf], writes=[self.b_identf])
        S.op("pool", lambda e: e.tensor_copy(out=self.identb[:], in_=self.identf[:]), reads=[self.b_identf],
             writes=[self.b_identb])
        S.op("pool", lambda e: e.memset(self.epsc[:], EPS), writes=[self.b_eps])

    def finish(self, out_bufs):
        self.S.wait_all("sp", out_bufs)
        self.S.emit()
        self.MP.close()
        self.S.close()
        return self.nc


def mm_group(P, out_ap, pairs, reads, writes):
    n = len(pairs)

    def fn(e):
        ins = None
        for i, (l, r) in enumerate(pairs):
            ins = e.matmul(out_ap, lhsT=l, rhs=r, start=(i == 0), stop=(i == n - 1))
        return ins
    P.S.op("pe", fn, reads=reads, writes=writes)


def build_M():
    P = Prog()
    S, M = P.S, P.M
    cT5 = P.din("cT5", [128, KC, 5])
    wm = P.din("wmod_sh", [DEPTH, D, 1536])
    bmT = P.din("bmodT", [128, DEPTH, 12])
    out = P.dout("mod_sh", [128, DEPTH * 12 * 5])
    ct = M.sb("ct", [128, KC, 5], F32)
    st = M.sb("st", [128, KC, 5], F32)
    bt = M.sb("bt", [128, DEPTH, 12], F32)
    res = M.sb("res", [128, DEPTH * 12 * 5], F32)
    wbuf = [M.sb("wm%d" % i, [128, KC, 768], F32) for i in range(2)]
    b_ct, b_st, b_bt, b_res = Buf(), Buf(), Buf(), Buf()
    b_w = [Buf(), Buf()]
    S.dma("sp", ct[:], cT5, writes=[b_ct])
    S.dma("sp", bt[:], bmT, writes=[b_bt])
    S.op("act", lambda e: e.activation(out=st[:], in_=ct[:], func=AF.Silu), reads=[b_ct], writes=[b_st])
    it = 0
    for l in range(DEPTH):
        for hh in range(2):
            wb, bw = wbuf[it % 2], b_w[it % 2]
            it += 1
            S.dma("sp" if hh == 0 else "act", wb[:],
                  wm[l, :, hh * 768:(hh + 1) * 768].rearrange("(k p) c -> p k c", p=128), writes=[bw])
            for q6 in range(6):
                q = hh * 6 + q6
                bank = P.rr("m", 4)
                ps = P.pb[bank][:, 0:5]
                mm_group(P, ps, [(wb[:, k, q6 * 128:(q6 + 1) * 128], st[:, k, :]) for k in range(KC)],
                         reads=[bw, b_st], writes=[P.pbb[bank]])
                o = (l * 12 + q) * 5
                S.op("dve", lambda e, ps=ps, o=o, l=l, q=q: e.tensor_scalar(
                    out=res[:, o:o + 5], in0=ps, scalar1=bt[:, l, q:q + 1], scalar2=None, op0=ALU.add),
                    reads=[P.pbb[bank], b_bt], writes=[b_res])
    bo = Buf()
    S.dma("sp", out, res[:], reads=[b_res], writes=[bo])
    return P.finish([bo]), P


def stream_x(P, x_in):
    if P.fused:
        return
    P.Xs = P.M.sb("Xs", [128, 2, D], F32)
    P.bXs = [Buf(), Buf()]
    P.x_src = x_in


def xtile(P, t):
    if hasattr(P, "x_src"):
        i = t % 2
        P.S.dma("sp" if i == 0 else "act", P.Xs[:, i, :], P.x_src[t * 128:(t + 1) * 128, :], writes=[P.bXs[i]])
        return P.Xs[:, i, :], P.bXs[i]
    return P.X[:, t, :], P.bX[t]


def load_x(P, x_in):
    if hasattr(P, "X"):
        return
    P.X = P.MP.sb("X", [128, NT, D], F32)
    P.bX = [Buf() for _ in range(NT)]
    for t in range(NT):
        P.S.dma("sp" if t % 2 == 0 else "act", P.X[:, t, :], x_in[t * 128:(t + 1) * 128, :], writes=[P.bX[t]])


def prenorm(P, A, B, bAB, name):
    S, M = P.S, P.M
    if getattr(P, "_pn_phase", None) != P.phase_id:
        P._pn_phase = P.phase_id
        P.actT = M.sb("actT", [128, KC, T], BF16)
        P.bact = [Buf() for _ in range(NT)]
        P.ss = M.sb("ss", [128, NT], F32)
        P.bss = [Buf() for _ in range(NT)]
        P.xn = [M.sb("xn%d" % i, [128, D], BF16) for i in range(1)]
        P.bxn = [Buf()]
    for t in range(NT):
        typ = 0 if t < 8 else 1
        ss = P.ss[:, t:t + 1]
        xt_, bxt_ = xtile(P, t)
        S.op("act", lambda e, xt_=xt_, ss=ss: e.activation(out=P.xn[0][:], in_=xt_, func=AF.Square, accum_out=ss),
             reads=[bxt_], writes=[P.bxn[0], P.bss[t]])
        S.op("act", lambda e, ss=ss: e.activation(out=ss, in_=ss, func=AF.Sqrt, bias=P.epsc[:, 0:1], scale=1.0 / D),
             reads=[P.bss[t], P.b_eps], writes=[P.bss[t]])
        S.op("dve", lambda e, ss=ss: e.reciprocal(out=ss, in_=ss), reads=[P.bss[t]], writes=[P.bss[t]])
        xi = 0
        xn, bxn = P.xn[xi], P.bxn[xi]
        S.op("dve", lambda e, xt_=xt_, xn=xn, ss=ss: e.tensor_scalar(out=xn[:], in0=xt_, scalar1=ss, scalar2=None,
                                                                op0=ALU.mult),
             reads=[bxt_, P.bss[t]], writes=[bxn])
        for g in range(2):
            bank = P.rr("tp", 2)
            pv = P.pb[bank][:, :].bitcast(BF16)

            def tr(e, g=g, pv=pv, xn=xn):
                ins = None
                for j in range(8):
                    k = g * 8 + j
                    ins = e.transpose(pv[:, j * 128:(j + 1) * 128], xn[:, k * 128:(k + 1) * 128], P.identb[:])
                return ins
            S.op("pe", tr, reads=[bxn, P.b_identb], writes=[P.pbb[bank]])
            for j in range(8):
                k = g * 8 + j
                eng = "act" if j % 2 == 0 else "dve"
                dst = P.actT[:, k, t * 128:(t + 1) * 128]
                src = pv[:, j * 128:(j + 1) * 128]
                if eng == "act":
                    S.op("act", lambda e, dst=dst, src=src, k=k, typ=typ: e.activation(
                        out=dst, in_=src, func=AF.Identity, bias=B[:, typ, k:k + 1], scale=A[:, typ, k:k + 1]),
                        reads=[P.pbb[bank], bAB], writes=[P.bact[t]])
                else:
                    S.op("dve", lambda e, dst=dst, src=src, k=k, typ=typ: e.tensor_scalar(
                        out=dst, in0=src, scalar1=A[:, typ, k:k + 1], scalar2=B[:, typ, k:k + 1],
                        op0=ALU.mult, op1=ALU.add),
                        reads=[P.pbb[bank], bAB], writes=[P.bact[t]])


def mod_vectors(P, modl, g_in, part_scale, part_shift, name):
    S, M = P.S, P.M
    A = M.sb("A_" + name, [128, 2, KC], F32)
    B = M.sb("B_" + name, [128, 2, KC], F32)
    bAB = Buf()
    for typ in range(2):
        S.op("dve", lambda e, typ=typ: e.tensor_scalar(
            out=A[:, typ, :], in0=modl[:, typ, part_scale * 16:(part_scale + 1) * 16], scalar1=1.0, scalar2=None,
            op0=ALU.add), reads=[P.b_modl], writes=[bAB])
        S.op("dve", lambda e, typ=typ: e.tensor_tensor(out=A[:, typ, :], in0=A[:, typ, :], in1=g_in, op=ALU.mult),
             reads=[bAB, P.b_g], writes=[bAB])
        S.op("dve", lambda e, typ=typ: e.tensor_copy(
            out=B[:, typ, :], in_=modl[:, typ, part_shift * 16:(part_shift + 1) * 16]), reads=[P.b_modl], writes=[bAB])
    return A, B, bAB


def bcast_rows(P, vec, bvec, name):
    S, M = P.S, P.M
    if getattr(P, "_bc_phase", None) != P.phase_id:
        P._bc_phase = P.phase_id
        P.diag = [M.sb("diag%d" % i, [128, 512], F32) for i in range(2)]
        P.bdiag = [Buf(), Buf()]
    out = M.sb("bc_" + name, [128, 2, D], BF16)
    bout = Buf()
    for typ in range(2):
        for c4 in range(4):
            di = P.rr("diag", 2)
            dg, bdg = P.diag[di], P.bdiag[di]
            for j in range(4):
                k = c4 * 4 + j
                S.op("dve", lambda e, dg=dg, j=j, k=k, typ=typ: e.tensor_scalar(
                    out=dg[:, j * 128:(j + 1) * 128], in0=P.identf[:], scalar1=vec[:, typ, k:k + 1], scalar2=None,
                    op0=ALU.mult), reads=[P.b_identf, bvec], writes=[bdg])
            bank = P.rr("bc", 2) + 2
            mm_group(P, P.pb[bank][:, :], [(P.ones128[:], dg[:])], reads=[bdg, P.b_ones], writes=[P.pbb[bank]])
            S.op("act", lambda e, bank=bank, typ=typ, c4=c4: e.copy(out=out[:, typ, c4 * 512:(c4 + 1) * 512],
                                                                   in_=P.pb[bank][:, :]),
                 reads=[P.pbb[bank]], writes=[bout])
    return out, bout


def load_small(P, name, shape, dtype=F32, q="sp"):
    if name in P.sb_bind:
        return P.sb_bind[name]
    ap = P.din(name, shape, dtype)
    t = P.M.sb("s_" + name, shape, dtype)
    b = Buf()
    P.S.dma(q, t[:], ap, writes=[b])
    return t, b


def stream_slabs(P, w_ap, ncols, cgs, name):
    raise NotImplementedError


def emit_A_even(P):
    S, M = P.S, P.M
    x_in = P.din("x_in", [T, D])
    w_in = P.din("w_in", [D, 4096])
    q_out = P.dout("q_out", [T, 1024], BF16)
    kvu_out = P.dout("kvu_out", [T, 3072], BF16)
    modl, P.b_modl = load_small(P, "modl", [128, 2, 96])
    gmix, P.b_g = load_small(P, "gmix", [128, KC])
    gqk, b_gqk = load_small(P, "gqk", [128, 2, 64])
    rope, b_rope = load_small(P, "rope", [128, NT, 2, 32])
    stream_x(P, x_in)
    A, B, bAB = mod_vectors(P, modl, gmix[:], 1, 0, "mix")
    prenorm(P, A, B, bAB, "mix")
    if getattr(P, "debugA", False):
        nc = P.nc
        dbo = Buf()
        d1 = nc.dram_tensor("dbgA_actT", [128, KC * T], BF16, kind="ExternalOutput").ap()
        S.dma("sp", d1, P.actT.rearrange("p k t -> p (k t)"), reads=P.bact, writes=[dbo])
        d2 = nc.dram_tensor("dbgA_AB", [128, 64], F32, kind="ExternalOutput").ap()
        S.dma("sp", d2[:, 0:32], A.rearrange("p a b -> p (a b)"), reads=[bAB], writes=[dbo])
        S.dma("sp", d2[:, 32:64], B.rearrange("p a b -> p (a b)"), reads=[bAB], writes=[dbo])
        d3 = nc.dram_tensor("dbgA_ss", [128, NT], F32, kind="ExternalOutput").ap()
        S.dma("sp", d3, P.ss, reads=P.bss, writes=[dbo])
        d4 = nc.dram_tensor("dbgA_x", [128, NT * D], F32, kind="ExternalOutput").ap()
        S.dma("sp", d4, P.X[:].rearrange("p t d -> p (t d)"), reads=P.bX, writes=[dbo])
        S.barrier()
    slabs = [M.sb("slab%d" % i, [128, KC, 512], BF16) for i in range(2)]
    bslab = [Buf(), Buf()]
    stg = [M.sb("stg%d" % i, [128, 512], BF16) for i in range(3)]
    bstg = [Buf() for _ in range(3)]
    sqb = [M.sb("sqb%d" % i, [128, 512], F32) for i in range(2)]
    bsq = [Buf(), Buf()]
    qn = [M.sb("qn%d" % i, [128, 512], F32) for i in range(2)]
    bqn = [Buf(), Buf()]
    ra = [M.sb("ra%d" % i, [128, 8, 32], F32) for i in range(4)]
    bra = [Buf() for _ in range(4)]
    rs = M.sb("rs", [128, 2, 8], F32)
    brs = [Buf(), Buf()]
    bo = Buf()
    for cg in range(8):
        sl, bsl = slabs[cg % 2], bslab[cg % 2]
        S.dma("pool", sl[:], w_in[:, cg * 512:(cg + 1) * 512].rearrange("(k p) c -> p k c", p=128), writes=[bsl])
        for t in range(NT):
            bank = 4 + P.rr("ip", 4)
            ps = P.pb[bank]
            mm_group(P, ps[:, :], [(P.actT[:, k, t * 128:(t + 1) * 128], sl[:, k, :]) for k in range(KC)],
                     reads=[P.bact[t], bsl], writes=[P.pbb[bank]])
            si = P.rr("stg", 3)
            sg, bsg = stg[si], bstg[si]
            rows = slice(t * 128, (t + 1) * 128)
            if cg < 4:
                which = 0 if cg < 2 else 1
                i2 = P.rr("sq", 2)
                sq, bq, q_, bq_, rsv, brsv = sqb[i2], bsq[i2], qn[i2], bqn[i2], rs[:, i2, :], brs[i2]
                S.op("act", lambda e, sq=sq, ps=ps: e.activation(out=sq[:], in_=ps[:, :], func=AF.Square),
                     reads=[P.pbb[bank]], writes=[bq])
                S.op("dve", lambda e, sq=sq, rsv=rsv: e.tensor_reduce(
                    out=rsv, in_=sq[:].rearrange("p (g d) -> p g d", d=64), axis=AX.X, op=ALU.add),
                    reads=[bq], writes=[brsv])
                S.op("act", lambda e, rsv=rsv: e.activation(out=rsv, in_=rsv, func=AF.Sqrt, bias=P.epsc[:, 0:1],
                                                         scale=1.0 / 64), reads=[brsv, P.b_eps], writes=[brsv])
                S.op("dve", lambda e, rsv=rsv: e.reciprocal(out=rsv, in_=rsv), reads=[brsv], writes=[brsv])
                S.op("dve", lambda e, q_=q_, ps=ps, rsv=rsv: e.tensor_tensor(
                    out=q_[:].rearrange("p (g d) -> p g d", d=64), in0=ps[:, :].rearrange("p (g d) -> p g d", d=64),
                    in1=rsv.unsqueeze(2).to_broadcast([128, 8, 64]), op=ALU.mult),
                    reads=[P.pbb[bank], brsv], writes=[bq_])
                S.op("pool", lambda e, q_=q_, which=which: e.tensor_tensor(
                    out=q_[:].rearrange("p (g d) -> p g d", d=64), in0=q_[:].rearrange("p (g d) -> p g d", d=64),
                    in1=gqk[:, which, :].unsqueeze(1).to_broadcast([128, 8, 64]), op=ALU.mult),
                    reads=[bq_, b_gqk], writes=[bq_])
                q4 = q_[:].rearrange("p (g h d) -> p g h d", h=2, d=32)
                x1, x2 = q4[:, :, 0, :], q4[:, :, 1, :]
                cosb = rope[:, t, 0, :].unsqueeze(1).to_broadcast([128, 8, 32])
                sinb = rope[:, t, 1, :].unsqueeze(1).to_broadcast([128, 8, 32])
                r4 = [P.rr("ra", 4) for _ in range(4)]
                tmp = [(ra[i], bra[i]) for i in r4]
                S.op("dve", lambda e, o=tmp[0][0], x1=x1, cosb=cosb: e.tensor_tensor(out=o[:], in0=x1, in1=cosb, op=ALU.mult),
                     reads=[bq_, b_rope], writes=[tmp[0][1]])
                S.op("pool", lambda e, o=tmp[1][0], x2=x2, sinb=sinb: e.tensor_tensor(out=o[:], in0=x2, in1=sinb, op=ALU.mult),
                     reads=[bq_, b_rope], writes=[tmp[1][1]])
                S.op("dve", lambda e, o=tmp[2][0], x2=x2, cosb=cosb: e.tensor_tensor(out=o[:], in0=x2, in1=cosb, op=ALU.mult),
                     reads=[bq_, b_rope], writes=[tmp[2][1]])
                S.op("pool", lambda e, o=tmp[3][0], x1=x1, sinb=sinb: e.tensor_tensor(out=o[:], in0=x1, in1=sinb, op=ALU.mult),
                     reads=[bq_, b_rope], writes=[tmp[3][1]])
                s4 = sg[:].rearrange("p (g h d) -> p g h d", h=2, d=32)
                S.op("dve", lambda e, s4=s4, a=tmp[0][0], b=tmp[1][0]: e.tensor_tensor(
                    out=s4[:, :, 0, :], in0=a[:], in1=b[:], op=ALU.subtract),
                    reads=[tmp[0][1], tmp[1][1]], writes=[bsg])
                S.op("pool", lambda e, s4=s4, a=tmp[2][0], b=tmp[3][0]: e.tensor_tensor(
                    out=s4[:, :, 1, :], in0=a[:], in1=b[:], op=ALU.add),
                    reads=[tmp[2][1], tmp[3][1]], writes=[bsg])
                if cg < 2:
                    dst = q_out[rows, cg * 512:(cg + 1) * 512]
                else:
                    dst = kvu_out[rows, (cg - 2) * 512:(cg - 1) * 512]
            else:
                S.op("act", lambda e, sg=sg, ps=ps: e.copy(out=sg[:], in_=ps[:, :]), reads=[P.pbb[bank]], writes=[bsg])
                dst = kvu_out[rows, 1024 + (cg - 4) * 512:1024 + (cg - 3) * 512]
            S.dma("sp", dst, sg[:], reads=[bsg], writes=[bo])
    return bo


def build_A_even():
    P = Prog()
    P.consts()
    bo = emit_A_even(P)
    return P.finish([bo]), P

def out_proj(P, w_out, gate_bc, b_gate):
    S, M = P.S, P.M
    CW = 256
    slabs = [M.sb("oslab%d" % i, [128, KC, CW], BF16) for i in range(2)]
    bslab = [Buf(), Buf()]
    tmp = [M.sb("otmp%d" % i, [128, CW], F32) for i in range(2)]
    btmp = [Buf(), Buf()]
    for cg in range(D // CW):
        sl, bsl = slabs[cg % 2], bslab[cg % 2]
        S.dma("pool", sl[:], w_out[:, cg * CW:(cg + 1) * CW].rearrange("(k p) c -> p k c", p=128), writes=[bsl])
        for t in range(NT):
            typ = 0 if t < 8 else 1
            bank = 6 + P.rr("op", 2)
            ps = P.pb[bank][:, 0:CW]
            mm_group(P, ps, [(P.actT[:, k, t * 128:(t + 1) * 128], sl[:, k, :]) for k in range(KC)],
                     reads=[P.bact[t], bsl], writes=[P.pbb[bank]])
            ti = P.rr("otmp", 2)
            tm, btm = tmp[ti], btmp[ti]
            S.op("dve", lambda e, tm=tm, ps=ps, typ=typ, cg=cg: e.tensor_tensor(
                out=tm[:], in0=ps, in1=gate_bc[:, typ, cg * CW:(cg + 1) * CW], op=ALU.mult),
                reads=[P.pbb[bank], b_gate], writes=[btm])
            xs = P.X[:, t, cg * CW:(cg + 1) * CW]
            S.op("dve", lambda e, xs=xs, tm=tm: e.tensor_tensor(out=xs, in0=xs, in1=tm[:], op=ALU.add),
                 reads=[btm, P.bX[t]], writes=[P.bX[t]])


def moe(P, w_router, b_router_bc, w_gu_r, b_guT, w_down, b_down, gate_bc, b_gate, n_exp=NEXP):
    S, M = P.S, P.M
    wr = M.sb("wr", [128, KC, NEXP], BF16)
    bwr = Buf()
    S.dma("pool", wr[:], w_router, writes=[bwr])
    brt, bbrt = b_router_bc
    G = M.sb("G", [128, NT, NEXP], F32)
    bG = [Buf() for _ in range(NT)]
    lg = M.sb("lg", [128, NT, NEXP], F32)
    mx = M.sb("mx", [128, NT, 8], F32)
    den = M.sb("den", [128, NT, 2], F32)
    for t in range(NT):
        bank = P.rr("rt", 2)
        ps = P.pb[bank][:, 0:NEXP]
        mm_group(P, ps, [(P.actT[:, k, t * 128:(t + 1) * 128], wr[:, k, :]) for k in range(KC)],
                 reads=[P.bact[t], bwr], writes=[P.pbb[bank]])
        l_, m_, g_, d_ = lg[:, t, :], mx[:, t, :], G[:, t, :], den[:, t, :]
        S.op("dve", lambda e, l_=l_, ps=ps: e.tensor_tensor(out=l_, in0=ps, in1=brt[:], op=ALU.add),
             reads=[P.pbb[bank], bbrt], writes=[bG[t]])
        S.op("dve", lambda e, l_=l_, m_=m_: e.max(out=m_, in_=l_), reads=[bG[t]], writes=[bG[t]])
        S.op("dve", lambda e, m_=m_, d_=d_: e.tensor_scalar(out=d_[:, 0:1], in0=m_[:, 0:1], scalar1=-1.0, scalar2=None,
                                                         op0=ALU.mult), reads=[bG[t]], writes=[bG[t]])
        S.op("act", lambda e, g_=g_, l_=l_, d_=d_: e.activation(out=g_, in_=l_, func=AF.Exp, bias=d_[:, 0:1], scale=1.0),
             reads=[bG[t]], writes=[bG[t]])
        S.op("dve", lambda e, l_=l_, m_=m_: e.tensor_scalar(out=l_, in0=l_, scalar1=m_[:, 3:4], scalar2=None, op0=ALU.is_ge),
             reads=[bG[t]], writes=[bG[t]])
        S.op("dve", lambda e, g_=g_, l_=l_: e.tensor_tensor(out=g_, in0=g_, in1=l_, op=ALU.mult), reads=[bG[t]], writes=[bG[t]])
        S.op("dve", lambda e, g_=g_, d_=d_: e.tensor_reduce(out=d_[:, 1:2], in_=g_, axis=AX.X, op=ALU.add),
             reads=[bG[t]], writes=[bG[t]])
        S.op("dve", lambda e, d_=d_: e.reciprocal(out=d_[:, 1:2], in_=d_[:, 1:2]), reads=[bG[t]], writes=[bG[t]])
        S.op("dve", lambda e, g_=g_, d_=d_: e.tensor_scalar(out=g_, in0=g_, scalar1=d_[:, 1:2], scalar2=None, op0=ALU.mult),
             reads=[bG[t]], writes=[bG[t]])
    bgu = M.sb("bgu", [128, NEXP, 12], F32)
    bbgu = Buf()
    S.dma("sp", bgu[:], b_guT, writes=[bbgu])
    bdn = M.sb("bdn", [NEXP, D], BF16)
    bbdn = Buf()
    S.dma("pool", bdn[:], b_down, writes=[bbdn])
    GT = M.sb("GT", [NEXP, T], BF16)
    bGT = [Buf() for _ in range(NT)]
    tmp = [M.sb("mtmp%d" % i, [128, 512], F32) for i in range(2)]
    btmp = [Buf(), Buf()]
    for t in range(NT):
        typ = 0 if t < 8 else 1
        bank = P.rr("rt", 2)
        pt = P.pb[bank][0:NEXP, 0:128]
        S.op("pe", lambda e, pt=pt, t=t: e.transpose(pt, G[:, t, :], P.identf[:]), reads=[bG[t], P.b_identf],
             writes=[P.pbb[bank]])
        S.op("act", lambda e, pt=pt, t=t: e.copy(out=GT[:, t * 128:(t + 1) * 128], in_=pt), reads=[P.pbb[bank]],
             writes=[bGT[t]])
        for nb in range(4):
            bank = 4 + nb
            ps = P.pb[bank]
            mm_group(P, ps[:, :], [(GT[:, t * 128:(t + 1) * 128], bdn[:, nb * 512:(nb + 1) * 512])],
                     reads=[bGT[t], bbdn], writes=[P.pbb[bank]])
            ti = P.rr("mtmp", 2)
            tm, btm = tmp[ti], btmp[ti]
            S.op("dve", lambda e, tm=tm, ps=ps, typ=typ, nb=nb: e.tensor_tensor(
                out=tm[:], in0=ps[:, :], in1=gate_bc[:, typ, nb * 512:(nb + 1) * 512], op=ALU.mult),
                reads=[P.pbb[bank], b_gate], writes=[btm])
            xs = P.X[:, t, nb * 512:(nb + 1) * 512]
            S.op("dve", lambda e, xs=xs, tm=tm: e.tensor_tensor(out=xs, in0=xs, in1=tm[:], op=ALU.add),
                 reads=[btm, P.bX[t]], writes=[P.bX[t]])
    slabs = [M.sb("gslab%d" % i, [128, KC, 256], BF16) for i in range(2)]
    bslab = [Buf(), Buf()]
    wd = M.sb("wd", [128, 6, D], BF16)
    bwd = Buf()
    gact = M.sb("gact", [128, 6, T], BF16)
    bga = [Buf() for _ in range(3)]
    gc = [M.sb("gc%d" % i, [128, 512], F32) for i in range(2)]
    sg = [M.sb("sgm%d" % i, [128, 512], BF16) for i in range(2)]
    uc = [M.sb("uc%d" % i, [128, 512], BF16) for i in range(2)]
    bgc, bsg, buc = [Buf(), Buf()], [Buf(), Buf()], [Buf(), Buf()]
    si = 0
    for e_ in range(n_exp):
        S.dma("pool", wd[:], w_down[e_].rearrange("(k p) c -> p k c", p=128), writes=[bwd])
        for i in range(6):
            sl, bsl = slabs[si % 2], bslab[si % 2]
            si += 1
            S.dma("pool", sl[:], w_gu_r[e_, i].rearrange("(k p) c -> p k c", p=128), writes=[bsl])
            for jj in range(1):
                for ng, (n0, nsz) in enumerate(NTILES_N):
                    tl = [n0 // 128 + x for x in range(nsz // 128)]
                    rd = [P.bact[t] for t in tl]
                    bkg, bku = P.rr("gu", 2) * 2, None
                    bku = bkg + 1
                    pg, pu = P.pb[bkg][:, 0:nsz], P.pb[bku][:, 0:nsz]
                    mm_group(P, pg, [(sl[:, k, 0:128], P.actT[:, k, n0:n0 + nsz]) for k in range(KC)],
                             reads=rd + [bsl], writes=[P.pbb[bkg]])
                    mm_group(P, pu, [(sl[:, k, 128:256], P.actT[:, k, n0:n0 + nsz]) for k in range(KC)],
                             reads=rd + [bsl], writes=[P.pbb[bku]])
                    x = P.rr("sw", 2)
                    gcv, sgv, ucv = gc[x][:, 0:nsz], sg[x][:, 0:nsz], uc[x][:, 0:nsz]
                    S.op("dve", lambda e, gcv=gcv, pg=pg, e_=e_, i=i: e.tensor_scalar(
                        out=gcv, in0=pg, scalar1=bgu[:, e_, i:i + 1], scalar2=7.0, op0=ALU.add, op1=ALU.min),
                        reads=[P.pbb[bkg], bbgu], writes=[bgc[x]])
                    S.op("act", lambda e, sgv=sgv, gcv=gcv: e.activation(out=sgv, in_=gcv, func=AF.Sigmoid, scale=1.702),
                         reads=[bgc[x]], writes=[bsg[x]])
                    S.op("dve", lambda e, ucv=ucv, pu=pu, e_=e_, i=i: e.tensor_scalar(
                        out=ucv, in0=pu, scalar1=bgu[:, e_, 6 + i:7 + i], scalar2=7.0, op0=ALU.add, op1=ALU.min),
                        reads=[P.pbb[bku], bbgu], writes=[buc[x]])
                    S.op("dve", lambda e, ucv=ucv: e.tensor_scalar(out=ucv, in0=ucv, scalar1=-7.0, scalar2=1.0,
                                                                   op0=ALU.max, op1=ALU.add), reads=[buc[x]], writes=[buc[x]])
                    S.op("dve", lambda e, gcv=gcv, sgv=sgv: e.tensor_tensor(out=gcv, in0=gcv, in1=sgv, op=ALU.mult),
                         reads=[bgc[x], bsg[x]], writes=[bgc[x]])
                    S.op("dve", lambda e, gcv=gcv, ucv=ucv, i=i, n0=n0, nsz=nsz: e.tensor_tensor(
                        out=gact[:, i, n0:n0 + nsz], in0=gcv, in1=ucv, op=ALU.mult),
                        reads=[bgc[x], buc[x]], writes=[bga[ng]])
        for t in range(NT):
            typ = 0 if t < 8 else 1
            ng = 0 if t < 4 else (1 if t < 8 else 2)
            for nb in range(4):
                bank = 4 + nb
                ps = P.pb[bank]
                pairs = [(gact[:, i, t * 128:(t + 1) * 128], wd[:, i, nb * 512:(nb + 1) * 512]) for i in range(6)]
                mm_group(P, ps[:, :], pairs, reads=[bga[ng], bwd], writes=[P.pbb[bank]])
                ti = P.rr("mtmp", 2)
                tm, btm = tmp[ti], btmp[ti]
                S.op("dve", lambda e, tm=tm, ps=ps, t=t, e_=e_, typ=typ, nb=nb: e.scalar_tensor_tensor(
                    out=tm[:], in0=ps[:, :], scalar=G[:, t, e_:e_ + 1], in1=gate_bc[:, typ, nb * 512:(nb + 1) * 512],
                    op0=ALU.mult, op1=ALU.mult), reads=[P.pbb[bank], bG[t], b_gate], writes=[btm])
                xs = P.X[:, t, nb * 512:(nb + 1) * 512]
                S.op("dve", lambda e, xs=xs, tm=tm: e.tensor_tensor(out=xs, in0=xs, in1=tm[:], op=ALU.add),
                     reads=[btm, P.bX[t]], writes=[P.bX[t]])


def store_x(P, x_out, rows=NT):
    bo = Buf()
    if P.fused and not getattr(P, "final", False):
        return bo
    for t in range(rows):
        P.S.dma("sp", x_out[t * 128:(t + 1) * 128, :], P.X[:, t, :], reads=[P.bX[t]], writes=[bo])
    return bo


def moe_inputs(P):
    w_router = P.din("w_router", [128, KC, NEXP])
    brt = load_small(P, "b_router", [128, NEXP])
    w_gu_r = P.din("w_gu_r", [NEXP, 6, D, 256])
    b_guT = P.din("b_guT", [128, NEXP, 12])
    w_down = P.din("w_down", [NEXP, DEXP, D])
    b_down = P.din("b_down", [NEXP, D])
    return w_router, brt, w_gu_r, b_guT, w_down, b_down


def emit_moe_test(P, n_exp=NEXP):
    x_in = P.din("x_in", [T, D])
    x_out = P.dout("x_out", [T, D])
    modl, P.b_modl = load_small(P, "modl", [128, 2, 96])
    gffn, P.b_g = load_small(P, "gffn", [128, KC])
    mi = moe_inputs(P)
    load_x(P, x_in)
    A, B, bAB = mod_vectors(P, modl, gffn[:], 4, 3, "ffn")
    prenorm(P, A, B, bAB, "ffn")
    g2 = P.M.sb("g2v", [128, 2, KC], F32)
    bg2 = Buf()
    P.S.op("dve", lambda e: e.tensor_copy(out=g2[:], in_=modl[:, :, 80:96]), reads=[P.b_modl], writes=[bg2])
    gate_bc, b_gate = bcast_rows(P, g2, bg2, "g2")
    moe(P, *mi, gate_bc, b_gate, n_exp=n_exp)
    bo = store_x(P, x_out)
    return bo


def build_moe_test(n_exp=NEXP):
    P = Prog()
    P.consts()
    bo = emit_moe_test(P, n_exp=NEXP)
    return P.finish([bo]), P

QGROUPS = [(0, 512), (512, 512), (1024, 128)]
QG256 = [(0, 256), (256, 256), (512, 256), (768, 256), (1024, 128)]


def emit_B1_even(P, lam_init):
    S, M = P.S, P.M
    x_in = P.din("x_in", [T, D])
    q_in = P.din("q_in", [T, 1024], BF16)
    kvu = P.din("kvu_in", [2304, 3072], BF16)
    poolA = P.din("poolA", [4, NT, 3, 128, 128], BF16)
    w_pool = P.din("w_pool", [4, 256, 256])
    w_out = P.din("w_out", [D, D])
    x_out = P.dout("x_out", [T, D])
    modl, P.b_modl = load_small(P, "modl", [128, 2, 96])
    spool, b_spool = load_small(P, "s_poolT", [128, 8])
    lamr, b_lamr = load_small(P, "lam", [128, 256])
    gsub, b_gsub = load_small(P, "g_subln", [128, 128])
    load_x(P, x_in)
    P.actT = M.sb("actT", [128, KC, T], BF16)
    P.bact = [Buf() for _ in range(NT)]
    sc = M.sb("sc", [128, 8], F32)
    bsc = Buf()
    lp = M.sb("lp", [128, 128], F32)
    S.op("dve", lambda e: e.tensor_tensor(out=lp[:, 0:64], in0=lamr[:, 0:64], in1=lamr[:, 64:128], op=ALU.mult),
         reads=[b_lamr], writes=[bsc])
    S.op("dve", lambda e: e.tensor_tensor(out=lp[:, 64:128], in0=lamr[:, 128:192], in1=lamr[:, 192:256], op=ALU.mult),
         reads=[b_lamr, bsc], writes=[bsc])
    S.op("dve", lambda e: e.tensor_reduce(out=sc[:, 0:2], in_=lp[:].rearrange("p (a d) -> p a d", d=64), axis=AX.X, op=ALU.add),
         reads=[bsc], writes=[bsc])
    S.op("act", lambda e: e.activation(out=sc[:, 2:4], in_=sc[:, 0:2], func=AF.Exp), reads=[bsc], writes=[bsc])
    S.op("dve", lambda e: e.tensor_tensor(out=sc[:, 4:5], in0=sc[:, 3:4], in1=sc[:, 2:3], op=ALU.subtract), reads=[bsc], writes=[bsc])
    S.op("dve", lambda e: e.tensor_scalar(out=sc[:, 4:5], in0=sc[:, 4:5], scalar1=-float(lam_init), scalar2=None, op0=ALU.add),
         reads=[bsc], writes=[bsc])
    neglam = sc[:, 4:5]
    S.op("dve", lambda e: e.tensor_scalar(out=gsub[:], in0=gsub[:], scalar1=float(1.0 - lam_init), scalar2=None, op0=ALU.mult),
         reads=[b_gsub], writes=[b_gsub])
    Kl = M.sb("Kl", [128, 18, 128], BF16)
    Va = M.sb("Va", [128, 18, 129], BF16)
    Ql = M.sb("Ql", [128, NT, 128], BF16)
    KT = M.sb("KT", [128, 2304], BF16)
    QT = M.sb("QT", [128, T], BF16)
    PT = [M.sb("PT%d" % i, [128, 18, 256], BF16) for i in range(1)]
    o0 = M.sb("o0", [128, NT, 128], F32)
    ob = M.sb("ob", [128, 128], F32)
    ytok = M.sb("ytok", [128, 128], BF16)
    sm = M.sb("sm", [128, 8], F32)
    bKl, bVa, bQl, bKT, bQT, bo0, bob, byt, bsm = [Buf() for _ in range(9)]
    bPT = [Buf()]
    S.op("pool", lambda e: e.memset(Va[:, :, 128:129], 1.0), writes=[bVa])
    for h in range(8):
        S.dma("sp", Kl[:], kvu[:, h * 128:(h + 1) * 128].rearrange("(t p) c -> p t c", p=128), writes=[bKl])
        S.dma("act", Va[:, :, 0:128], kvu[:, 1024 + h * 128:1024 + (h + 1) * 128].rearrange("(t p) c -> p t c", p=128),
              writes=[bVa])
        S.dma("sp", Ql[:], q_in[:, h * 128:(h + 1) * 128].rearrange("(t p) c -> p t c", p=128), writes=[bQl])
        for (src, bsrc, ntl, dst, bdst) in ((Kl, bKl, 18, KT, bKT), (Ql, bQl, NT, QT, bQT)):
            for g0 in range(0, ntl, 8):
                n = min(8, ntl - g0)
                bank = P.rr("tp", 2)
                pv = P.pb[bank][:, :].bitcast(BF16)

                def tr(e, src=src, g0=g0, n=n, pv=pv):
                    ins = None
                    for j in range(n):
                        ins = e.transpose(pv[:, j * 128:(j + 1) * 128], src[:, g0 + j, :], P.identb[:])
                    return ins
                S.op("pe", tr, reads=[bsrc, P.b_identb], writes=[P.pbb[bank]])
                eng = "act" if (g0 // 8) % 2 == 0 else "dve"
                if eng == "act":
                    S.op("act", lambda e, dst=dst, g0=g0, n=n, pv=pv: e.copy(out=dst[:, g0 * 128:(g0 + n) * 128], in_=pv[:, 0:n * 128]),
                         reads=[P.pbb[bank]], writes=[bdst])
                else:
                    S.op("dve", lambda e, dst=dst, g0=g0, n=n, pv=pv: e.tensor_copy(out=dst[:, g0 * 128:(g0 + n) * 128], in_=pv[:, 0:n * 128]),
                         reads=[P.pbb[bank]], writes=[bdst])
        for m in range(2):
            ms = slice(m * 64, (m + 1) * 64)
            for (q0, qn_) in QG256:
                ktiles = list(range(18)) if q0 < 1024 else [16, 17]
                pi = 0
                pt, bpt = PT[pi], bPT[pi]
                for kt in ktiles:
                    bank = 2 + P.rr("sc", 2)
                    ps = P.pb[bank][:, 0:qn_]
                    mm_group(P, ps, [(KT[ms, kt * 128:(kt + 1) * 128], QT[ms, q0:q0 + qn_])], reads=[bKT, bQT],
                             writes=[P.pbb[bank]])
                    S.op("act", lambda e, pt=pt, kt=kt, ps=ps, qn_=qn_: e.activation(
                        out=pt[:, kt, 0:qn_], in_=ps, func=AF.Exp, scale=0.125), reads=[P.pbb[bank]], writes=[bpt])
                for qq in range(qn_ // 128):
                    t = q0 // 128 + qq
                    bank = 4 + P.rr("av", 2)
                    po = P.pb[bank][:, 0:129]
                    mm_group(P, po, [(pt[:, kt, qq * 128:(qq + 1) * 128], Va[:, kt, :]) for kt in ktiles],
                             reads=[bpt, bVa], writes=[P.pbb[bank]])
                    S.op("dve", lambda e, po=po: e.reciprocal(out=sm[:, 0:1], in_=po[:, 128:129]), reads=[P.pbb[bank]],
                         writes=[bsm])
                    if m == 0:
                        S.op("dve", lambda e, po=po, t=t: e.tensor_scalar(out=o0[:, t, :], in0=po[:, 0:128], scalar1=sm[:, 0:1],
                                                                        scalar2=None, op0=ALU.mult),
                             reads=[P.pbb[bank], bsm], writes=[bo0])
                    else:
                        S.op("dve", lambda e: e.tensor_tensor(out=sm[:, 1:2], in0=sm[:, 0:1], in1=neglam, op=ALU.mult),
                             reads=[bsm, bsc], writes=[bsm])
                        S.op("dve", lambda e, po=po, t=t: e.scalar_tensor_tensor(
                            out=ob[:], in0=po[:, 0:128], scalar=sm[:, 1:2], in1=o0[:, t, :], op0=ALU.mult, op1=ALU.add),
                            reads=[P.pbb[bank], bsm, bo0], writes=[bob])
                        S.op("act", lambda e: e.activation(out=ytok[:], in_=ob[:], func=AF.Square, accum_out=sm[:, 2:3]),
                             reads=[bob], writes=[byt, bsm])
                        S.op("act", lambda e: e.activation(out=sm[:, 2:3], in_=sm[:, 2:3], func=AF.Sqrt, bias=P.epsc[:, 0:1],
                                                           scale=1.0 / 128), reads=[bsm, P.b_eps], writes=[bsm])
                        S.op("dve", lambda e: e.reciprocal(out=sm[:, 2:3], in_=sm[:, 2:3]), reads=[bsm], writes=[bsm])
                        S.op("dve", lambda e: e.scalar_tensor_tensor(out=ytok[:], in0=ob[:], scalar=sm[:, 2:3], in1=gsub[:],
                                                                     op0=ALU.mult, op1=ALU.mult),
                             reads=[bob, bsm, b_gsub], writes=[byt])
                        bk2 = P.rr("tp", 2)
                        pv = P.pb[bk2][:, :].bitcast(BF16)
                        S.op("pe", lambda e, pv=pv: e.transpose(pv[:, 0:128], ytok[:], P.identb[:]), reads=[byt, P.b_identb],
                             writes=[P.pbb[bk2]])
                        S.op("act", lambda e, pv=pv, h=h, t=t: e.copy(out=P.actT[:, h, t * 128:(t + 1) * 128], in_=pv[:, 0:128]),
                             reads=[P.pbb[bk2]], writes=[P.bact[t]])
    Ug = M.sb("Ug", [128, 18, 256], BF16)
    PA = M.sb("PA", [128, NT * 3, 128], BF16)
    pooledT = M.sb("pooledT", [128, 2, T], BF16)
    wp = M.sb("wp", [128, 4, 2, 256], BF16)
    bUg, bPA, bpl, bwp = Buf(), Buf(), Buf(), Buf()
    S.dma("pool", wp[:], w_pool.rearrange("g (c p) e -> p g c e", p=128), writes=[bwp])
    for g in range(4):
        S.dma("sp", Ug[:], kvu[:, 2048 + g * 256:2048 + (g + 1) * 256].rearrange("(t p) c -> p t c", p=128), writes=[bUg])
        S.dma("act", PA[:], poolA[g].rearrange("j k p t -> p (j k) t"), writes=[bPA])
        for j in range(NT):
            if j < 8:
                kts = [(j - 1) if j > 0 else 15, j, (j + 1) if j < 7 else 8]
            else:
                kts = [17, 16, 17]
            bank = 6 + P.rr("op", 2)
            for cc in range(2):
                ps = P.pb[bank][:, cc * 128:(cc + 1) * 128]
                mm_group(P, ps, [(Ug[:, kts[kk], cc * 128:(cc + 1) * 128], PA[:, j * 3 + kk, :]) for kk in range(3)],
                         reads=[bUg, bPA] + ([P.pbb[bank]] if cc else []), writes=[P.pbb[bank]])
            S.op("act", lambda e, bank=bank, j=j: e.copy(
                out=pooledT[:, :, j * 128:(j + 1) * 128], in_=P.pb[bank][:, 0:256].rearrange("p (c t) -> p c t", c=2)),
                reads=[P.pbb[bank]], writes=[bpl])
        for ec in range(2):
            for (n0, nsz) in QGROUPS:
                bank = 6 + P.rr("op", 2)
                ps = P.pb[bank][:, 0:nsz]
                mm_group(P, ps, [(wp[:, g, cc, ec * 128:(ec + 1) * 128], pooledT[:, cc, n0:n0 + nsz]) for cc in range(2)],
                         reads=[bwp, bpl], writes=[P.pbb[bank]])
                ch = 8 + g * 2 + ec
                tl = [P.bact[n0 // 128 + x] for x in range(nsz // 128)]
                S.op("act", lambda e, ps=ps, ch=ch, n0=n0, nsz=nsz, g=g, ec=ec: e.activation(
                    out=P.actT[:, ch, n0:n0 + nsz], in_=ps, func=AF.Identity, scale=spool[:, g * 2 + ec:g * 2 + ec + 1]),
                    reads=[P.pbb[bank], b_spool], writes=tl)
    g1 = M.sb("g1v", [128, 2, KC], F32)
    bg1 = Buf()
    S.op("dve", lambda e: e.tensor_copy(out=g1[:], in_=modl[:, :, 32:48]), reads=[P.b_modl], writes=[bg1])
    gate_bc, b_gate = bcast_rows(P, g1, bg1, "g1")
    out_proj(P, w_out, gate_bc, b_gate)
    bo = store_x(P, x_out)
    return bo


def build_B1_even(lam_init):
    P = Prog()
    P.consts()
    bo = emit_B1_even(P, lam_init)
    return P.finish([bo]), P

def rms_rstd(P, sq_ap, out_ap, n, reads, bout):
    S = P.S
    S.op("act", lambda e: e.activation(out=out_ap, in_=out_ap, func=AF.Sqrt, bias=P.epsc[:, 0:1], scale=1.0 / n),
         reads=[bout, P.b_eps] + reads, writes=[bout])
    S.op("dve", lambda e: e.reciprocal(out=out_ap, in_=out_ap), reads=[bout], writes=[bout])


def emit_A_odd(P):
    S, M = P.S, P.M
    x_in = P.din("x_in", [T, D])
    w_in = P.din("w_in", [D, 1600])
    w_qb = P.din("w_qb", [512, 2304])
    w_kvb = P.din("w_kvb", [512, 3072])
    q_out = P.dout("q_out", [T, 2304], BF16)
    kvu_out = P.dout("kvu_out", [T, 4352], BF16)
    modl, P.b_modl = load_small(P, "modl", [128, 2, 96])
    gmix, P.b_g = load_small(P, "gmix", [128, KC])
    gqa, b_gqa = load_small(P, "g_qa", [128, 512])
    gkva, b_gkva = load_small(P, "g_kva", [128, 512])
    gmq, b_gmq = load_small(P, "g_mq", [128, 192])
    gmk, b_gmk = load_small(P, "g_mk", [128, 192])
    rope, b_rope = load_small(P, "rope", [128, NT, 2, 32])
    stream_x(P, x_in)
    A, B, bAB = mod_vectors(P, modl, gmix[:], 1, 0, "mix")
    prenorm(P, A, B, bAB, "mix")
    wq = M.sb("wq", [128, 4, 2304], BF16)
    wkvs = [M.sb("wkv%d" % i, [128, 4, 512], BF16) for i in range(2)]
    bwkvs = [Buf(), Buf()]
    bwq = Buf()
    S.dma("pool", wq[:], w_qb.rearrange("(k p) c -> p k c", p=128), writes=[bwq])
    slabs = [M.sb("slab%d" % i, [128, KC, 512], BF16) for i in range(1)] * 2
    bslab = [Buf()] * 2
    cT = [M.sb("cqT", [128, 4, T], BF16), M.sb("ckvT", [128, 4, T], BF16)]
    bcT = [[Buf() for _ in range(NT)] for _ in range(2)]
    kpe = M.sb("kpe", [128, NT, 64], F32)
    bkpe = [Buf() for _ in range(NT)]
    sspe = M.sb("sspe", [128, NT], F32)
    stg = [M.sb("stg%d" % i, [128, 640], BF16) for i in range(2)]
    bstg = [Buf(), Buf()]
    sqb = M.sb("sqb", [128, 512], F32)
    bsq = Buf()
    cn = M.sb("cn", [128, 512], BF16)
    bcn = Buf()
    qn = M.sb("qn", [128, 512], F32)
    bqn = Buf()
    ra = [M.sb("ra%d" % i, [128, 2, 32], F32) for i in range(4)]
    bra = [Buf() for _ in range(4)]
    rs = M.sb("rs", [128, 4], F32)
    brs = Buf()
    bo = Buf()
    cgs = [(0, 512), (512, 512), (1024, 512), (1536, 64)]
    for cg, (c0, cw) in enumerate(cgs):
        sl, bsl = slabs[cg % 2], bslab[cg % 2]
        S.dma("pool", sl[:, :, 0:cw], w_in[:, c0:c0 + cw].rearrange("(k p) c -> p k c", p=128), writes=[bsl])
        for t in range(NT):
            bank = 4 + P.rr("ip", 4)
            ps = P.pb[bank][:, 0:cw]
            mm_group(P, ps, [(P.actT[:, k, t * 128:(t + 1) * 128], sl[:, k, 0:cw]) for k in range(KC)],
                     reads=[P.bact[t], bsl], writes=[P.pbb[bank]])
            rows = slice(t * 128, (t + 1) * 128)
            if cg < 2:
                gvec, bg_ = (gqa, b_gqa) if cg == 0 else (gkva, b_gkva)
                S.op("act", lambda e, ps=ps: e.activation(out=sqb[:], in_=ps, func=AF.Square, accum_out=rs[:, 0:1]),
                     reads=[P.pbb[bank]], writes=[bsq, brs])
                rms_rstd(P, None, rs[:, 0:1], 512, [], brs)
                S.op("dve", lambda e, ps=ps, gvec=gvec: e.scalar_tensor_tensor(
                    out=cn[:], in0=ps, scalar=rs[:, 0:1], in1=gvec[:], op0=ALU.mult, op1=ALU.mult),
                    reads=[P.pbb[bank], brs, bg_], writes=[bcn])
                bk2 = P.rr("tp", 2)
                pv = P.pb[bk2][:, :].bitcast(BF16)

                def tr(e, pv=pv):
                    ins = None
                    for j in range(4):
                        ins = e.transpose(pv[:, j * 128:(j + 1) * 128], cn[:, j * 128:(j + 1) * 128], P.identb[:])
                    return ins
                S.op("pe", tr, reads=[bcn, P.b_identb], writes=[P.pbb[bk2]])
                S.op("act", lambda e, pv=pv, cg=cg, t=t: e.copy(
                    out=cT[cg][:, :, t * 128:(t + 1) * 128], in_=pv[:, 0:512].rearrange("p (c t) -> p c t", c=4)),
                    reads=[P.pbb[bk2]], writes=[bcT[cg][t]])
            elif cg == 2:
                si = P.rr("stg", 2)
                S.op("act", lambda e, si=si, ps=ps: e.copy(out=stg[si][:, 0:512], in_=ps), reads=[P.pbb[bank]], writes=[bstg[si]])
                S.dma("sp", kvu_out[rows, 3840:4352], stg[si][:, 0:512], reads=[bstg[si]], writes=[bo])
            else:
                S.op("act", lambda e, ps=ps, t=t: e.copy(out=kpe[:, t, :], in_=ps), reads=[P.pbb[bank]], writes=[bkpe[t]])
                S.op("act", lambda e, t=t: e.activation(out=sqb[:, 0:64], in_=kpe[:, t, :], func=AF.Square,
                                                      accum_out=sspe[:, t:t + 1]), reads=[bkpe[t]], writes=[bsq, bkpe[t]])

    def rope2(src4, dst4, t, breads, bdst):
        x1, x2 = src4[:, :, 0, :], src4[:, :, 1, :]
        cosb = rope[:, t, 0, :].unsqueeze(1).to_broadcast([128, 2, 32])
        sinb = rope[:, t, 1, :].unsqueeze(1).to_broadcast([128, 2, 32])
        ids = [P.rr("ra", 4) for _ in range(4)]
        tm = [(ra[i], bra[i]) for i in ids]
        S.op("dve", lambda e: e.tensor_tensor(out=tm[0][0][:], in0=x1, in1=cosb, op=ALU.mult), reads=breads + [b_rope], writes=[tm[0][1]])
        S.op("pool", lambda e: e.tensor_tensor(out=tm[1][0][:], in0=x2, in1=sinb, op=ALU.mult), reads=breads + [b_rope], writes=[tm[1][1]])
        S.op("dve", lambda e: e.tensor_tensor(out=tm[2][0][:], in0=x2, in1=cosb, op=ALU.mult), reads=breads + [b_rope], writes=[tm[2][1]])
        S.op("pool", lambda e: e.tensor_tensor(out=tm[3][0][:], in0=x1, in1=sinb, op=ALU.mult), reads=breads + [b_rope], writes=[tm[3][1]])
        S.op("dve", lambda e: e.tensor_tensor(out=dst4[:, :, 0, :], in0=tm[0][0][:], in1=tm[1][0][:], op=ALU.subtract),
             reads=[tm[0][1], tm[1][1]], writes=[bdst])
        S.op("pool", lambda e: e.tensor_tensor(out=dst4[:, :, 1, :], in0=tm[2][0][:], in1=tm[3][0][:], op=ALU.add),
             reads=[tm[2][1], tm[3][1]], writes=[bdst])

    for g6 in range(6):
        for t in range(NT):
            bank = 4 + P.rr("ip", 4)
            ps = P.pb[bank][:, 0:384]
            mm_group(P, ps, [(cT[0][:, c, t * 128:(t + 1) * 128], wq[:, c, g6 * 384:(g6 + 1) * 384]) for c in range(4)],
                     reads=[bcT[0][t], bwq], writes=[P.pbb[bank]])
            S.op("act", lambda e, ps=ps: e.activation(out=sqb[:, 0:384], in_=ps, func=AF.Square), reads=[P.pbb[bank]], writes=[bsq])
            S.op("dve", lambda e: e.tensor_reduce(out=rs[:, 0:2], in_=sqb[:, 0:384].rearrange("p (h d) -> p h d", d=192),
                                                  axis=AX.X, op=ALU.add), reads=[bsq], writes=[brs])
            rms_rstd(P, None, rs[:, 0:2], 192, [], brs)
            q3 = qn[:, 0:384].rearrange("p (h d) -> p h d", d=192)
            S.op("dve", lambda e, ps=ps, q3=q3: e.tensor_tensor(out=q3, in0=ps.rearrange("p (h d) -> p h d", d=192),
                                                              in1=rs[:, 0:2].unsqueeze(2).to_broadcast([128, 2, 192]), op=ALU.mult),
                 reads=[P.pbb[bank], brs], writes=[bqn])
            S.op("pool", lambda e, q3=q3: e.tensor_tensor(out=q3, in0=q3, in1=gmq[:].unsqueeze(1).to_broadcast([128, 2, 192]),
                                                         op=ALU.mult), reads=[bqn, b_gmq], writes=[bqn])
            si = P.rr("stg", 2)
            s3 = stg[si][:, 0:384].rearrange("p (h d) -> p h d", d=192)
            S.op("act", lambda e, s3=s3, q3=q3: e.copy(out=s3[:, :, 0:128], in_=q3[:, :, 0:128]), reads=[bqn], writes=[bstg[si]])
            rope2(q3[:, :, 128:192].rearrange("p h (a d) -> p h a d", a=2), s3[:, :, 128:192].rearrange("p h (a d) -> p h a d", a=2),
                  t, [bqn], bstg[si])
            S.dma("sp", q_out[t * 128:(t + 1) * 128, g6 * 384:(g6 + 1) * 384], stg[si][:, 0:384], reads=[bstg[si]], writes=[bo])
    for g6 in range(6):
        wkv, bwkv = wkvs[g6 % 2], bwkvs[g6 % 2]
        S.dma("pool", wkv[:], w_kvb[:, g6 * 512:(g6 + 1) * 512].rearrange("(k p) c -> p k c", p=128), writes=[bwkv])
        for t in range(NT):
            bank = 4 + P.rr("ip", 4)
            ps = P.pb[bank][:, 0:512]
            mm_group(P, ps, [(cT[1][:, c, t * 128:(t + 1) * 128], wkv[:, c, :]) for c in range(4)],
                     reads=[bcT[1][t], bwkv], writes=[P.pbb[bank]])
            p4 = ps.rearrange("p (h a d) -> p h a d", h=2, a=2)
            S.op("act", lambda e, p4=p4: e.activation(out=sqb[:, 0:256].rearrange("p (h d) -> p h d", d=128), in_=p4[:, :, 0, :],
                                                    func=AF.Square), reads=[P.pbb[bank]], writes=[bsq])
            S.op("dve", lambda e: e.tensor_reduce(out=rs[:, 0:2], in_=sqb[:, 0:256].rearrange("p (h d) -> p h d", d=128),
                                                  axis=AX.X, op=ALU.add), reads=[bsq], writes=[brs])
            S.op("dve", lambda e, t=t: e.tensor_scalar(out=rs[:, 0:2], in0=rs[:, 0:2], scalar1=sspe[:, t:t + 1], scalar2=None,
                                                      op0=ALU.add), reads=[brs, bkpe[t]], writes=[brs])
            rms_rstd(P, None, rs[:, 0:2], 192, [], brs)
            si = P.rr("stg", 2)
            s3 = stg[si][:, 0:640].rearrange("p (h d) -> p h d", d=320)
            k3 = qn[:, 0:256].rearrange("p (h d) -> p h d", d=128)
            S.op("dve", lambda e, p4=p4, k3=k3: e.tensor_tensor(out=k3, in0=p4[:, :, 0, :],
                                                              in1=rs[:, 0:2].unsqueeze(2).to_broadcast([128, 2, 128]), op=ALU.mult),
                 reads=[P.pbb[bank], brs], writes=[bqn])
            S.op("pool", lambda e, k3=k3, s3=s3: e.tensor_tensor(out=s3[:, :, 0:128], in0=k3,
                                                                in1=gmk[:, 0:128].unsqueeze(1).to_broadcast([128, 2, 128]), op=ALU.mult),
                 reads=[bqn, b_gmk], writes=[bstg[si]])
            kp = qn[:, 256:384].rearrange("p (h d) -> p h d", d=64)
            S.op("dve", lambda e, kp=kp, t=t: e.tensor_tensor(out=kp, in0=kpe[:, t, :].unsqueeze(1).to_broadcast([128, 2, 64]),
                                                             in1=rs[:, 0:2].unsqueeze(2).to_broadcast([128, 2, 64]), op=ALU.mult),
                 reads=[bkpe[t], brs, bqn], writes=[bqn])
            S.op("pool", lambda e, kp=kp: e.tensor_tensor(out=kp, in0=kp, in1=gmk[:, 128:192].unsqueeze(1).to_broadcast([128, 2, 64]),
                                                         op=ALU.mult), reads=[bqn, b_gmk], writes=[bqn])
            rope2(kp.rearrange("p h (a d) -> p h a d", a=2), s3[:, :, 128:192].rearrange("p h (a d) -> p h a d", a=2), t, [bqn], bstg[si])
            S.op("act", lambda e, s3=s3, p4=p4: e.copy(out=s3[:, :, 192:320], in_=p4[:, :, 1, :]), reads=[P.pbb[bank]], writes=[bstg[si]])
            S.dma("sp", kvu_out[t * 128:(t + 1) * 128, g6 * 640:(g6 + 1) * 640], stg[si][:, 0:640], reads=[bstg[si]], writes=[bo])
    return bo


def build_A_odd():
    P = Prog()
    P.consts()
    bo = emit_A_odd(P)
    return P.finish([bo]), P


def emit_B1_odd(P):
    S, M = P.S, P.M
    x_in = P.din("x_in", [T, D])
    q_in = P.din("q_in", [T, 2304], BF16)
    kvu = P.din("kvu_in", [2304, 4352], BF16)
    CN = P.din("dftC", [2048, 1024], BF16)
    SN = P.din("dftS", [2048, 1024], BF16)
    CNc = P.din("dftCc", [256, 128], BF16)
    SNc = P.din("dftSc", [256, 128], BF16)
    CCd = P.din("dftCC", [128, 128], BF16)
    SCd = P.din("dftSC", [128, 128], BF16)
    w_f = P.din("w_fourier", [4, 128, 128])
    w_out = P.din("w_out", [D, D])
    x_out = P.dout("x_out", [T, D])
    modl, P.b_modl = load_small(P, "modl", [128, 2, 96])
    load_x(P, x_in)
    P.actT = M.sb("actT", [128, KC, T], BF16)
    P.bact = [Buf() for _ in range(NT)]
    Kl = M.sb("Kl", [128, 18, 192], BF16)
    Va = M.sb("Va", [128, 18, 129], BF16)
    Ql = M.sb("Ql", [128, NT, 192], BF16)
    KT0 = M.sb("KT0", [128, 2304], BF16)
    KT1 = M.sb("KT1", [64, 2304], BF16)
    QT0 = M.sb("QT0", [128, T], BF16)
    QT1 = M.sb("QT1", [64, T], BF16)
    PT = M.sb("PT", [128, 18, 256], BF16)
    ytok = M.sb("ytok", [128, 128], BF16)
    sm = M.sb("sm", [128, 4], F32)
    bKl, bVa, bQl, bKT, bQT, bPT, byt, bsm = [Buf() for _ in range(8)]
    S.op("pool", lambda e: e.memset(Va[:, :, 128:129], 1.0), writes=[bVa])
    scale = 192 ** -0.5
    for h in range(12):
        S.dma("sp", Kl[:], kvu[:, h * 320:h * 320 + 192].rearrange("(t p) c -> p t c", p=128), writes=[bKl])
        S.dma("act", Va[:, :, 0:128], kvu[:, h * 320 + 192:(h + 1) * 320].rearrange("(t p) c -> p t c", p=128), writes=[bVa])
        S.dma("sp", Ql[:], q_in[:, h * 192:(h + 1) * 192].rearrange("(t p) c -> p t c", p=128), writes=[bQl])
        for (src, bsrc, ntl, d0, d1, bdst) in ((Kl, bKl, 18, KT0, KT1, bKT), (Ql, bQl, NT, QT0, QT1, bQT)):
            for g0 in range(0, ntl, 8):
                n = min(8, ntl - g0)
                for part in range(2):
                    bank = P.rr("tp", 2)
                    pv = P.pb[bank][:, :].bitcast(BF16)
                    np_ = 128 if part == 0 else 64
                    c0 = 0 if part == 0 else 128

                    def tr(e, src=src, g0=g0, n=n, pv=pv, np_=np_, c0=c0):
                        ins = None
                        for j in range(n):
                            ins = e.transpose(pv[0:np_, j * 128:(j + 1) * 128], src[:, g0 + j, c0:c0 + np_], P.identb[:])
                        return ins
                    S.op("pe", tr, reads=[bsrc, P.b_identb], writes=[P.pbb[bank]])
                    dst = d0 if part == 0 else d1
                    if part == 0:
                        S.op("act", lambda e, dst=dst, g0=g0, n=n, pv=pv, np_=np_: e.copy(
                            out=dst[0:np_, g0 * 128:(g0 + n) * 128], in_=pv[0:np_, 0:n * 128]), reads=[P.pbb[bank]], writes=[bdst])
                    else:
                        S.op("dve", lambda e, dst=dst, g0=g0, n=n, pv=pv, np_=np_: e.tensor_copy(
                            out=dst[0:np_, g0 * 128:(g0 + n) * 128], in_=pv[0:np_, 0:n * 128]), reads=[P.pbb[bank]], writes=[bdst])
        for (q0, qn_) in QG256:
            ktiles = list(range(18)) if q0 < 1024 else [16, 17]
            for kt in ktiles:
                bank = 2 + P.rr("sc", 2)
                ps = P.pb[bank][:, 0:qn_]
                mm_group(P, ps, [(KT0[:, kt * 128:(kt + 1) * 128], QT0[:, q0:q0 + qn_]),
                                 (KT1[:, kt * 128:(kt + 1) * 128], QT1[:, q0:q0 + qn_])], reads=[bKT, bQT], writes=[P.pbb[bank]])
                S.op("act", lambda e, kt=kt, ps=ps, qn_=qn_: e.activation(out=PT[:, kt, 0:qn_], in_=ps, func=AF.Exp, scale=scale),
                     reads=[P.pbb[bank]], writes=[bPT])
            for qq in range(qn_ // 128):
                t = q0 // 128 + qq
                bank = 4 + P.rr("av", 2)
                po = P.pb[bank][:, 0:129]
                mm_group(P, po, [(PT[:, kt, qq * 128:(qq + 1) * 128], Va[:, kt, :]) for kt in ktiles], reads=[bPT, bVa],
                         writes=[P.pbb[bank]])
                S.op("dve", lambda e, po=po: e.reciprocal(out=sm[:, 0:1], in_=po[:, 128:129]), reads=[P.pbb[bank]], writes=[bsm])
                S.op("dve", lambda e, po=po: e.tensor_scalar(out=ytok[:], in0=po[:, 0:128], scalar1=sm[:, 0:1], scalar2=None,
                                                            op0=ALU.mult), reads=[P.pbb[bank], bsm], writes=[byt])
                bk2 = P.rr("tp", 2)
                pv = P.pb[bk2][:, :].bitcast(BF16)
                S.op("pe", lambda e, pv=pv: e.transpose(pv[:, 0:128], ytok[:], P.identb[:]), reads=[byt, P.b_identb],
                     writes=[P.pbb[bk2]])
                S.op("act", lambda e, pv=pv, h=h, t=t: e.copy(out=P.actT[:, h, t * 128:(t + 1) * 128], in_=pv[:, 0:128]),
                     reads=[P.pbb[bk2]], writes=[P.bact[t]])
    Ug = M.sb("Ug", [128, 18, 128], BF16)
    Csl = [M.sb("Csl%d" % i, [128, 16, 128], BF16) for i in range(2)]
    Ssl = [M.sb("Ssl%d" % i, [128, 16, 256], BF16) for i in range(2)] if False else None
    Cc = M.sb("Cc", [128, 2, 2, 128], BF16)
    AB = M.sb("AB", [128, 2, T], BF16)
    W12 = M.sb("W12", [128, 2, 4, 128], BF16)
    CS = M.sb("CS", [128, 2, 128], BF16)
    wf = M.sb("wf", [128, 4, 128], BF16)
    bUg, bCc, bAB, bW, bCS, bwf = [Buf() for _ in range(6)]
    bCsl = [Buf(), Buf()]
    S.dma("sp", Cc[:, 0], CNc.rearrange("(t p) c -> p t c", p=128), writes=[bCc])
    S.dma("sp", Cc[:, 1], SNc.rearrange("(t p) c -> p t c", p=128), writes=[bCc])
    S.dma("sp", CS[:, 0, :], CCd, writes=[bCS])
    S.dma("sp", CS[:, 1, :], SCd, writes=[bCS])
    S.dma("pool", wf[:], w_f.rearrange("g c e -> c g e"), writes=[bwf])
    for g in range(4):
        for ab in range(2):
            bank = 6 + P.rr("op", 2)
            ps = P.pb[bank][:, 0:128]
            mm_group(P, ps, [(CS[:, ab, :], wf[:, g, :])], reads=[bCS, bwf], writes=[P.pbb[bank]])
            S.op("act", lambda e, ps=ps, ab=ab, g=g: e.activation(out=W12[:, ab, g, :], in_=ps, func=AF.Identity,
                                                                scale=(1.0 if ab == 0 else -1.0)), reads=[P.pbb[bank]], writes=[bW])
    for g in range(4):
        S.dma("sp", Ug[:], kvu[:, 3840 + g * 128:3840 + (g + 1) * 128].rearrange("(t p) c -> p t c", p=128), writes=[bUg])
        for ab, tab in enumerate((CN, SN)):
            for og in range(8):
                ci = P.rr("csl", 2)
                S.dma("act" if ci else "sp", Csl[ci][:], tab[:, og * 128:(og + 1) * 128].rearrange("(t p) c -> p t c", p=128),
                      writes=[bCsl[ci]])
                bank = 6 + P.rr("op", 2)
                ps = P.pb[bank][:, 0:128]
                mm_group(P, ps, [(Ug[:, kt, :], Csl[ci][:, kt, :]) for kt in range(16)], reads=[bUg, bCsl[ci]], writes=[P.pbb[bank]])
                S.op("act" if og % 2 else "dve", (lambda e, ps=ps, ab=ab, g=g, og=og: e.copy(out=AB[:, ab, og * 128:(og + 1) * 128], in_=ps))
                     if og % 2 else (lambda e, ps=ps, ab=ab, g=g, og=og: e.tensor_copy(out=AB[:, ab, og * 128:(og + 1) * 128], in_=ps)),
                     reads=[P.pbb[bank]], writes=[bAB])
            bank = 6 + P.rr("op", 2)
            ps = P.pb[bank][:, 0:128]
            mm_group(P, ps, [(Ug[:, 16 + kt, :], Cc[:, ab, kt, :]) for kt in range(2)], reads=[bUg, bCc], writes=[P.pbb[bank]])
            S.op("act", lambda e, ps=ps, ab=ab, g=g: e.copy(out=AB[:, ab, 1024:1152], in_=ps), reads=[P.pbb[bank]], writes=[bAB])
        for (n0, nsz) in QGROUPS:
            bank = 6 + P.rr("op", 2)
            ps = P.pb[bank][:, 0:nsz]
            mm_group(P, ps, [(W12[:, 0, g, :], AB[:, 0, n0:n0 + nsz]), (W12[:, 1, g, :], AB[:, 1, n0:n0 + nsz])],
                     reads=[bW, bAB], writes=[P.pbb[bank]])
            tl = [P.bact[n0 // 128 + x] for x in range(nsz // 128)]
            S.op("act", lambda e, ps=ps, g=g, n0=n0, nsz=nsz: e.copy(out=P.actT[:, 12 + g, n0:n0 + nsz], in_=ps),
                 reads=[P.pbb[bank]], writes=tl)
    g1 = M.sb("g1v", [128, 2, KC], F32)
    bg1 = Buf()
    S.op("dve", lambda e: e.tensor_copy(out=g1[:], in_=modl[:, :, 32:48]), reads=[P.b_modl], writes=[bg1])
    gate_bc, b_gate = bcast_rows(P, g1, bg1, "g1")
    out_proj(P, w_out, gate_bc, b_gate)
    bo = store_x(P, x_out)
    return bo


def build_B1_odd():
    P = Prog()
    P.consts()
    bo = emit_B1_odd(P)
    return P.finish([bo]), P

def _fm(v):
    return np.ascontiguousarray(np.asarray(v, np.float32).reshape(-1, 128).T)


def _bc(v):
    return np.ascontiguousarray(np.tile(np.asarray(v, np.float32).reshape(1, -1), (128, 1)))


def _rope_tab(rot_dim):
    rows = 2048 // 64
    row = np.repeat(np.arange(rows, dtype=np.float32), 64)
    col = np.tile(np.arange(64, dtype=np.float32), rows)
    axis_dim = rot_dim // 2
    inv = (10000.0 ** (-np.arange(0, axis_dim, 2, dtype=np.float32) / axis_dim)).astype(np.float32)
    ang = np.concatenate([row[:, None] * inv, col[:, None] * inv], -1)
    return np.cos(ang).astype(np.float32), np.sin(ang).astype(np.float32)


def _rope_core(rot_dim, half):
    c, s = _rope_tab(rot_dim)
    rope = np.zeros((T, 2, rot_dim // 2), np.float32)
    rope[:, 0] = 1.0
    rope[:TL, 0] = c[half * TL:(half + 1) * TL]
    rope[:TL, 1] = s[half * TL:(half + 1) * TL]
    return np.ascontiguousarray(rope.reshape(NT, 128, 2, rot_dim // 2).transpose(1, 0, 2, 3))


def _pool_mats(half):
    out = np.zeros((4, NT, 3, 128, 128), np.float32)
    for wi, w in enumerate((2, 4, 8, 16)):
        for j in range(NT):
            n = 2048 if j < 8 else 256
            gt = (half * 8 + j) if j < 8 else half
            t = gt * 128 + np.arange(128)
            lo = np.clip(t - w // 2, 0, n)
            hi = np.clip(t + w - w // 2, 0, n)
            cnt = (hi - lo).astype(np.float32)
            for kk in range(3):
                gi = gt + kk - 1
                if gi < 0 or gi >= n // 128:
                    continue
                tp = gi * 128 + np.arange(128)
                m = ((tp[:, None] >= lo[None, :]) & (tp[:, None] < hi[None, :])).astype(np.float32) / cnt[None, :]
                m -= (tp[:, None] == t[None, :]).astype(np.float32)
                out[wi, j, kk] = m
    return out.astype(NPBF)


def _dft_tabs(half):
    i = np.arange(2048)
    n_in = np.where(i < 1024, half * 1024 + i, (1 - half) * 1024 + (i - 1024)).astype(np.int64)
    n_out = (half * 1024 + np.arange(1024)).astype(np.int64)
    ang = 2 * np.pi * ((n_in[:, None] * n_out[None, :]) % 2048).astype(np.float64) / 2048
    nrm = 1.0 / np.sqrt(2048 * 128)
    d = {"dftC": (np.cos(ang) * nrm).astype(NPBF), "dftS": (np.sin(ang) * nrm).astype(NPBF)}
    i = np.arange(256)
    n_in = np.where(i < 128, half * 128 + i, (1 - half) * 128 + (i - 128)).astype(np.int64)
    n_out = (half * 128 + np.arange(128)).astype(np.int64)
    ang = 2 * np.pi * ((n_in[:, None] * n_out[None, :]) % 256).astype(np.float64) / 256
    nrm = 1.0 / np.sqrt(256 * 128)
    d["dftCc"] = (np.cos(ang) * nrm).astype(NPBF)
    d["dftSc"] = (np.sin(ang) * nrm).astype(NPBF)
    c = np.arange(128)
    ang = 2 * np.pi * ((c[:, None] * c[None, :]) % 128).astype(np.float64) / 128
    d["dftCC"] = np.cos(ang).astype(NPBF)
    d["dftSC"] = np.sin(ang).astype(NPBF)
    return d


def _modl_core(mod, l, b):
    m = np.zeros((128, 2, 96), np.float32)
    for j in range(6):
        m[:, 0, j * 16:(j + 1) * 16] = _fm(mod[l, b, j * 2048:(j + 1) * 2048])
        m[:, 1, j * 16:(j + 1) * 16] = _fm(mod[l, 4, j * 2048:(j + 1) * 2048])
    return m


def _kv_pair(own, partner):
    return np.ascontiguousarray(np.concatenate([own[:TL], partner[:TL], own[TL:], partner[TL:]], 0))


def _moe_host(inp, l):
    wg = inp["w_gu"][l]
    g = wg[:, :, :DEXP].reshape(NEXP, D, 6, 1, 128)
    u = wg[:, :, DEXP:].reshape(NEXP, D, 6, 1, 128)
    w_gu_r = np.ascontiguousarray(np.concatenate([g, u], 3).transpose(0, 2, 1, 3, 4).reshape(NEXP, 6, D, 256))
    b_guT = np.ascontiguousarray(inp["b_gu"][l].reshape(NEXP, 12, 128).transpose(2, 0, 1))
    return {"w_router": np.ascontiguousarray(inp["w_router"][l].reshape(KC, 128, NEXP).transpose(1, 0, 2)),
            "b_router": _bc(inp["b_router"][l]), "w_gu_r": w_gu_r, "b_guT": b_guT,
            "w_down": np.ascontiguousarray(inp["w_down"][l]), "b_down": np.ascontiguousarray(inp["b_down"][l])}


_PROGS = {}


def _prog(key, fn):
    if key not in _PROGS:
        _PROGS[key] = fn()[0]
    return _PROGS[key]


def _run(nc, in_maps):
    res = run_bass_kernel_spmd(nc, in_maps, core_ids=list(range(NCORE)))
    return res.results


def kernel_unfused(**inp):
    inp = {k: np.asarray(v) for k, v in inp.items()}
    x, c, ctx, c_ctx = inp["x"], inp["c"], inp["ctx"], inp["c_ctx"]
    cores = [(cid // 2, cid % 2) for cid in range(NCORE)]
    C5 = np.concatenate([c, c_ctx[None]], 0).astype(np.float32)
    cT5 = np.ascontiguousarray(C5.reshape(5, KC, 128).transpose(2, 1, 0))
    ims = []
    for cid in range(NCORE):
        sl = slice(cid * 1536, (cid + 1) * 1536)
        ims.append({"cT5": cT5, "wmod_sh": np.ascontiguousarray(inp["w_mod"][:, :, sl]),
                    "bmodT": np.ascontiguousarray(inp["b_mod"][:, sl].reshape(DEPTH, 12, 128).transpose(2, 0, 1))})
    rM = _run(_prog("M", build_M), ims)
    mod = np.zeros((DEPTH, 5, 6 * D), np.float32)
    for cid in range(NCORE):
        o = rM[cid]["mod_sh"].reshape(128, DEPTH, 12, 5)
        mod[:, :, cid * 1536:(cid + 1) * 1536] = o.transpose(1, 3, 2, 0).reshape(DEPTH, 5, 1536)
    X = [np.ascontiguousarray(np.concatenate([x[b, h * TL:(h + 1) * TL], ctx[b, h * TCX:(h + 1) * TCX]], 0)) for (b, h) in cores]
    ropes = [_rope_core(64, h) for h in range(2)]
    perm = np.concatenate([np.arange(0, 1024), np.arange(1088, 1600), np.arange(1024, 1088)])
    for l in range(DEPTH):
        i = l // 2
        modls = [_modl_core(mod, l, b) for b in range(4)]
        gmix = _fm(inp["g_mix"][l])
        w_out = np.ascontiguousarray(inp["w_out"][l])
        if l % 2 == 0:
            lam_init = 0.8 - 0.6 * math.exp(-0.3 * l)
            w_in = np.ascontiguousarray(inp["w_in_ab"][i])
            gqk = np.ascontiguousarray(np.stack([_bc(inp["g_aq"][i]), _bc(inp["g_ak"][i])], 1))
            ims = [{"x_in": X[cid], "w_in": w_in, "modl": modls[b], "gmix": gmix, "gqk": gqk, "rope": ropes[h]}
                   for cid, (b, h) in enumerate(cores)]
            rA = _run(_prog("Ae", build_A_even), ims)
            pm = [_pool_mats(h) for h in range(2)]
            w_pool = np.ascontiguousarray(inp["w_pool"][i])
            sp = _fm(inp["s_pool"][i])
            lamr = _bc(inp["lam"][i].reshape(-1))
            gs = _bc(inp["g_subln"][i])
            ims = [{"x_in": X[cid], "q_in": rA[cid]["q_out"], "kvu_in": _kv_pair(rA[cid]["kvu_out"], rA[cid ^ 1]["kvu_out"]),
                    "poolA": pm[h], "w_pool": w_pool, "w_out": w_out, "modl": modls[b], "s_poolT": sp, "lam": lamr, "g_subln": gs}
                   for cid, (b, h) in enumerate(cores)]
            rB = _run(_prog("Be%d" % l, lambda: build_B1_even(lam_init)), ims)
        else:
            w_in = np.ascontiguousarray(inp["w_in_cd"][i][:, perm])
            w_qb = np.ascontiguousarray(inp["w_qb"][i])
            w_kvb = np.ascontiguousarray(inp["w_kvb"][i])
            gq, gk, gmq, gmk = _bc(inp["g_qa"][i]), _bc(inp["g_kva"][i]), _bc(inp["g_mq"][i]), _bc(inp["g_mk"][i])
            ims = [{"x_in": X[cid], "w_in": w_in, "w_qb": w_qb, "w_kvb": w_kvb, "modl": modls[b], "gmix": gmix, "g_qa": gq,
                    "g_kva": gk, "g_mq": gmq, "g_mk": gmk, "rope": ropes[h]} for cid, (b, h) in enumerate(cores)]
            rA = _run(_prog("Ao", build_A_odd), ims)
            dft = [_dft_tabs(h) for h in range(2)]
            w_f = np.ascontiguousarray(inp["w_fourier"][i])
            ims = []
            for cid, (b, h) in enumerate(cores):
                d = {"x_in": X[cid], "q_in": rA[cid]["q_out"], "kvu_in": _kv_pair(rA[cid]["kvu_out"], rA[cid ^ 1]["kvu_out"]),
                     "w_fourier": w_f, "w_out": w_out, "modl": modls[b]}
                d.update(dft[h])
                ims.append(d)
            rB = _run(_prog("Bo", build_B1_odd), ims)
        X = [np.ascontiguousarray(rB[cid]["x_out"]) for cid in range(NCORE)]
        mh = _moe_host(inp, l)
        gffn = _fm(inp["g_ffn"][l])
        ims = []
        for cid, (b, h) in enumerate(cores):
            d = {"x_in": X[cid], "modl": modls[b], "gffn": gffn}
            d.update(mh)
            ims.append(d)
        rC = _run(_prog("C", build_moe_test), ims)
        X = [np.ascontiguousarray(rC[cid]["x_out"]) for cid in range(NCORE)]
    out = np.zeros((4, 2048, D), np.float32)
    for cid, (b, h) in enumerate(cores):
        out[b, h * TL:(h + 1) * TL] = X[cid][:TL]
    return out


I32 = mybir.dt.int32
ARENA_BYTES = 150 * 1024


def emit_mods_fused(P):
    S, M = P.S, P.M
    nc = P.nc
    cT5 = P.din("cT5", [128, KC, 5])
    wm = P.din("wmod_sh", [DEPTH, D, 1536])
    bmT = P.din("bmodT", [128, DEPTH, 12])
    bsel_d = P.din("bsel", [128, 5])
    ct = M.sb("ct", [128, KC, 5], F32)
    st = M.sb("st", [128, KC, 5], F32)
    bt = M.sb("bt", [128, DEPTH, 12], F32)
    bsel = M.sb("bsel", [128, 5], F32)
    res = M.sb("res", [128, DEPTH * 12 * 5], F32)
    wbuf = [M.sb("wm%d" % i, [128, KC, 768], F32) for i in range(2)]
    b_ct, b_st, b_bt, b_res, b_bsel = Buf(), Buf(), Buf(), Buf(), Buf()
    b_w = [Buf(), Buf()]
    S.dma("sp", ct, cT5, writes=[b_ct])
    S.dma("sp", bt, bmT, writes=[b_bt])
    S.dma("sp", bsel, bsel_d, writes=[b_bsel])
    S.op("act", lambda e: e.activation(out=st, in_=ct, func=AF.Silu), reads=[b_ct], writes=[b_st])
    it = 0
    for l in range(DEPTH):
        for hh in range(2):
            wb, bw = wbuf[it % 2], b_w[it % 2]
            it += 1
            S.dma("sp" if hh == 0 else "act", wb,
                  wm[l, :, hh * 768:(hh + 1) * 768].rearrange("(k p) c -> p k c", p=128), writes=[bw])
            for q6 in range(6):
                q = hh * 6 + q6
                bank = P.rr("m", 4)
                ps = P.pb[bank][:, 0:5]
                mm_group(P, ps, [(wb[:, k, q6 * 128:(q6 + 1) * 128], st[:, k, :]) for k in range(KC)],
                         reads=[bw, b_st], writes=[P.pbb[bank]])
                o = (l * 12 + q) * 5
                S.op("dve", lambda e, ps=ps, o=o, l=l, q=q: e.tensor_scalar(
                    out=res[:, o:o + 5], in0=ps, scalar1=bt[:, l, q:q + 1], scalar2=None, op0=ALU.add),
                    reads=[P.pbb[bank], b_bt], writes=[b_res])
    b_own, b_all, b_m5, b_tmp = Buf(), Buf(), Buf(), Buf()
    m5 = M.sb("m5", [128, NCORE, 240], F32)
    for l in range(DEPTH):
        mod_own = nc.dram_tensor("mod_own%d" % l, [128, 60], F32).ap()
        mod_all = nc.dram_tensor("mod_all%d" % l, [NCORE * 128, 60], F32).ap()
        S.dma("sp", mod_own, res[:, l * 60:(l + 1) * 60], reads=[b_res], writes=[b_own])
        if getattr(P, "fake_ag", False):
            mod_all = P.din("fake_mod_all%d" % l, [NCORE * 128, 60])
        else:
            S.custom("pool", lambda e, mod_own=mod_own, mod_all=mod_all: e.collective_compute(
                "AllGather", ALU.bypass, replica_groups=[list(range(NCORE))], ins=[mod_own.opt()], outs=[mod_all.opt()]),
                reads=[b_own], writes=[b_all], inc=1)
        S.dma("sp", m5[:, :, l * 60:(l + 1) * 60], mod_all.rearrange("(r p) x -> p r x", p=128), reads=[b_all], writes=[b_m5])
    tmp = M.sb("mtmp", [128, NCORE * 48, 5], F32)
    sel = M.sb("msel", [128, NCORE * 48], F32)
    m5f = m5.rearrange("p r (x w) -> p (r x) w", w=5)
    S.op("dve", lambda e: e.tensor_tensor(out=tmp, in0=m5f, in1=bsel.unsqueeze(1).to_broadcast([128, NCORE * 48, 5]),
                                          op=ALU.mult), reads=[b_m5, b_bsel], writes=[b_tmp])
    S.op("dve", lambda e: e.tensor_reduce(out=sel, in_=tmp, axis=AX.X, op=ALU.add), reads=[b_tmp], writes=[b_tmp])
    sel4 = sel.rearrange("p (r l q) -> p r l q", r=NCORE, l=DEPTH)
    m54 = m5.rearrange("p r (l q w) -> p r l q w", l=DEPTH, w=5)
    for l in range(DEPTH):
        S.op("dve", lambda e, l=l: e.tensor_copy(out=P.modl_all[:, l, 0, :].rearrange("p (r q) -> p r q", q=12),
                                                in_=sel4[:, :, l, :]), reads=[b_tmp], writes=[P.b_modl_all])
        S.op("dve", lambda e, l=l: e.tensor_copy(out=P.modl_all[:, l, 1, :].rearrange("p (r q) -> p r q", q=12),
                                                in_=m54[:, :, l, :, 4]), reads=[b_m5], writes=[P.b_modl_all])


def emit_exchange(P, W):
    S, M = P.S, P.M
    nc = P.nc
    own = P.bind["kvu_out"]
    shr = nc.dram_tensor(P.tag + "kvu_shr", [2 * T, W], BF16, addr_space="Shared").ap()
    mine = nc.dram_tensor(P.tag + "kvu_mine", [2304, W], BF16).ap()
    bar_in = nc.dram_tensor(P.tag + "bar_in", [128, 16], F32).ap()
    bar_out = nc.dram_tensor(P.tag + "bar_out", [NCORE * 128, 16], F32).ap()
    b_shr, b_bar, b_mine, b_bi = Buf(), Buf(), Buf(), Buf()
    g = [M.sb("xg%d" % i, [128, W], BF16) for i in range(2)]
    bg = [Buf(), Buf()]
    S.dma("sp", bar_in, P.ones128[:, 0:16], reads=[P.b_ones], writes=[b_bi])
    for t in range(NT):
        gi = t % 2
        S.dma("sp", g[gi], own[t * 128:(t + 1) * 128, :], writes=[bg[gi]])
        S.custom("pool", lambda e, gi=gi, t=t: e.indirect_dma_start(
            out=shr, out_offset=bass.IndirectOffsetOnAxis(ap=P.widx[:, t:t + 1], axis=0), in_=g[gi], in_offset=None),
            reads=[bg[gi], P.b_kidx], writes=[b_shr], inc=16)
    S.custom("pool", lambda e: e.collective_compute("AllGather", ALU.bypass, replica_groups=[list(range(NCORE))],
                                                    ins=[bar_in.opt()], outs=[bar_out.opt()]),
             reads=[b_shr, b_bi], writes=[b_bar], inc=1)
    for kt in range(18):
        gi = kt % 2
        S.custom("pool", lambda e, gi=gi, kt=kt: e.indirect_dma_start(
            out=g[gi], out_offset=None, in_=shr,
            in_offset=bass.IndirectOffsetOnAxis(ap=P.kidx[:, kt:kt + 1], axis=0)),
            reads=[b_bar, P.b_kidx], writes=[bg[gi]], inc=16)
        S.dma("sp", mine[kt * 128:(kt + 1) * 128, :], g[gi], reads=[bg[gi]], writes=[b_mine])
    P.bind["kvu_in"] = mine
    P.bind["q_in"] = P.bind["q_out"]


def build_fused(n_layers=DEPTH, n_exp=NEXP, debug=False):
    P = Prog(fused=True, arena_bytes=ARENA_BYTES)
    P.debugA = False
    P.consts()
    S = P.S
    nc = P.nc
    x_in = P.din("x_in", [T, D])
    x_out = nc.dram_tensor("x_out", [TL, D], F32, kind="ExternalOutput").ap()
    load_x(P, x_in)
    kidx_d = P.din("kidx", [128, 18], I32)
    P.kidx = P.MP.sb("kidx_sb", [128, 18], I32)
    P.b_kidx = Buf()
    S.dma("sp", P.kidx[:], kidx_d, writes=[P.b_kidx])
    widx_d = P.din("widx", [128, NT], I32)
    P.widx = P.MP.sb("widx_sb", [128, NT], I32)
    S.dma("sp", P.widx[:], widx_d, writes=[P.b_kidx])
    P.modl_all = P.MP.sb("modl_all", [128, DEPTH, 2, 96], F32)
    P.b_modl_all = Buf()
    P.make_arena()
    emit_mods_fused(P)
    for l in range(n_layers):
        P.new_phase("L%d_" % l)
        P.bind = {"x_in": x_in}
        P.sb_bind = {"modl": (P.modl_all[:, l], P.b_modl_all)}
        if l % 2 == 0:
            emit_A_even(P)
            W = 3072
        else:
            emit_A_odd(P)
            W = 4352
        P.new_phase()
        if debug and l == 0:
            dbo0 = Buf()
            dqa = nc.dram_tensor("dbg_qA", [T, 1024], BF16, kind="ExternalOutput").ap()
            S.dma("sp", dqa, P.bind["q_out"], writes=[dbo0])
            dka = nc.dram_tensor("dbg_kvuA", [T, 3072], BF16, kind="ExternalOutput").ap()
            S.dma("sp", dka, P.bind["kvu_out"], writes=[dbo0])
            P.S.barrier()
        emit_exchange(P, W)
        P.new_phase()
        if l % 2 == 0:
            emit_B1_even(P, 0.8 - 0.6 * math.exp(-0.3 * l))
        else:
            emit_B1_odd(P)
        P.new_phase()
        if debug and l == 0:
            dbo = Buf()
            dx = nc.dram_tensor("dbg_xmid", [T, D], F32, kind="ExternalOutput").ap()
            for t in range(NT):
                S.dma("sp", dx[t * 128:(t + 1) * 128, :], P.X[:, t, :], reads=[P.bX[t]], writes=[dbo])
            dm = nc.dram_tensor("dbg_modl", [128, DEPTH * 2 * 96], F32, kind="ExternalOutput").ap()
            S.dma("sp", dm, P.modl_all[:].rearrange("p a b c -> p (a b c)"), reads=[P.b_modl_all], writes=[dbo])
            dk = nc.dram_tensor("dbg_kvu", [2304, 3072], BF16, kind="ExternalOutput").ap()
            S.dma("sp", dk, P.bind["kvu_in"], writes=[dbo])
            dq = nc.dram_tensor("dbg_q", [T, 1024], BF16, kind="ExternalOutput").ap()
            S.dma("sp", dq, P.bind["q_in"], writes=[dbo])
            P.S.barrier()
        emit_moe_test(P, n_exp)
    S.barrier()
    bo = Buf()
    for t in range(8):
        S.dma("sp" if t % 2 == 0 else "act", x_out[t * 128:(t + 1) * 128, :], P.X[:, t, :], reads=[P.bX[t]], writes=[bo])
    return P.finish([bo]), P


def _fused_inputs(inp):
    x, c, ctx, c_ctx = inp["x"], inp["c"], inp["ctx"], inp["c_ctx"]
    cores = [(cid // 2, cid % 2) for cid in range(NCORE)]
    C5 = np.concatenate([c, c_ctx[None]], 0).astype(np.float32)
    shared = {"cT5": np.ascontiguousarray(C5.reshape(5, KC, 128).transpose(2, 1, 0))}
    per_half = [dict(), dict()]
    ropes = [_rope_core(64, h) for h in range(2)]
    perm = np.concatenate([np.arange(0, 1024), np.arange(1088, 1600), np.arange(1024, 1088)])
    for l in range(DEPTH):
        i = l // 2
        tg = "L%d_" % l
        shared[tg + "gmix"] = _fm(inp["g_mix"][l])
        shared[tg + "gffn"] = _fm(inp["g_ffn"][l])
        shared[tg + "w_out"] = np.ascontiguousarray(inp["w_out"][l])
        for k, v in _moe_host(inp, l).items():
            shared[tg + k] = v
        for h in range(2):
            per_half[h][tg + "rope"] = ropes[h]
        if l % 2 == 0:
            shared[tg + "w_in"] = np.ascontiguousarray(inp["w_in_ab"][i])
            shared[tg + "gqk"] = np.ascontiguousarray(np.stack([_bc(inp["g_aq"][i]), _bc(inp["g_ak"][i])], 1))
            shared[tg + "w_pool"] = np.ascontiguousarray(inp["w_pool"][i])
            shared[tg + "s_poolT"] = _fm(inp["s_pool"][i])
            shared[tg + "lam"] = _bc(inp["lam"][i].reshape(-1))
            shared[tg + "g_subln"] = _bc(inp["g_subln"][i])
            for h in range(2):
                per_half[h][tg + "poolA"] = _pool_mats(h)
        else:
            shared[tg + "w_in"] = np.ascontiguousarray(inp["w_in_cd"][i][:, perm])
            shared[tg + "w_qb"] = np.ascontiguousarray(inp["w_qb"][i])
            shared[tg + "w_kvb"] = np.ascontiguousarray(inp["w_kvb"][i])
            shared[tg + "g_qa"] = _bc(inp["g_qa"][i])
            shared[tg + "g_kva"] = _bc(inp["g_kva"][i])
            shared[tg + "g_mq"] = _bc(inp["g_mq"][i])
            shared[tg + "g_mk"] = _bc(inp["g_mk"][i])
            shared[tg + "w_fourier"] = np.ascontiguousarray(inp["w_fourier"][i])
            for h in range(2):
                for k, v in _dft_tabs(h).items():
                    per_half[h][tg + k] = v
    ims = []
    for cid, (b, h) in enumerate(cores):
        d = dict(shared)
        d.update(per_half[h])
        d["x_in"] = np.ascontiguousarray(np.concatenate([x[b, h * TL:(h + 1) * TL], ctx[b, h * TCX:(h + 1) * TCX]], 0))
        ids = np.concatenate([h * T + np.arange(TL), (1 - h) * T + np.arange(TL), h * T + TL + np.arange(TCX),
                              (1 - h) * T + TL + np.arange(TCX)]).astype(np.int32)
        d["kidx"] = np.ascontiguousarray(ids.reshape(18, 128).T)
        d["widx"] = np.ascontiguousarray((h * T + np.arange(T)).astype(np.int32).reshape(NT, 128).T)
        sl = slice(cid * 1536, (cid + 1) * 1536)
        d["wmod_sh"] = np.ascontiguousarray(inp["w_mod"][:, :, sl])
        d["bmodT"] = np.ascontiguousarray(inp["b_mod"][:, sl].reshape(DEPTH, 12, 128).transpose(2, 0, 1))
        bs = np.zeros((128, 5), np.float32)
        bs[:, b] = 1.0
        d["bsel"] = bs
        ims.append(d)
    return ims


def kernel_fused(**inp):
    inp = {k: np.asarray(v) for k, v in inp.items()}
    if "F" not in _PROGS:
        _PROGS["F"] = build_fused()[0]
    ims = _fused_inputs(inp)
    res = run_bass_kernel_spmd(_PROGS["F"], ims, core_ids=list(range(NCORE))).results
    out = np.zeros((4, 2048, D), np.float32)
    for cid in range(NCORE):
        b, h = cid // 2, cid % 2
        out[b, h * TL:(h + 1) * TL] = res[cid]["x_out"]
    return out


def kernel(**inp):
    return kernel_unfused(**inp)
```

```python
import math
import numpy as np
import ml_dtypes
import concourse.bass as bass
import concourse.mybir as mybir
from concourse.bass_utils import run_bass_kernel_spmd

F32 = mybir.dt.float32
BF16 = mybir.dt.bfloat16
ALU = mybir.AluOpType
AF = mybir.ActivationFunctionType
AX = mybir.AxisListType
NPBF = ml_dtypes.bfloat16

D = 2048
KC = 16
DEPTH = 4
NCORE = 8
TL = 1024
TCX = 128
T = TL + TCX
NT = T // 128
EPS = 1e-6
NEXP = 32
DEXP = 768
NTILES_N = [(0, 512), (512, 512), (1024, 128)]


class Buf:
    __slots__ = ("w", "r")

    def __init__(self):
        self.w = None
        self.r = {}


class Sched:
    ENGS = ("pe", "act", "dve", "pool", "sp")

    def __init__(self, nc, n_dma_sems=8):
        self.nc = nc
        self.ops = {e: [] for e in self.ENGS}
        self.sems = {}
        self.cnt = {e: 0 for e in self.ENGS}
        self.seen = {e: {} for e in self.ENGS}
        self._ctx = []
        for e in self.ENGS:
            self.sems[e] = self._sem("s_" + e)
        self.dq = {}
        for q in ("sp", "act", "pool"):
            lst = []
            for i in range(n_dma_sems):
                k = "d_%s_%d" % (q, i)
                self.sems[k] = self._sem(k)
                lst.append(k)
            self.dq[q] = {"keys": lst, "n": 0}

    def _sem(self, name):
        cm = self.nc.semaphore(name)
        h = cm.__enter__()
        self._ctx.append(cm)
        return h

    def _need(self, eng, reads, writes):
        need = {}

        def add(ev):
            if ev is None:
                return
            k, v = ev
            if need.get(k, 0) < v:
                need[k] = v
        for b in reads:
            add(b.w)
        for b in writes:
            add(b.w)
            for k, v in b.r.items():
                add((k, v))
        out = []
        for k, v in need.items():
            if k == "pe" and eng == "pe":
                continue
            if self.seen[eng].get(k, 0) < v:
                self.seen[eng][k] = v
                out.append((k, v))
        return out

    def _emit_waits(self, eng, waits):
        for k, v in waits:
            h = self.sems[k]
            self.ops[eng].append(lambda e, h=h, v=v: e.wait_ge(h, v))

    def _mark(self, ev, reads, writes):
        k, v = ev
        for b in reads:
            if b.r.get(k, 0) < v:
                b.r[k] = v
        for b in writes:
            b.w = ev
            b.r = {}

    def op(self, eng, fn, reads=(), writes=()):
        waits = self._need(eng, reads, writes)
        self._emit_waits(eng, waits)
        self.cnt[eng] += 1
        v = self.cnt[eng]
        h = self.sems[eng]
        self.ops[eng].append(lambda e, fn=fn, h=h: fn(e).then_inc(h, 1))
        self._mark((eng, v), reads, writes)

    def _slot(self, q, waits):
        d = self.dq[q]
        i = d["n"]
        d["n"] += 1
        n = len(d["keys"])
        k = d["keys"][i % n]
        val = 16 * (i // n + 1)
        if i >= n:
            prev = 16 * (i // n)
            if self.seen[q].get(k, 0) < prev:
                self.seen[q][k] = prev
                waits.append((k, prev))
        return k, val

    def dma(self, q, out, in_, reads=(), writes=(), **kw):
        waits = self._need(q, reads, writes)
        k, val = self._slot(q, waits)
        self._emit_waits(q, waits)
        h = self.sems[k]
        self.ops[q].append(lambda e, out=out, in_=in_, h=h, kw=kw: e.dma_start(out=out, in_=in_, **kw).then_inc(h, 16))
        self._mark((k, val), reads, writes)

    def custom(self, q, fn, reads=(), writes=(), inc=16):
        waits = self._need(q, reads, writes)
        k, val = self._slot(q, waits)
        self._emit_waits(q, waits)
        h = self.sems[k]

        def run(e, fn=fn, h=h, inc=inc):
            fn(e).then_inc(h, inc)
            if inc < 16:
                e.sem_inc(h, 16 - inc)
        self.ops[q].append(run)
        self._mark((k, val), reads, writes)

    def barrier(self):
        evs = [(e, self.cnt[e]) for e in self.ENGS if self.cnt[e] > 0]
        for q, d in self.dq.items():
            n = len(d["keys"])
            for j, k in enumerate(d["keys"]):
                cntk = (d["n"] - j + n - 1) // n if d["n"] > j else 0
                if cntk > 0:
                    evs.append((k, 16 * cntk))
        for eng in self.ENGS:
            waits = []
            for k, v in evs:
                if k == "pe" and eng == "pe":
                    continue
                if self.seen[eng].get(k, 0) < v:
                    self.seen[eng][k] = v
                    waits.append((k, v))
            self._emit_waits(eng, waits)

    def wait_all(self, eng, bufs):
        self._emit_waits(eng, self._need(eng, bufs, bufs))

    def emit(self):
        ops = self.ops
        with self.nc.Block() as block:
            @block.tensor
            def _(e):
                for f in ops["pe"]:
                    f(e)

            @block.scalar
            def _(e):
                for f in ops["act"]:
                    f(e)

            @block.vector
            def _(e):
                for f in ops["dve"]:
                    f(e)

            @block.gpsimd
            def _(e):
                for f in ops["pool"]:
                    f(e)

            @block.sync
            def _(e):
                for f in ops["sp"]:
                    f(e)

    def close(self):
        for cm in reversed(self._ctx):
            cm.__exit__(None, None, None)
        self._ctx = []


class Mem:
    def __init__(self, nc):
        self.nc = nc
        self._ctx = []

    def sb(self, name, shape, dtype):
        cm = self.nc.sbuf_tensor(name, list(shape), dtype)
        t = cm.__enter__()
        self._ctx.append(cm)
        return t

    def ps(self, name, shape, dtype):
        cm = self.nc.psum_tensor(name, list(shape), dtype)
        t = cm.__enter__()
        self._ctx.append(cm)
        return t

    def close(self):
        for cm in reversed(self._ctx):
            cm.__exit__(None, None, None)
        self._ctx = []


class ArenaMem:
    def __init__(self, arena, nbytes):
        self.arena = arena
        self.nbytes = nbytes
        self.off = 0

    def reset(self):
        self.off = 0

    def sb(self, name, shape, dtype):
        esz = 4 if dtype == F32 or str(dtype).endswith("int32") else 2
        n = 1
        for d in shape[1:]:
            n *= d
        nb = (n * esz + 63) // 64 * 64
        assert self.off + nb <= self.nbytes, ("arena overflow", name, self.off, nb, self.nbytes)
        v = self.arena[0:shape[0], self.off // 2:(self.off + n * esz) // 2]
        self.off += nb
        if esz == 4:
            v = v.bitcast(dtype)
        if len(shape) == 3:
            v = v.rearrange("p (a b) -> p a b", a=shape[1])
        elif len(shape) == 4:
            v = v.rearrange("p (a b c) -> p a b c", a=shape[1], b=shape[2])
        return v


class Prog:
    def __init__(self, fused=False, arena_bytes=150 * 1024):
        self.nc = bass.Bass("TRN2", target_bir_lowering=False)
        self.S = Sched(self.nc)
        self.MP = Mem(self.nc)
        self.fused = fused
        self.tag = ""
        self.bind = {}
        self.sb_bind = {}
        self.phase_id = 0
        self.M = self.MP
        self.pb = [self.MP.ps("pb%d" % i, [128, 512], F32) for i in range(8)]
        self.pbb = [Buf() for _ in range(8)]
        self.ins = {}
        self.outs = {}
        self._rr = {}

    def din(self, name, shape, dtype=F32):
        if name in self.bind:
            return self.bind[name]
        full = self.tag + name
        if full in self.ins:
            return self.ins[full]
        t = self.nc.dram_tensor(full, list(shape), dtype, kind="ExternalInput").ap()
        self.ins[full] = t
        return t

    def dout(self, name, shape, dtype=F32):
        if name in self.bind:
            return self.bind[name]
        if self.fused:
            t = self.nc.dram_tensor(self.tag + name, list(shape), dtype).ap()
            self.bind[name] = t
            return t
        t = self.nc.dram_tensor(name, list(shape), dtype, kind="ExternalOutput").ap()
        self.outs[name] = t
        return t

    def make_arena(self):
        nbytes = (int(self.nc.sbuf_bytes_remaining) - 512) // 64 * 64
        self.arena = self.MP.sb("arena", [128, nbytes // 2], BF16)
        self.M = ArenaMem(self.arena, nbytes)

    def new_phase(self, tag=None):
        self.S.barrier()
        self.phase_id += 1
        if self.fused:
            self.M.reset()
        if tag is not None:
            self.tag = tag

    def rr(self, key, n):
        v = self._rr.get(key, 0)
        self._rr[key] = v + 1
        return v % n

    def consts(self):
        S, M = self.S, self.MP
        self.ones128 = M.sb("ones128", [128, 128], F32)
        self.b_ones = Buf()
        S.op("pool", lambda e: e.memset(self.ones128[:], 1.0), writes=[self.b_ones])
        self.identb = M.sb("identb", [128, 128], BF16)
        self.b_identb = Buf()
        self.identf = M.sb("identf", [128, 128], F32)
        self.b_identf = Buf()
        self.epsc = M.sb("epsc", [128, 1], F32)
        self.b_eps = Buf()
        S.op("pool", lambda e: e.memset(self.identf[:], 1.0), writes=[self.b_identf])
        S.op("pool", lambda e: e.affine_select(out=self.identf[:], in_=self.identf[:], pattern=[[-1, 128]],
                                                compare_op=ALU.is_equal, fill=0.0, base=0, channel_multiplier=1),
             reads=[self.b_identf], writes=[self.b_identf])
        S.op("pool", lambda e: e.tensor_copy(out=self.identb[:], in_=self.identf[:]), reads=[self.b_identf],
             writes=[self.b_identb])
        S.op("pool", lambda e: e.memset(self.epsc[:], EPS), writes=[self.b_eps])

    def finish(self, out_bufs):
        self.S.wait_all("sp", out_bufs)
        self.S.emit()
        self.MP.close()
        self.S.close()
        return self.nc


def mm_group(P, out_ap, pairs, reads, writes):
    n = len(pairs)

    def fn(e):
        ins = None
        for i, (l, r) in enumerate(pairs):
            ins = e.matmul(out_ap, lhsT=l, rhs=r, start=(i == 0), stop=(i == n - 1))
        return ins
    P.S.op("pe", fn, reads=reads, writes=writes)


def build_M():
    P = Prog()
    S, M = P.S, P.M
    cT5 = P.din("cT5", [128, KC, 5])
    wm = P.din("wmod_sh", [DEPTH, D, 1536])
    bmT = P.din("bmodT", [128, DEPTH, 12])
    out = P.dout("mod_sh", [128, DEPTH * 12 * 5])
    ct = M.sb("ct", [128, KC, 5], F32)
    st = M.sb("st", [128, KC, 5], F32)
    bt = M.sb("bt", [128, DEPTH, 12], F32)
    res = M.sb("res", [128, DEPTH * 12 * 5], F32)
    wbuf = [M.sb("wm%d" % i, [128, KC, 768], F32) for i in range(2)]
    b_ct, b_st, b_bt, b_res = Buf(), Buf(), Buf(), Buf()
    b_w = [Buf(), Buf()]
    S.dma("sp", ct[:], cT5, writes=[b_ct])
    S.dma("sp", bt[:], bmT, writes=[b_bt])
    S.op("act", lambda e: e.activation(out=st[:], in_=ct[:], func=AF.Silu), reads=[b_ct], writes=[b_st])
    it = 0
    for l in range(DEPTH):
        for hh in range(2):
            wb, bw = wbuf[it % 2], b_w[it % 2]
            it += 1
            S.dma("sp" if hh == 0 else "act", wb[:],
                  wm[l, :, hh * 768:(hh + 1) * 768].rearrange("(k p) c -> p k c", p=128), writes=[bw])
            for q6 in range(6):
                q = hh * 6 + q6
                bank = P.rr("m", 4)
                ps = P.pb[bank][:, 0:5]
                mm_group(P, ps, [(wb[:, k, q6 * 128:(q6 + 1) * 128], st[:, k, :]) for k in range(KC)],
                         reads=[bw, b_st], writes=[P.pbb[bank]])
                o = (l * 12 + q) * 5
                S.op("dve", lambda e, ps=ps, o=o, l=l, q=q: e.tensor_scalar(
                    out=res[:, o:o + 5], in0=ps, scalar1=bt[:, l, q:q + 1], scalar2=None, op0=ALU.add),
                    reads=[P.pbb[bank], b_bt], writes=[b_res])
    bo = Buf()
    S.dma("sp", out, res[:], reads=[b_res], writes=[bo])
    return P.finish([bo]), P


def stream_x(P, x_in):
    if P.fused:
        return
    P.Xs = P.M.sb("Xs", [128, 2, D], F32)
    P.bXs = [Buf(), Buf()]
    P.x_src = x_in


def xtile(P, t):
    if hasattr(P, "x_src"):
        i = t % 2
        P.S.dma("sp" if i == 0 else "act", P.Xs[:, i, :], P.x_src[t * 128:(t + 1) * 128, :], writes=[P.bXs[i]])
        return P.Xs[:, i, :], P.bXs[i]
    return P.X[:, t, :], P.bX[t]


def load_x(P, x_in):
    if hasattr(P, "X"):
        return
    P.X = P.MP.sb("X", [128, NT, D], F32)
    P.bX = [Buf() for _ in range(NT)]
    for t in range(NT):
        P.S.dma("sp" if t % 2 == 0 else "act", P.X[:, t, :], x_in[t * 128:(t + 1) * 128, :], writes=[P.bX[t]])


def prenorm(P, A, B, bAB, name):
    S, M = P.S, P.M
    if getattr(P, "_pn_phase", None) != P.phase_id:
        P._pn_phase = P.phase_id
        P.actT = M.sb("actT", [128, KC, T], BF16)
        P.bact = [Buf() for _ in range(NT)]
        P.ss = M.sb("ss", [128, NT], F32)
        P.bss = [Buf() for _ in range(NT)]
        P.xn = [M.sb("xn%d" % i, [128, D], BF16) for i in range(1)]
        P.bxn = [Buf()]
    for t in range(NT):
        typ = 0 if t < 8 else 1
        ss = P.ss[:, t:t + 1]
        xt_, bxt_ = xtile(P, t)
        S.op("act", lambda e, xt_=xt_, ss=ss: e.activation(out=P.xn[0][:], in_=xt_, func=AF.Square, accum_out=ss),
             reads=[bxt_], writes=[P.bxn[0], P.bss[t]])
        S.op("act", lambda e, ss=ss: e.activation(out=ss, in_=ss, func=AF.Sqrt, bias=P.epsc[:, 0:1], scale=1.0 / D),
             reads=[P.bss[t], P.b_eps], writes=[P.bss[t]])
        S.op("dve", lambda e, ss=ss: e.reciprocal(out=ss, in_=ss), reads=[P.bss[t]], writes=[P.bss[t]])
        xi = 0
        xn, bxn = P.xn[xi], P.bxn[xi]
        S.op("dve", lambda e, xt_=xt_, xn=xn, ss=ss: e.tensor_scalar(out=xn[:], in0=xt_, scalar1=ss, scalar2=None,
                                                                op0=ALU.mult),
             reads=[bxt_, P.bss[t]], writes=[bxn])
        for g in range(2):
            bank = P.rr("tp", 2)
            pv = P.pb[bank][:, :].bitcast(BF16)

            def tr(e, g=g, pv=pv, xn=xn):
                ins = None
                for j in range(8):
                    k = g * 8 + j
                    ins = e.transpose(pv[:, j * 128:(j + 1) * 128], xn[:, k * 128:(k + 1) * 128], P.identb[:])
                return ins
            S.op("pe", tr, reads=[bxn, P.b_identb], writes=[P.pbb[bank]])
            for j in range(8):
                k = g * 8 + j
                eng = "act" if j % 2 == 0 else "dve"
                dst = P.actT[:, k, t * 128:(t + 1) * 128]
                src = pv[:, j * 128:(j + 1) * 128]
                if eng == "act":
                    S.op("act", lambda e, dst=dst, src=src, k=k, typ=typ: e.activation(
                        out=dst, in_=src, func=AF.Identity, bias=B[:, typ, k:k + 1], scale=A[:, typ, k:k + 1]),
                        reads=[P.pbb[bank], bAB], writes=[P.bact[t]])
                else:
                    S.op("dve", lambda e, dst=dst, src=src, k=k, typ=typ: e.tensor_scalar(
                        out=dst, in0=src, scalar1=A[:, typ, k:k + 1], scalar2=B[:, typ, k:k + 1],
                        op0=ALU.mult, op1=ALU.add),
                        reads=[P.pbb[bank], bAB], writes=[P.bact[t]])


def mod_vectors(P, modl, g_in, part_scale, part_shift, name):
    S, M = P.S, P.M
    A = M.sb("A_" + name, [128, 2, KC], F32)
    B = M.sb("B_" + name, [128, 2, KC], F32)
    bAB = Buf()
    for typ in range(2):
        S.op("dve", lambda e, typ=typ: e.tensor_scalar(
            out=A[:, typ, :], in0=modl[:, typ, part_scale * 16:(part_scale + 1) * 16], scalar1=1.0, scalar2=None,
            op0=ALU.add), reads=[P.b_modl], writes=[bAB])
        S.op("dve", lambda e, typ=typ: e.tensor_tensor(out=A[:, typ, :], in0=A[:, typ, :], in1=g_in, op=ALU.mult),
             reads=[bAB, P.b_g], writes=[bAB])
        S.op("dve", lambda e, typ=typ: e.tensor_copy(
            out=B[:, typ, :], in_=modl[:, typ, part_shift * 16:(part_shift + 1) * 16]), reads=[P.b_modl], writes=[bAB])
    return A, B, bAB


def bcast_rows(P, vec, bvec, name):
    S, M = P.S, P.M
    if getattr(P, "_bc_phase", None) != P.phase_id:
        P._bc_phase = P.phase_id
        P.diag = [M.sb("diag%d" % i, [128, 512], F32) for i in range(2)]
        P.bdiag = [Buf(), Buf()]
    out = M.sb("bc_" + name, [128, 2, D], BF16)
    bout = Buf()
    for typ in range(2):
        for c4 in range(4):
            di = P.rr("diag", 2)
            dg, bdg = P.diag[di], P.bdiag[di]
            for j in range(4):
                k = c4 * 4 + j
                S.op("dve", lambda e, dg=dg, j=j, k=k, typ=typ: e.tensor_scalar(
                    out=dg[:, j * 128:(j + 1) * 128], in0=P.identf[:], scalar1=vec[:, typ, k:k + 1], scalar2=None,
                    op0=ALU.mult), reads=[P.b_identf, bvec], writes=[bdg])
            bank = P.rr("bc", 2) + 2
            mm_group(P, P.pb[bank][:, :], [(P.ones128[:], dg[:])], reads=[bdg, P.b_ones], writes=[P.pbb[bank]])
            S.op("act", lambda e, bank=bank, typ=typ, c4=c4: e.copy(out=out[:, typ, c4 * 512:(c4 + 1) * 512],
                                                                   in_=P.pb[bank][:, :]),
                 reads=[P.pbb[bank]], writes=[bout])
    return out, bout


def load_small(P, name, shape, dtype=F32, q="sp"):
    if name in P.sb_bind:
        return P.sb_bind[name]
    ap = P.din(name, shape, dtype)
    t = P.M.sb("s_" + name, shape, dtype)
    b = Buf()
    P.S.dma(q, t[:], ap, writes=[b])
    return t, b


def stream_slabs(P, w_ap, ncols, cgs, name):
    raise NotImplementedError


def emit_A_even(P):
    S, M = P.S, P.M
    x_in = P.din("x_in", [T, D])
    w_in = P.din("w_in", [D, 4096])
    q_out = P.dout("q_out", [T, 1024], BF16)
    kvu_out = P.dout("kvu_out", [T, 3072], BF16)
    modl, P.b_modl = load_small(P, "modl", [128, 2, 96])
    gmix, P.b_g = load_small(P, "gmix", [128, KC])
    gqk, b_gqk = load_small(P, "gqk", [128, 2, 64])
    rope, b_rope = load_small(P, "rope", [128, NT, 2, 32])
    stream_x(P, x_in)
    A, B, bAB = mod_vectors(P, modl, gmix[:], 1, 0, "mix")
    prenorm(P, A, B, bAB, "mix")
    if getattr(P, "debugA", False):
        nc = P.nc
        dbo = Buf()
        d1 = nc.dram_tensor("dbgA_actT", [128, KC * T], BF16, kind="ExternalOutput").ap()
        S.dma("sp", d1, P.actT.rearrange("p k t -> p (k t)"), reads=P.bact, writes=[dbo])
        d2 = nc.dram_tensor("dbgA_AB", [128, 64], F32, kind="ExternalOutput").ap()
        S.dma("sp", d2[:, 0:32], A.rearrange("p a b -> p (a b)"), reads=[bAB], writes=[dbo])
        S.dma("sp", d2[:, 32:64], B.rearrange("p a b -> p (a b)"), reads=[bAB], writes=[dbo])
        d3 = nc.dram_tensor("dbgA_ss", [128, NT], F32, kind="ExternalOutput").ap()
        S.dma("sp", d3, P.ss, reads=P.bss, writes=[dbo])
        d4 = nc.dram_tensor("dbgA_x", [128, NT * D], F32, kind="ExternalOutput").ap()
        S.dma("sp", d4, P.X[:].rearrange("p t d -> p (t d)"), reads=P.bX, writes=[dbo])
        S.barrier()
    slabs = [M.sb("slab%d" % i, [128, KC, 512], BF16) for i in range(2)]
    bslab = [Buf(), Buf()]
    stg = [M.sb("stg%d" % i, [128, 512], BF16) for i in range(3)]
    bstg = [Buf() for _ in range(3)]
    sqb = [M.sb("sqb%d" % i, [128, 512], F32) for i in range(2)]
    bsq = [Buf(), Buf()]
    qn = [M.sb("qn%d" % i, [128, 512], F32) for i in range(2)]
    bqn = [Buf(), Buf()]
    ra = [M.sb("ra%d" % i, [128, 8, 32], F32) for i in range(4)]
    bra = [Buf() for _ in range(4)]
    rs = M.sb("rs", [128, 2, 8], F32)
    brs = [Buf(), Buf()]
    bo = Buf()
    for cg in range(8):
        sl, bsl = slabs[cg % 2], bslab[cg % 2]
        S.dma("pool", sl[:], w_in[:, cg * 512:(cg + 1) * 512].rearrange("(k p) c -> p k c", p=128), writes=[bsl])
        for t in range(NT):
            bank = 4 + P.rr("ip", 4)
            ps = P.pb[bank]
            mm_group(P, ps[:, :], [(P.actT[:, k, t * 128:(t + 1) * 128], sl[:, k, :]) for k in range(KC)],
                     reads=[P.bact[t], bsl], writes=[P.pbb[bank]])
            si = P.rr("stg", 3)
            sg, bsg = stg[si], bstg[si]
            rows = slice(t * 128, (t + 1) * 128)
            if cg < 4:
                which = 0 if cg < 2 else 1
                i2 = P.rr("sq", 2)
                sq, bq, q_, bq_, rsv, brsv = sqb[i2], bsq[i2], qn[i2], bqn[i2], rs[:, i2, :], brs[i2]
                S.op("act", lambda e, sq=sq, ps=ps: e.activation(out=sq[:], in_=ps[:, :], func=AF.Square),
                     reads=[P.pbb[bank]], writes=[bq])
                S.op("dve", lambda e, sq=sq, rsv=rsv: e.tensor_reduce(
                    out=rsv, in_=sq[:].rearrange("p (g d) -> p g d", d=64), axis=AX.X, op=ALU.add),
                    reads=[bq], writes=[brsv])
                S.op("act", lambda e, rsv=rsv: e.activation(out=rsv, in_=rsv, func=AF.Sqrt, bias=P.epsc[:, 0:1],
                                                         scale=1.0 / 64), reads=[brsv, P.b_eps], writes=[brsv])
                S.op("dve", lambda e, rsv=rsv: e.reciprocal(out=rsv, in_=rsv), reads=[brsv], writes=[brsv])
                S.op("dve", lambda e, q_=q_, ps=ps, rsv=rsv: e.tensor_tensor(
                    out=q_[:].rearrange("p (g d) -> p g d", d=64), in0=ps[:, :].rearrange("p (g d) -> p g d", d=64),
                    in1=rsv.unsqueeze(2).to_broadcast([128, 8, 64]), op=ALU.mult),
                    reads=[P.pbb[bank], brsv], writes=[bq_])
                S.op("dve", lambda e, q_=q_, which=which: e.tensor_tensor(
                    out=q_[:].rearrange("p (g d) -> p g d", d=64), in0=q_[:].rearrange("p (g d) -> p g d", d=64),
                    in1=gqk[:, which, :].unsqueeze(1).to_broadcast([128, 8, 64]), op=ALU.mult),
                    reads=[bq_, b_gqk], writes=[bq_])
                q4 = q_[:].rearrange("p (g h d) -> p g h d", h=2, d=32)
                x1, x2 = q4[:, :, 0, :], q4[:, :, 1, :]
                cosb = rope[:, t, 0, :].unsqueeze(1).to_broadcast([128, 8, 32])
                sinb = rope[:, t, 1, :].unsqueeze(1).to_broadcast([128, 8, 32])
                r4 = [P.rr("ra", 4) for _ in range(4)]
                tmp = [(ra[i], bra[i]) for i in r4]
                S.op("dve", lambda e, o=tmp[0][0], x1=x1, cosb=cosb: e.tensor_tensor(out=o[:], in0=x1, in1=cosb, op=ALU.mult),
                     reads=[bq_, b_rope], writes=[tmp[0][1]])
                S.op("dve", lambda e, o=tmp[1][0], x2=x2, sinb=sinb: e.tensor_tensor(out=o[:], in0=x2, in1=sinb, op=ALU.mult),
                     reads=[bq_, b_rope], writes=[tmp[1][1]])
                S.op("dve", lambda e, o=tmp[2][0], x2=x2, cosb=cosb: e.tensor_tensor(out=o[:], in0=x2, in1=cosb, op=ALU.mult),
                     reads=[bq_, b_rope], writes=[tmp[2][1]])
                S.op("dve", lambda e, o=tmp[3][0], x1=x1, sinb=sinb: e.tensor_tensor(out=o[:], in0=x1, in1=sinb, op=ALU.mult),
                     reads=[bq_, b_rope], writes=[tmp[3][1]])
                s4 = sg[:].rearrange("p (g h d) -> p g h d", h=2, d=32)
                S.op("dve", lambda e, s4=s4, a=tmp[0][0], b=tmp[1][0]: e.tensor_tensor(
                    out=s4[:, :, 0, :], in0=a[:], in1=b[:], op=ALU.subtract),
                    reads=[tmp[0][1], tmp[1][1]], writes=[bsg])
                S.op("dve", lambda e, s4=s4, a=tmp[2][0], b=tmp[3][0]: e.tensor_tensor(
                    out=s4[:, :, 1, :], in0=a[:], in1=b[:], op=ALU.add),
                    reads=[tmp[2][1], tmp[3][1]], writes=[bsg])
                if cg < 2:
                    dst = q_out[rows, cg * 512:(cg + 1) * 512]
                else:
                    dst = kvu_out[rows, (cg - 2) * 512:(cg - 1) * 512]
            else:
                S.op("act", lambda e, sg=sg, ps=ps: e.copy(out=sg[:], in_=ps[:, :]), reads=[P.pbb[bank]], writes=[bsg])
                dst = kvu_out[rows, 1024 + (cg - 4) * 512:1024 + (cg - 3) * 512]
            S.dma("sp", dst, sg[:], reads=[bsg], writes=[bo])
    return bo


def build_A_even():
    P = Prog()
    P.consts()
    bo = emit_A_even(P)
    return P.finish([bo]), P

def out_proj(P, w_out, gate_bc, b_gate):
    S, M = P.S, P.M
    CW = 256
    slabs = [M.sb("oslab%d" % i, [128, KC, CW], BF16) for i in range(2)]
    bslab = [Buf(), Buf()]
    tmp = [M.sb("otmp%d" % i, [128, CW], F32) for i in range(2)]
    btmp = [Buf(), Buf()]
    for cg in range(D // CW):
        sl, bsl = slabs[cg % 2], bslab[cg % 2]
        S.dma("pool", sl[:], w_out[:, cg * CW:(cg + 1) * CW].rearrange("(k p) c -> p k c", p=128), writes=[bsl])
        for t in range(NT):
            typ = 0 if t < 8 else 1
            bank = 6 + P.rr("op", 2)
            ps = P.pb[bank][:, 0:CW]
            mm_group(P, ps, [(P.actT[:, k, t * 128:(t + 1) * 128], sl[:, k, :]) for k in range(KC)],
                     reads=[P.bact[t], bsl], writes=[P.pbb[bank]])
            ti = P.rr("otmp", 2)
            tm, btm = tmp[ti], btmp[ti]
            S.op("dve", lambda e, tm=tm, ps=ps, typ=typ, cg=cg: e.tensor_tensor(
                out=tm[:], in0=ps, in1=gate_bc[:, typ, cg * CW:(cg + 1) * CW], op=ALU.mult),
                reads=[P.pbb[bank], b_gate], writes=[btm])
            xs = P.X[:, t, cg * CW:(cg + 1) * CW]
            S.op("dve", lambda e, xs=xs, tm=tm: e.tensor_tensor(out=xs, in0=xs, in1=tm[:], op=ALU.add),
                 reads=[btm, P.bX[t]], writes=[P.bX[t]])


def moe(P, w_router, b_router_bc, w_gu_r, b_guT, w_down, b_down, gate_bc, b_gate, n_exp=NEXP):
    S, M = P.S, P.M
    wr = M.sb("wr", [128, KC, NEXP], BF16)
    bwr = Buf()
    S.dma("pool", wr[:], w_router, writes=[bwr])
    brt, bbrt = b_router_bc
    G = M.sb("G", [128, NT, NEXP], F32)
    bG = [Buf() for _ in range(NT)]
    lg = M.sb("lg", [128, NT, NEXP], F32)
    mx = M.sb("mx", [128, NT, 8], F32)
    den = M.sb("den", [128, NT, 2], F32)
    for t in range(NT):
        bank = P.rr("rt", 2)
        ps = P.pb[bank][:, 0:NEXP]
        mm_group(P, ps, [(P.actT[:, k, t * 128:(t + 1) * 128], wr[:, k, :]) for k in range(KC)],
                 reads=[P.bact[t], bwr], writes=[P.pbb[bank]])
        l_, m_, g_, d_ = lg[:, t, :], mx[:, t, :], G[:, t, :], den[:, t, :]
        S.op("dve", lambda e, l_=l_, ps=ps: e.tensor_tensor(out=l_, in0=ps, in1=brt[:], op=ALU.add),
             reads=[P.pbb[bank], bbrt], writes=[bG[t]])
        S.op("dve", lambda e, l_=l_, m_=m_: e.max(out=m_, in_=l_), reads=[bG[t]], writes=[bG[t]])
        S.op("dve", lambda e, m_=m_, d_=d_: e.tensor_scalar(out=d_[:, 0:1], in0=m_[:, 0:1], scalar1=-1.0, scalar2=None,
                                                         op0=ALU.mult), reads=[bG[t]], writes=[bG[t]])
        S.op("act", lambda e, g_=g_, l_=l_, d_=d_: e.activation(out=g_, in_=l_, func=AF.Exp, bias=d_[:, 0:1], scale=1.0),
             reads=[bG[t]], writes=[bG[t]])
        S.op("dve", lambda e, l_=l_, m_=m_: e.tensor_scalar(out=l_, in0=l_, scalar1=m_[:, 3:4], scalar2=None, op0=ALU.is_ge),
             reads=[bG[t]], writes=[bG[t]])
        S.op("dve", lambda e, g_=g_, l_=l_: e.tensor_tensor(out=g_, in0=g_, in1=l_, op=ALU.mult), reads=[bG[t]], writes=[bG[t]])
        S.op("dve", lambda e, g_=g_, d_=d_: e.tensor_reduce(out=d_[:, 1:2], in_=g_, axis=AX.X, op=ALU.add),
             reads=[bG[t]], writes=[bG[t]])
        S.op("dve", lambda e, d_=d_: e.reciprocal(out=d_[:, 1:2], in_=d_[:, 1:2]), reads=[bG[t]], writes=[bG[t]])
        S.op("dve", lambda e, g_=g_, d_=d_: e.tensor_scalar(out=g_, in0=g_, scalar1=d_[:, 1:2], scalar2=None, op0=ALU.mult),
             reads=[bG[t]], writes=[bG[t]])
    bgu = M.sb("bgu", [128, NEXP, 12], F32)
    bbgu = Buf()
    S.dma("sp", bgu[:], b_guT, writes=[bbgu])
    bdn = M.sb("bdn", [NEXP, D], BF16)
    bbdn = Buf()
    S.dma("pool", bdn[:], b_down, writes=[bbdn])
    GT = M.sb("GT", [NEXP, T], BF16)
    bGT = [Buf() for _ in range(NT)]
    tmp = [M.sb("mtmp%d" % i, [128, 512], F32) for i in range(2)]
    btmp = [Buf(), Buf()]
    for t in range(NT):
        typ = 0 if t < 8 else 1
        bank = P.rr("rt", 2)
        pt = P.pb[bank][0:NEXP, 0:128]
        S.op("pe", lambda e, pt=pt, t=t: e.transpose(pt, G[:, t, :], P.identf[:]), reads=[bG[t], P.b_identf],
             writes=[P.pbb[bank]])
        S.op("act", lambda e, pt=pt, t=t: e.copy(out=GT[:, t * 128:(t + 1) * 128], in_=pt), reads=[P.pbb[bank]],
             writes=[bGT[t]])
        for nb in range(4):
            bank = 4 + nb
            ps = P.pb[bank]
            mm_group(P, ps[:, :], [(GT[:, t * 128:(t + 1) * 128], bdn[:, nb * 512:(nb + 1) * 512])],
                     reads=[bGT[t], bbdn], writes=[P.pbb[bank]])
            ti = P.rr("mtmp", 2)
            tm, btm = tmp[ti], btmp[ti]
            S.op("dve", lambda e, tm=tm, ps=ps, typ=typ, nb=nb: e.tensor_tensor(
                out=tm[:], in0=ps[:, :], in1=gate_bc[:, typ, nb * 512:(nb + 1) * 512], op=ALU.mult),
                reads=[P.pbb[bank], b_gate], writes=[btm])
            xs = P.X[:, t, nb * 512:(nb + 1) * 512]
            S.op("dve", lambda e, xs=xs, tm=tm: e.tensor_tensor(out=xs, in0=xs, in1=tm[:], op=ALU.add),
                 reads=[btm, P.bX[t]], writes=[P.bX[t]])
    slabs = [M.sb("gslab%d" % i, [128, KC, 256], BF16) for i in range(2)]
    bslab = [Buf(), Buf()]
    wd = M.sb("wd", [128, 6, D], BF16)
    bwd = Buf()
    gact = M.sb("gact", [128, 6, T], BF16)
    bga = [Buf() for _ in range(3)]
    gc = [M.sb("gc%d" % i, [128, 512], F32) for i in range(2)]
    sg = [M.sb("sgm%d" % i, [128, 512], BF16) for i in range(2)]
    uc = [M.sb("uc%d" % i, [128, 512], BF16) for i in range(2)]
    bgc, bsg, buc = [Buf(), Buf()], [Buf(), Buf()], [Buf(), Buf()]
    si = 0
    for e_ in range(n_exp):
        S.dma("pool", wd[:], w_down[e_].rearrange("(k p) c -> p k c", p=128), writes=[bwd])
        for i in range(6):
            sl, bsl = slabs[si % 2], bslab[si % 2]
            si += 1
            S.dma("pool", sl[:], w_gu_r[e_, i].rearrange("(k p) c -> p k c", p=128), writes=[bsl])
            for jj in range(1):
                for ng, (n0, nsz) in enumerate(NTILES_N):
                    tl = [n0 // 128 + x for x in range(nsz // 128)]
                    rd = [P.bact[t] for t in tl]
                    bkg, bku = P.rr("gu", 2) * 2, None
                    bku = bkg + 1
                    pg, pu = P.pb[bkg][:, 0:nsz], P.pb[bku][:, 0:nsz]
                    mm_group(P, pg, [(sl[:, k, 0:128], P.actT[:, k, n0:n0 + nsz]) for k in range(KC)],
                             reads=rd + [bsl], writes=[P.pbb[bkg]])
                    mm_group(P, pu, [(sl[:, k, 128:256], P.actT[:, k, n0:n0 + nsz]) for k in range(KC)],
                             reads=rd + [bsl], writes=[P.pbb[bku]])
                    x = P.rr("sw", 2)
                    gcv, sgv, ucv = gc[x][:, 0:nsz], sg[x][:, 0:nsz], uc[x][:, 0:nsz]
                    S.op("dve", lambda e, gcv=gcv, pg=pg, e_=e_, i=i: e.tensor_scalar(
                        out=gcv, in0=pg, scalar1=bgu[:, e_, i:i + 1], scalar2=7.0, op0=ALU.add, op1=ALU.min),
                        reads=[P.pbb[bkg], bbgu], writes=[bgc[x]])
                    S.op("act", lambda e, sgv=sgv, gcv=gcv: e.activation(out=sgv, in_=gcv, func=AF.Sigmoid, scale=1.702),
                         reads=[bgc[x]], writes=[bsg[x]])
                    S.op("dve", lambda e, ucv=ucv, pu=pu, e_=e_, i=i: e.tensor_scalar(
                        out=ucv, in0=pu, scalar1=bgu[:, e_, 6 + i:7 + i], scalar2=7.0, op0=ALU.add, op1=ALU.min),
                        reads=[P.pbb[bku], bbgu], writes=[buc[x]])
                    S.op("dve", lambda e, ucv=ucv: e.tensor_scalar(out=ucv, in0=ucv, scalar1=-7.0, scalar2=1.0,
                                                                   op0=ALU.max, op1=ALU.add), reads=[buc[x]], writes=[buc[x]])
                    S.op("dve", lambda e, gcv=gcv, sgv=sgv: e.tensor_tensor(out=gcv, in0=gcv, in1=sgv, op=ALU.mult),
                         reads=[bgc[x], bsg[x]], writes=[bgc[x]])
                    S.op("dve", lambda e, gcv=gcv, ucv=ucv, i=i, n0=n0, nsz=nsz: e.tensor_tensor(
                        out=gact[:, i, n0:n0 + nsz], in0=gcv, in1=ucv, op=ALU.mult),
                        reads=[bgc[x], buc[x]], writes=[bga[ng]])
        for t in range(NT):
            typ = 0 if t < 8 else 1
            ng = 0 if t < 4 else (1 if t < 8 else 2)
            for nb in range(4):
                bank = 4 + nb
                ps = P.pb[bank]
                pairs = [(gact[:, i, t * 128:(t + 1) * 128], wd[:, i, nb * 512:(nb + 1) * 512]) for i in range(6)]
                mm_group(P, ps[:, :], pairs, reads=[bga[ng], bwd], writes=[P.pbb[bank]])
                ti = P.rr("mtmp", 2)
                tm, btm = tmp[ti], btmp[ti]
                S.op("dve", lambda e, tm=tm, ps=ps, t=t, e_=e_, typ=typ, nb=nb: e.scalar_tensor_tensor(
                    out=tm[:], in0=ps[:, :], scalar=G[:, t, e_:e_ + 1], in1=gate_bc[:, typ, nb * 512:(nb + 1) * 512],
                    op0=ALU.mult, op1=ALU.mult), reads=[P.pbb[bank], bG[t], b_gate], writes=[btm])
                xs = P.X[:, t, nb * 512:(nb + 1) * 512]
                S.op("dve", lambda e, xs=xs, tm=tm: e.tensor_tensor(out=xs, in0=xs, in1=tm[:], op=ALU.add),
                     reads=[btm, P.bX[t]], writes=[P.bX[t]])


def store_x(P, x_out, rows=NT):
    bo = Buf()
    if P.fused and not getattr(P, "final", False):
        return bo
    for t in range(rows):
        P.S.dma("sp", x_out[t * 128:(t + 1) * 128, :], P.X[:, t, :], reads=[P.bX[t]], writes=[bo])
    return bo


def moe_inputs(P):
    w_router = P.din("w_router", [128, KC, NEXP])
    brt = load_small(P, "b_router", [128, NEXP])
    w_gu_r = P.din("w_gu_r", [NEXP, 6, D, 256])
    b_guT = P.din("b_guT", [128, NEXP, 12])
    w_down = P.din("w_down", [NEXP, DEXP, D])
    b_down = P.din("b_down", [NEXP, D])
    return w_router, brt, w_gu_r, b_guT, w_down, b_down


def emit_moe_test(P, n_exp=NEXP):
    x_in = P.din("x_in", [T, D])
    x_out = P.dout("x_out", [T, D])
    modl, P.b_modl = load_small(P, "modl", [128, 2, 96])
    gffn, P.b_g = load_small(P, "gffn", [128, KC])
    mi = moe_inputs(P)
    load_x(P, x_in)
    A, B, bAB = mod_vectors(P, modl, gffn[:], 4, 3, "ffn")
    prenorm(P, A, B, bAB, "ffn")
    g2 = P.M.sb("g2v", [128, 2, KC], F32)
    bg2 = Buf()
    P.S.op("dve", lambda e: e.tensor_copy(out=g2[:], in_=modl[:, :, 80:96]), reads=[P.b_modl], writes=[bg2])
    gate_bc, b_gate = bcast_rows(P, g2, bg2, "g2")
    moe(P, *mi, gate_bc, b_gate, n_exp=n_exp)
    bo = store_x(P, x_out)
    return bo


def build_moe_test(n_exp=NEXP):
    P = Prog()
    P.consts()
    bo = emit_moe_test(P, n_exp=NEXP)
    return P.finish([bo]), P

QGROUPS = [(0, 512), (512, 512), (1024, 128)]
QG256 = [(0, 256), (256, 256), (512, 256), (768, 256), (1024, 128)]


def emit_B1_even(P, lam_init):
    S, M = P.S, P.M
    x_in = P.din("x_in", [T, D])
    q_in = P.din("q_in", [T, 1024], BF16)
    kvu = P.din("kvu_in", [2304, 3072], BF16)
    poolA = P.din("poolA", [4, NT, 3, 128, 128], BF16)
    w_pool = P.din("w_pool", [4, 256, 256])
    w_out = P.din("w_out", [D, D])
    x_out = P.dout("x_out", [T, D])
    modl, P.b_modl = load_small(P, "modl", [128, 2, 96])
    spool, b_spool = load_small(P, "s_poolT", [128, 8])
    lamr, b_lamr = load_small(P, "lam", [128, 256])
    gsub, b_gsub = load_small(P, "g_subln", [128, 128])
    load_x(P, x_in)
    P.actT = M.sb("actT", [128, KC, T], BF16)
    P.bact = [Buf() for _ in range(NT)]
    sc = M.sb("sc", [128, 8], F32)
    bsc = Buf()
    lp = M.sb("lp", [128, 128], F32)
    S.op("dve", lambda e: e.tensor_tensor(out=lp[:, 0:64], in0=lamr[:, 0:64], in1=lamr[:, 64:128], op=ALU.mult),
         reads=[b_lamr], writes=[bsc])
    S.op("dve", lambda e: e.tensor_tensor(out=lp[:, 64:128], in0=lamr[:, 128:192], in1=lamr[:, 192:256], op=ALU.mult),
         reads=[b_lamr, bsc], writes=[bsc])
    S.op("dve", lambda e: e.tensor_reduce(out=sc[:, 0:2], in_=lp[:].rearrange("p (a d) -> p a d", d=64), axis=AX.X, op=ALU.add),
         reads=[bsc], writes=[bsc])
    S.op("act", lambda e: e.activation(out=sc[:, 2:4], in_=sc[:, 0:2], func=AF.Exp), reads=[bsc], writes=[bsc])
    S.op("dve", lambda e: e.tensor_tensor(out=sc[:, 4:5], in0=sc[:, 3:4], in1=sc[:, 2:3], op=ALU.subtract), reads=[bsc], writes=[bsc])
    S.op("dve", lambda e: e.tensor_scalar(out=sc[:, 4:5], in0=sc[:, 4:5], scalar1=-float(lam_init), scalar2=None, op0=ALU.add),
         reads=[bsc], writes=[bsc])
    neglam = sc[:, 4:5]
    S.op("dve", lambda e: e.tensor_scalar(out=gsub[:], in0=gsub[:], scalar1=float(1.0 - lam_init), scalar2=None, op0=ALU.mult),
         reads=[b_gsub], writes=[b_gsub])
    Kl = M.sb("Kl", [128, 18, 128], BF16)
    Va = M.sb("Va", [128, 18, 129], BF16)
    Ql = M.sb("Ql", [128, NT, 128], BF16)
    KT = M.sb("KT", [128, 2304], BF16)
    QT = M.sb("QT", [128, T], BF16)
    PT = [M.sb("PT%d" % i, [128, 18, 256], BF16) for i in range(1)]
    o0 = M.sb("o0", [128, NT, 128], F32)
    ob = M.sb("ob", [128, 128], F32)
    ytok = M.sb("ytok", [128, 128], BF16)
    sm = M.sb("sm", [128, 8], F32)
    bKl, bVa, bQl, bKT, bQT, bo0, bob, byt, bsm = [Buf() for _ in range(9)]
    bPT = [Buf()]
    S.op("pool", lambda e: e.memset(Va[:, :, 128:129], 1.0), writes=[bVa])
    for h in range(8):
        S.dma("sp", Kl[:], kvu[:, h * 128:(h + 1) * 128].rearrange("(t p) c -> p t c", p=128), writes=[bKl])
        S.dma("act", Va[:, :, 0:128], kvu[:, 1024 + h * 128:1024 + (h + 1) * 128].rearrange("(t p) c -> p t c", p=128),
              writes=[bVa])
        S.dma("sp", Ql[:], q_in[:, h * 128:(h + 1) * 128].rearrange("(t p) c -> p t c", p=128), writes=[bQl])
        for (src, bsrc, ntl, dst, bdst) in ((Kl, bKl, 18, KT, bKT), (Ql, bQl, NT, QT, bQT)):
            for g0 in range(0, ntl, 8):
                n = min(8, ntl - g0)
                bank = P.rr("tp", 2)
                pv = P.pb[bank][:, :].bitcast(BF16)

                def tr(e, src=src, g0=g0, n=n, pv=pv):
                    ins = None
                    for j in range(n):
                        ins = e.transpose(pv[:, j * 128:(j + 1) * 128], src[:, g0 + j, :], P.identb[:])
                    return ins
                S.op("pe", tr, reads=[bsrc, P.b_identb], writes=[P.pbb[bank]])
                eng = "act" if (g0 // 8) % 2 == 0 else "dve"
                if eng == "act":
                    S.op("act", lambda e, dst=dst, g0=g0, n=n, pv=pv: e.copy(out=dst[:, g0 * 128:(g0 + n) * 128], in_=pv[:, 0:n * 128]),
                         reads=[P.pbb[bank]], writes=[bdst])
                else:
                    S.op("dve", lambda e, dst=dst, g0=g0, n=n, pv=pv: e.tensor_copy(out=dst[:, g0 * 128:(g0 + n) * 128], in_=pv[:, 0:n * 128]),
                         reads=[P.pbb[bank]], writes=[bdst])
        for m in range(2):
            ms = slice(m * 64, (m + 1) * 64)
            for (q0, qn_) in QG256:
                ktiles = list(range(18)) if q0 < 1024 else [16, 17]
                pi = 0
                pt, bpt = PT[pi], bPT[pi]
                for kt in ktiles:
                    bank = 2 + P.rr("sc", 2)
                    ps = P.pb[bank][:, 0:qn_]
                    mm_group(P, ps, [(KT[ms, kt * 128:(kt + 1) * 128], QT[ms, q0:q0 + qn_])], reads=[bKT, bQT],
                             writes=[P.pbb[bank]])
                    S.op("act", lambda e, pt=pt, kt=kt, ps=ps, qn_=qn_: e.activation(
                        out=pt[:, kt, 0:qn_], in_=ps, func=AF.Exp, scale=0.125), reads=[P.pbb[bank]], writes=[bpt])
                for qq in range(qn_ // 128):
                    t = q0 // 128 + qq
                    bank = 4 + P.rr("av", 2)
                    po = P.pb[bank][:, 0:129]
                    mm_group(P, po, [(pt[:, kt, qq * 128:(qq + 1) * 128], Va[:, kt, :]) for kt in ktiles],
                             reads=[bpt, bVa], writes=[P.pbb[bank]])
                    S.op("dve", lambda e, po=po: e.reciprocal(out=sm[:, 0:1], in_=po[:, 128:129]), reads=[P.pbb[bank]],
                         writes=[bsm])
                    if m == 0:
                        S.op("dve", lambda e, po=po, t=t: e.tensor_scalar(out=o0[:, t, :], in0=po[:, 0:128], scalar1=sm[:, 0:1],
                                                                        scalar2=None, op0=ALU.mult),
                             reads=[P.pbb[bank], bsm], writes=[bo0])
                    else:
                        S.op("dve", lambda e: e.tensor_tensor(out=sm[:, 1:2], in0=sm[:, 0:1], in1=neglam, op=ALU.mult),
                             reads=[bsm, bsc], writes=[bsm])
                        S.op("dve", lambda e, po=po, t=t: e.scalar_tensor_tensor(
                            out=ob[:], in0=po[:, 0:128], scalar=sm[:, 1:2], in1=o0[:, t, :], op0=ALU.mult, op1=ALU.add),
                            reads=[P.pbb[bank], bsm, bo0], writes=[bob])
                        S.op("act", lambda e: e.activation(out=ytok[:], in_=ob[:], func=AF.Square, accum_out=sm[:, 2:3]),
                             reads=[bob], writes=[byt, bsm])
                        S.op("act", lambda e: e.activation(out=sm[:, 2:3], in_=sm[:, 2:3], func=AF.Sqrt, bias=P.epsc[:, 0:1],
                                                           scale=1.0 / 128), reads=[bsm, P.b_eps], writes=[bsm])
                        S.op("dve", lambda e: e.reciprocal(out=sm[:, 2:3], in_=sm[:, 2:3]), reads=[bsm], writes=[bsm])
                        S.op("dve", lambda e: e.scalar_tensor_tensor(out=ytok[:], in0=ob[:], scalar=sm[:, 2:3], in1=gsub[:],
                                                                     op0=ALU.mult, op1=ALU.mult),
                             reads=[bob, bsm, b_gsub], writes=[byt])
                        bk2 = P.rr("tp", 2)
                        pv = P.pb[bk2][:, :].bitcast(BF16)
                        S.op("pe", lambda e, pv=pv: e.transpose(pv[:, 0:128], ytok[:], P.identb[:]), reads=[byt, P.b_identb],
                             writes=[P.pbb[bk2]])
                        S.op("act", lambda e, pv=pv, h=h, t=t: e.copy(out=P.actT[:, h, t * 128:(t + 1) * 128], in_=pv[:, 0:128]),
                             reads=[P.pbb[bk2]], writes=[P.bact[t]])
    Ug = M.sb("Ug", [128, 18, 256], BF16)
    PA = M.sb("PA", [128, NT * 3, 128], BF16)
    pooledT = M.sb("pooledT", [128, 2, T], BF16)
    wp = M.sb("wp", [128, 4, 2, 256], BF16)
    bUg, bPA, bpl, bwp = Buf(), Buf(), Buf(), Buf()
    S.dma("pool", wp[:], w_pool.rearrange("g (c p) e -> p g c e", p=128), writes=[bwp])
    for g in range(4):
        S.dma("sp", Ug[:], kvu[:, 2048 + g * 256:2048 + (g + 1) * 256].rearrange("(t p) c -> p t c", p=128), writes=[bUg])
        S.dma("act", PA[:], poolA[g].rearrange("j k p t -> p (j k) t"), writes=[bPA])
        for j in range(NT):
            if j < 8:
                kts = [(j - 1) if j > 0 else 15, j, (j + 1) if j < 7 else 8]
            else:
                kts = [17, 16, 17]
            bank = 6 + P.rr("op", 2)
            for cc in range(2):
                ps = P.pb[bank][:, cc * 128:(cc + 1) * 128]
                mm_group(P, ps, [(Ug[:, kts[kk], cc * 128:(cc + 1) * 128], PA[:, j * 3 + kk, :]) for kk in range(3)],
                         reads=[bUg, bPA] + ([P.pbb[bank]] if cc else []), writes=[P.pbb[bank]])
            S.op("act", lambda e, bank=bank, j=j: e.copy(
                out=pooledT[:, :, j * 128:(j + 1) * 128], in_=P.pb[bank][:, 0:256].rearrange("p (c t) -> p c t", c=2)),
                reads=[P.pbb[bank]], writes=[bpl])
        for ec in range(2):
            for (n0, nsz) in QGROUPS:
                bank = 6 + P.rr("op", 2)
                ps = P.pb[bank][:, 0:nsz]
                mm_group(P, ps, [(wp[:, g, cc, ec * 128:(ec + 1) * 128], pooledT[:, cc, n0:n0 + nsz]) for cc in range(2)],
                         reads=[bwp, bpl], writes=[P.pbb[bank]])
                ch = 8 + g * 2 + ec
                tl = [P.bact[n0 // 128 + x] for x in range(nsz // 128)]
                S.op("act", lambda e, ps=ps, ch=ch, n0=n0, nsz=nsz, g=g, ec=ec: e.activation(
                    out=P.actT[:, ch, n0:n0 + nsz], in_=ps, func=AF.Identity, scale=spool[:, g * 2 + ec:g * 2 + ec + 1]),
                    reads=[P.pbb[bank], b_spool], writes=tl)
    g1 = M.sb("g1v", [128, 2, KC], F32)
    bg1 = Buf()
    S.op("dve", lambda e: e.tensor_copy(out=g1[:], in_=modl[:, :, 32:48]), reads=[P.b_modl], writes=[bg1])
    gate_bc, b_gate = bcast_rows(P, g1, bg1, "g1")
    out_proj(P, w_out, gate_bc, b_gate)
    bo = store_x(P, x_out)
    return bo


def build_B1_even(lam_init):
    P = Prog()
    P.consts()
    bo = emit_B1_even(P, lam_init)
    return P.finish([bo]), P

def rms_rstd(P, sq_ap, out_ap, n, reads, bout):
    S = P.S
    S.op("act", lambda e: e.activation(out=out_ap, in_=out_ap, func=AF.Sqrt, bias=P.epsc[:, 0:1], scale=1.0 / n),
         reads=[bout, P.b_eps] + reads, writes=[bout])
    S.op("dve", lambda e: e.reciprocal(out=out_ap, in_=out_ap), reads=[bout], writes=[bout])


def emit_A_odd(P):
    S, M = P.S, P.M
    x_in = P.din("x_in", [T, D])
    w_in = P.din("w_in", [D, 1600])
    w_qb = P.din("w_qb", [512, 2304])
    w_kvb = P.din("w_kvb", [512, 3072])
    q_out = P.dout("q_out", [T, 2304], BF16)
    kvu_out = P.dout("kvu_out", [T, 4352], BF16)
    modl, P.b_modl = load_small(P, "modl", [128, 2, 96])
    gmix, P.b_g = load_small(P, "gmix", [128, KC])
    gqa, b_gqa = load_small(P, "g_qa", [128, 512])
    gkva, b_gkva = load_small(P, "g_kva", [128, 512])
    gmq, b_gmq = load_small(P, "g_mq", [128, 192])
    gmk, b_gmk = load_small(P, "g_mk", [128, 192])
    rope, b_rope = load_small(P, "rope", [128, NT, 2, 32])
    stream_x(P, x_in)
    A, B, bAB = mod_vectors(P, modl, gmix[:], 1, 0, "mix")
    prenorm(P, A, B, bAB, "mix")
    wq = M.sb("wq", [128, 4, 2304], BF16)
    wkvs = [M.sb("wkv%d" % i, [128, 4, 512], BF16) for i in range(2)]
    bwkvs = [Buf(), Buf()]
    bwq = Buf()
    S.dma("pool", wq[:], w_qb.rearrange("(k p) c -> p k c", p=128), writes=[bwq])
    slabs = [M.sb("slab%d" % i, [128, KC, 512], BF16) for i in range(1)] * 2
    bslab = [Buf()] * 2
    cT = [M.sb("cqT", [128, 4, T], BF16), M.sb("ckvT", [128, 4, T], BF16)]
    bcT = [[Buf() for _ in range(NT)] for _ in range(2)]
    kpe = M.sb("kpe", [128, NT, 64], F32)
    bkpe = [Buf() for _ in range(NT)]
    sspe = M.sb("sspe", [128, NT], F32)
    stg = [M.sb("stg%d" % i, [128, 640], BF16) for i in range(2)]
    bstg = [Buf(), Buf()]
    sqb = M.sb("sqb", [128, 512], F32)
    bsq = Buf()
    cn = M.sb("cn", [128, 512], BF16)
    bcn = Buf()
    qn = M.sb("qn", [128, 512], F32)
    bqn = Buf()
    ra = [M.sb("ra%d" % i, [128, 2, 32], F32) for i in range(4)]
    bra = [Buf() for _ in range(4)]
    rs = M.sb("rs", [128, 4], F32)
    brs = Buf()
    bo = Buf()
    cgs = [(0, 512), (512, 512), (1024, 512), (1536, 64)]
    for cg, (c0, cw) in enumerate(cgs):
        sl, bsl = slabs[cg % 2], bslab[cg % 2]
        S.dma("pool", sl[:, :, 0:cw], w_in[:, c0:c0 + cw].rearrange("(k p) c -> p k c", p=128), writes=[bsl])
        for t in range(NT):
            bank = 4 + P.rr("ip", 4)
            ps = P.pb[bank][:, 0:cw]
            mm_group(P, ps, [(P.actT[:, k, t * 128:(t + 1) * 128], sl[:, k, 0:cw]) for k in range(KC)],
                     reads=[P.bact[t], bsl], writes=[P.pbb[bank]])
            rows = slice(t * 128, (t + 1) * 128)
            if cg < 2:
                gvec, bg_ = (gqa, b_gqa) if cg == 0 else (gkva, b_gkva)
                S.op("act", lambda e, ps=ps: e.activation(out=sqb[:], in_=ps, func=AF.Square, accum_out=rs[:, 0:1]),
                     reads=[P.pbb[bank]], writes=[bsq, brs])
                rms_rstd(P, None, rs[:, 0:1], 512, [], brs)
                S.op("dve", lambda e, ps=ps, gvec=gvec: e.scalar_tensor_tensor(
                    out=cn[:], in0=ps, scalar=rs[:, 0:1], in1=gvec[:], op0=ALU.mult, op1=ALU.mult),
                    reads=[P.pbb[bank], brs, bg_], writes=[bcn])
                bk2 = P.rr("tp", 2)
                pv = P.pb[bk2][:, :].bitcast(BF16)

                def tr(e, pv=pv):
                    ins = None
                    for j in range(4):
                        ins = e.transpose(pv[:, j * 128:(j + 1) * 128], cn[:, j * 128:(j + 1) * 128], P.identb[:])
                    return ins
                S.op("pe", tr, reads=[bcn, P.b_identb], writes=[P.pbb[bk2]])
                S.op("act", lambda e, pv=pv, cg=cg, t=t: e.copy(
                    out=cT[cg][:, :, t * 128:(t + 1) * 128], in_=pv[:, 0:512].rearrange("p (c t) -> p c t", c=4)),
                    reads=[P.pbb[bk2]], writes=[bcT[cg][t]])
            elif cg == 2:
                si = P.rr("stg", 2)
                S.op("act", lambda e, si=si, ps=ps: e.copy(out=stg[si][:, 0:512], in_=ps), reads=[P.pbb[bank]], writes=[bstg[si]])
                S.dma("sp", kvu_out[rows, 3840:4352], stg[si][:, 0:512], reads=[bstg[si]], writes=[bo])
            else:
                S.op("act", lambda e, ps=ps, t=t: e.copy(out=kpe[:, t, :], in_=ps), reads=[P.pbb[bank]], writes=[bkpe[t]])
                S.op("act", lambda e, t=t: e.activation(out=sqb[:, 0:64], in_=kpe[:, t, :], func=AF.Square,
                                                      accum_out=sspe[:, t:t + 1]), reads=[bkpe[t]], writes=[bsq, bkpe[t]])

    def rope2(src4, dst4, t, breads, bdst):
        x1, x2 = src4[:, :, 0, :], src4[:, :, 1, :]
        cosb = rope[:, t, 0, :].unsqueeze(1).to_broadcast([128, 2, 32])
        sinb = rope[:, t, 1, :].unsqueeze(1).to_broadcast([128, 2, 32])
        ids = [P.rr("ra", 4) for _ in range(4)]
        tm = [(ra[i], bra[i]) for i in ids]
        S.op("dve", lambda e: e.tensor_tensor(out=tm[0][0][:], in0=x1, in1=cosb, op=ALU.mult), reads=breads + [b_rope], writes=[tm[0][1]])
        S.op("dve", lambda e: e.tensor_tensor(out=tm[1][0][:], in0=x2, in1=sinb, op=ALU.mult), reads=breads + [b_rope], writes=[tm[1][1]])
        S.op("dve", lambda e: e.tensor_tensor(out=tm[2][0][:], in0=x2, in1=cosb, op=ALU.mult), reads=breads + [b_rope], writes=[tm[2][1]])
        S.op("dve", lambda e: e.tensor_tensor(out=tm[3][0][:], in0=x1, in1=sinb, op=ALU.mult), reads=breads + [b_rope], writes=[tm[3][1]])
        S.op("dve", lambda e: e.tensor_tensor(out=dst4[:, :, 0, :], in0=tm[0][0][:], in1=tm[1][0][:], op=ALU.subtract),
             reads=[tm[0][1], tm[1][1]], writes=[bdst])
        S.op("dve", lambda e: e.tensor_tensor(out=dst4[:, :, 1, :], in0=tm[2][0][:], in1=tm[3][0][:], op=ALU.add),
             reads=[tm[2][1], tm[3][1]], writes=[bdst])

    for g6 in range(6):
        for t in range(NT):
            bank = 4 + P.rr("ip", 4)
            ps = P.pb[bank][:, 0:384]
            mm_group(P, ps, [(cT[0][:, c, t * 128:(t + 1) * 128], wq[:, c, g6 * 384:(g6 + 1) * 384]) for c in range(4)],
                     reads=[bcT[0][t], bwq], writes=[P.pbb[bank]])
            S.op("act", lambda e, ps=ps: e.activation(out=sqb[:, 0:384], in_=ps, func=AF.Square), reads=[P.pbb[bank]], writes=[bsq])
            S.op("dve", lambda e: e.tensor_reduce(out=rs[:, 0:2], in_=sqb[:, 0:384].rearrange("p (h d) -> p h d", d=192),
                                                  axis=AX.X, op=ALU.add), reads=[bsq], writes=[brs])
            rms_rstd(P, None, rs[:, 0:2], 192, [], brs)
            q3 = qn[:, 0:384].rearrange("p (h d) -> p h d", d=192)
            S.op("dve", lambda e, ps=ps, q3=q3: e.tensor_tensor(out=q3, in0=ps.rearrange("p (h d) -> p h d", d=192),
                                                              in1=rs[:, 0:2].unsqueeze(2).to_broadcast([128, 2, 192]), op=ALU.mult),
                 reads=[P.pbb[bank], brs], writes=[bqn])
            S.op("dve", lambda e, q3=q3: e.tensor_tensor(out=q3, in0=q3, in1=gmq[:].unsqueeze(1).to_broadcast([128, 2, 192]),
                                                         op=ALU.mult), reads=[bqn, b_gmq], writes=[bqn])
            si = P.rr("stg", 2)
            s3 = stg[si][:, 0:384].rearrange("p (h d) -> p h d", d=192)
            S.op("act", lambda e, s3=s3, q3=q3: e.copy(out=s3[:, :, 0:128], in_=q3[:, :, 0:128]), reads=[bqn], writes=[bstg[si]])
            rope2(q3[:, :, 128:192].rearrange("p h (a d) -> p h a d", a=2), s3[:, :, 128:192].rearrange("p h (a d) -> p h a d", a=2),
                  t, [bqn], bstg[si])
            S.dma("sp", q_out[t * 128:(t + 1) * 128, g6 * 384:(g6 + 1) * 384], stg[si][:, 0:384], reads=[bstg[si]], writes=[bo])
    for g6 in range(6):
        wkv, bwkv = wkvs[g6 % 2], bwkvs[g6 % 2]
        S.dma("pool", wkv[:], w_kvb[:, g6 * 512:(g6 + 1) * 512].rearrange("(k p) c -> p k c", p=128), writes=[bwkv])
        for t in range(NT):
            bank = 4 + P.rr("ip", 4)
            ps = P.pb[bank][:, 0:512]
            mm_group(P, ps, [(cT[1][:, c, t * 128:(t + 1) * 128], wkv[:, c, :]) for c in range(4)],
                     reads=[bcT[1][t], bwkv], writes=[P.pbb[bank]])
            p4 = ps.rearrange("p (h a d) -> p h a d", h=2, a=2)
            S.op("act", lambda e, p4=p4: e.activation(out=sqb[:, 0:256].rearrange("p (h d) -> p h d", d=128), in_=p4[:, :, 0, :],
                                                    func=AF.Square), reads=[P.pbb[bank]], writes=[bsq])
            S.op("dve", lambda e: e.tensor_reduce(out=rs[:, 0:2], in_=sqb[:, 0:256].rearrange("p (h d) -> p h d", d=128),
                                                  axis=AX.X, op=ALU.add), reads=[bsq], writes=[brs])
            S.op("dve", lambda e, t=t: e.tensor_scalar(out=rs[:, 0:2], in0=rs[:, 0:2], scalar1=sspe[:, t:t + 1], scalar2=None,
                                                      op0=ALU.add), reads=[brs, bkpe[t]], writes=[brs])
            rms_rstd(P, None, rs[:, 0:2], 192, [], brs)
            si = P.rr("stg", 2)
            s3 = stg[si][:, 0:640].rearrange("p (h d) -> p h d", d=320)
            k3 = qn[:, 0:256].rearrange("p (h d) -> p h d", d=128)
            S.op("dve", lambda e, p4=p4, k3=k3: e.tensor_tensor(out=k3, in0=p4[:, :, 0, :],
                                                              in1=rs[:, 0:2].unsqueeze(2).to_broadcast([128, 2, 128]), op=ALU.mult),
                 reads=[P.pbb[bank], brs], writes=[bqn])
            S.op("dve", lambda e, k3=k3, s3=s3: e.tensor_tensor(out=s3[:, :, 0:128], in0=k3,
                                                                in1=gmk[:, 0:128].unsqueeze(1).to_broadcast([128, 2, 128]), op=ALU.mult),
                 reads=[bqn, b_gmk], writes=[bstg[si]])
            kp = qn[:, 256:384].rearrange("p (h d) -> p h d", d=64)
            S.op("dve", lambda e, kp=kp, t=t: e.tensor_tensor(out=kp, in0=kpe[:, t, :].unsqueeze(1).to_broadcast([128, 2, 64]),
                                                             in1=rs[:, 0:2].unsqueeze(2).to_broadcast([128, 2, 64]), op=ALU.mult),
                 reads=[bkpe[t], brs, bqn], writes=[bqn])
            S.op("dve", lambda e, kp=kp: e.tensor_tensor(out=kp, in0=kp, in1=gmk[:, 128:192].unsqueeze(1).to_broadcast([128, 2, 64]),
                                                         op=ALU.mult), reads=[bqn, b_gmk], writes=[bqn])
            rope2(kp.rearrange("p h (a d) -> p h a d", a=2), s3[:, :, 128:192].rearrange("p h (a d) -> p h a d", a=2), t, [bqn], bstg[si])
            S.op("act", lambda e, s3=s3, p4=p4: e.copy(out=s3[:, :, 192:320], in_=p4[:, :, 1, :]), reads=[P.pbb[bank]], writes=[bstg[si]])
            S.dma("sp", kvu_out[t * 128:(t + 1) * 128, g6 * 640:(g6 + 1) * 640], stg[si][:, 0:640], reads=[bstg[si]], writes=[bo])
    return bo


def build_A_odd():
    P = Prog()
    P.consts()
    bo = emit_A_odd(P)
    return P.finish([bo]), P


def emit_B1_odd(P):
    S, M = P.S, P.M
    x_in = P.din("x_in", [T, D])
    q_in = P.din("q_in", [T, 2304], BF16)
    kvu = P.din("kvu_in", [2304, 4352], BF16)
    CN = P.din("dftC", [2048, 1024], BF16)
    SN = P.din("dftS", [2048, 1024], BF16)
    CNc = P.din("dftCc", [256, 128], BF16)
    SNc = P.din("dftSc", [256, 128], BF16)
    CCd = P.din("dftCC", [128, 128], BF16)
    SCd = P.din("dftSC", [128, 128], BF16)
    w_f = P.din("w_fourier", [4, 128, 128])
    w_out = P.din("w_out", [D, D])
    x_out = P.dout("x_out", [T, D])
    modl, P.b_modl = load_small(P, "modl", [128, 2, 96])
    load_x(P, x_in)
    P.actT = M.sb("actT", [128, KC, T], BF16)
    P.bact = [Buf() for _ in range(NT)]
    Kl = M.sb("Kl", [128, 18, 192], BF16)
    Va = M.sb("Va", [128, 18, 129], BF16)
    Ql = M.sb("Ql", [128, NT, 192], BF16)
    KT0 = M.sb("KT0", [128, 2304], BF16)
    KT1 = M.sb("KT1", [64, 2304], BF16)
    QT0 = M.sb("QT0", [128, T], BF16)
    QT1 = M.sb("QT1", [64, T], BF16)
    PT = M.sb("PT", [128, 18, 256], BF16)
    ytok = M.sb("ytok", [128, 128], BF16)
    sm = M.sb("sm", [128, 4], F32)
    bKl, bVa, bQl, bKT, bQT, bPT, byt, bsm = [Buf() for _ in range(8)]
    S.op("pool", lambda e: e.memset(Va[:, :, 128:129], 1.0), writes=[bVa])
    scale = 192 ** -0.5
    for h in range(12):
        S.dma("sp", Kl[:], kvu[:, h * 320:h * 320 + 192].rearrange("(t p) c -> p t c", p=128), writes=[bKl])
        S.dma("act", Va[:, :, 0:128], kvu[:, h * 320 + 192:(h + 1) * 320].rearrange("(t p) c -> p t c", p=128), writes=[bVa])
        S.dma("sp", Ql[:], q_in[:, h * 192:(h + 1) * 192].rearrange("(t p) c -> p t c", p=128), writes=[bQl])
        for (src, bsrc, ntl, d0, d1, bdst) in ((Kl, bKl, 18, KT0, KT1, bKT), (Ql, bQl, NT, QT0, QT1, bQT)):
            for g0 in range(0, ntl, 8):
                n = min(8, ntl - g0)
                for part in range(2):
                    bank = P.rr("tp", 2)
                    pv = P.pb[bank][:, :].bitcast(BF16)
                    np_ = 128 if part == 0 else 64
                    c0 = 0 if part == 0 else 128

                    def tr(e, src=src, g0=g0, n=n, pv=pv, np_=np_, c0=c0):
                        ins = None
                        for j in range(n):
                            ins = e.transpose(pv[0:np_, j * 128:(j + 1) * 128], src[:, g0 + j, c0:c0 + np_], P.identb[:])
                        return ins
                    S.op("pe", tr, reads=[bsrc, P.b_identb], writes=[P.pbb[bank]])
                    dst = d0 if part == 0 else d1
                    if part == 0:
                        S.op("act", lambda e, dst=dst, g0=g0, n=n, pv=pv, np_=np_: e.copy(
                            out=dst[0:np_, g0 * 128:(g0 + n) * 128], in_=pv[0:np_, 0:n * 128]), reads=[P.pbb[bank]], writes=[bdst])
                    else:
                        S.op("dve", lambda e, dst=dst, g0=g0, n=n, pv=pv, np_=np_: e.tensor_copy(
                            out=dst[0:np_, g0 * 128:(g0 + n) * 128], in_=pv[0:np_, 0:n * 128]), reads=[P.pbb[bank]], writes=[bdst])
        for (q0, qn_) in QG256:
            ktiles = list(range(18)) if q0 < 1024 else [16, 17]
            for kt in ktiles:
                bank = 2 + P.rr("sc", 2)
                ps = P.pb[bank][:, 0:qn_]
                mm_group(P, ps, [(KT0[:, kt * 128:(kt + 1) * 128], QT0[:, q0:q0 + qn_]),
                                 (KT1[:, kt * 128:(kt + 1) * 128], QT1[:, q0:q0 + qn_])], reads=[bKT, bQT], writes=[P.pbb[bank]])
                S.op("act", lambda e, kt=kt, ps=ps, qn_=qn_: e.activation(out=PT[:, kt, 0:qn_], in_=ps, func=AF.Exp, scale=scale),
                     reads=[P.pbb[bank]], writes=[bPT])
            for qq in range(qn_ // 128):
                t = q0 // 128 + qq
                bank = 4 + P.rr("av", 2)
                po = P.pb[bank][:, 0:129]
                mm_group(P, po, [(PT[:, kt, qq * 128:(qq + 1) * 128], Va[:, kt, :]) for kt in ktiles], reads=[bPT, bVa],
                         writes=[P.pbb[bank]])
                S.op("dve", lambda e, po=po: e.reciprocal(out=sm[:, 0:1], in_=po[:, 128:129]), reads=[P.pbb[bank]], writes=[bsm])
                S.op("dve", lambda e, po=po: e.tensor_scalar(out=ytok[:], in0=po[:, 0:128], scalar1=sm[:, 0:1], scalar2=None,
                                                            op0=ALU.mult), reads=[P.pbb[bank], bsm], writes=[byt])
                bk2 = P.rr("tp", 2)
                pv = P.pb[bk2][:, :].bitcast(BF16)
                S.op("pe", lambda e, pv=pv: e.transpose(pv[:, 0:128], ytok[:], P.identb[:]), reads=[byt, P.b_identb],
                     writes=[P.pbb[bk2]])
                S.op("act", lambda e, pv=pv, h=h, t=t: e.copy(out=P.actT[:, h, t * 128:(t + 1) * 128], in_=pv[:, 0:128]),
                     reads=[P.pbb[bk2]], writes=[P.bact[t]])
    Ug = M.sb("Ug", [128, 18, 128], BF16)
    Csl = [M.sb("Csl%d" % i, [128, 16, 128], BF16) for i in range(2)]
    Ssl = [M.sb("Ssl%d" % i, [128, 16, 256], BF16) for i in range(2)] if False else None
    Cc = M.sb("Cc", [128, 2, 2, 128], BF16)
    AB = M.sb("AB", [128, 2, T], BF16)
    W12 = M.sb("W12", [128, 2, 4, 128], BF16)
    CS = M.sb("CS", [128, 2, 128], BF16)
    wf = M.sb("wf", [128, 4, 128], BF16)
    bUg, bCc, bAB, bW, bCS, bwf = [Buf() for _ in range(6)]
    bCsl = [Buf(), Buf()]
    S.dma("sp", Cc[:, 0], CNc.rearrange("(t p) c -> p t c", p=128), writes=[bCc])
    S.dma("sp", Cc[:, 1], SNc.rearrange("(t p) c -> p t c", p=128), writes=[bCc])
    S.dma("sp", CS[:, 0, :], CCd, writes=[bCS])
    S.dma("sp", CS[:, 1, :], SCd, writes=[bCS])
    S.dma("pool", wf[:], w_f.rearrange("g c e -> c g e"), writes=[bwf])
    for g in range(4):
        for ab in range(2):
            bank = 6 + P.rr("op", 2)
            ps = P.pb[bank][:, 0:128]
            mm_group(P, ps, [(CS[:, ab, :], wf[:, g, :])], reads=[bCS, bwf], writes=[P.pbb[bank]])
            S.op("act", lambda e, ps=ps, ab=ab, g=g: e.activation(out=W12[:, ab, g, :], in_=ps, func=AF.Identity,
                                                                scale=(1.0 if ab == 0 else -1.0)), reads=[P.pbb[bank]], writes=[bW])
    for g in range(4):
        S.dma("sp", Ug[:], kvu[:, 3840 + g * 128:3840 + (g + 1) * 128].rearrange("(t p) c -> p t c", p=128), writes=[bUg])
        for ab, tab in enumerate((CN, SN)):
            for og in range(8):
                ci = P.rr("csl", 2)
                S.dma("act" if ci else "sp", Csl[ci][:], tab[:, og * 128:(og + 1) * 128].rearrange("(t p) c -> p t c", p=128),
                      writes=[bCsl[ci]])
                bank = 6 + P.rr("op", 2)
                ps = P.pb[bank][:, 0:128]
                mm_group(P, ps, [(Ug[:, kt, :], Csl[ci][:, kt, :]) for kt in range(16)], reads=[bUg, bCsl[ci]], writes=[P.pbb[bank]])
                S.op("act" if og % 2 else "dve", (lambda e, ps=ps, ab=ab, g=g, og=og: e.copy(out=AB[:, ab, og * 128:(og + 1) * 128], in_=ps))
                     if og % 2 else (lambda e, ps=ps, ab=ab, g=g, og=og: e.tensor_copy(out=AB[:, ab, og * 128:(og + 1) * 128], in_=ps)),
                     reads=[P.pbb[bank]], writes=[bAB])
            bank = 6 + P.rr("op", 2)
            ps = P.pb[bank][:, 0:128]
            mm_group(P, ps, [(Ug[:, 16 + kt, :], Cc[:, ab, kt, :]) for kt in range(2)], reads=[bUg, bCc], writes=[P.pbb[bank]])
            S.op("act", lambda e, ps=ps, ab=ab, g=g: e.copy(out=AB[:, ab, 1024:1152], in_=ps), reads=[P.pbb[bank]], writes=[bAB])
        for (n0, nsz) in QGROUPS:
            bank = 6 + P.rr("op", 2)
            ps = P.pb[bank][:, 0:nsz]
            mm_group(P, ps, [(W12[:, 0, g, :], AB[:, 0, n0:n0 + nsz]), (W12[:, 1, g, :], AB[:, 1, n0:n0 + nsz])],
                     reads=[bW, bAB], writes=[P.pbb[bank]])
            tl = [P.bact[n0 // 128 + x] for x in range(nsz // 128)]
            S.op("act", lambda e, ps=ps, g=g, n0=n0, nsz=nsz: e.copy(out=P.actT[:, 12 + g, n0:n0 + nsz], in_=ps),
                 reads=[P.pbb[bank]], writes=tl)
    g1 = M.sb("g1v", [128, 2, KC], F32)
    bg1 = Buf()
    S.op("dve", lambda e: e.tensor_copy(out=g1[:], in_=modl[:, :, 32:48]), reads=[P.b_modl], writes=[bg1])
    gate_bc, b_gate = bcast_rows(P, g1, bg1, "g1")
    out_proj(P, w_out, gate_bc, b_gate)
    bo = store_x(P, x_out)
    return bo


def build_B1_odd():
    P = Prog()
    P.consts()
    bo = emit_B1_odd(P)
    return P.finish([bo]), P

def _fm(v):
    return np.ascontiguousarray(np.asarray(v, np.float32).reshape(-1, 128).T)


def _bc(v):
    return np.ascontiguousarray(np.tile(np.asarray(v, np.float32).reshape(1, -1), (128, 1)))


def _rope_tab(rot_dim):
    rows = 2048 // 64
    row = np.repeat(np.arange(rows, dtype=np.float32), 64)
    col = np.tile(np.arange(64, dtype=np.float32), rows)
    axis_dim = rot_dim // 2
    inv = (10000.0 ** (-np.arange(0, axis_dim, 2, dtype=np.float32) / axis_dim)).astype(np.float32)
    ang = np.concatenate([row[:, None] * inv, col[:, None] * inv], -1)
    return np.cos(ang).astype(np.float32), np.sin(ang).astype(np.float32)


def _rope_core(rot_dim, half):
    c, s = _rope_tab(rot_dim)
    rope = np.zeros((T, 2, rot_dim // 2), np.float32)
    rope[:, 0] = 1.0
    rope[:TL, 0] = c[half * TL:(half + 1) * TL]
    rope[:TL, 1] = s[half * TL:(half + 1) * TL]
    return np.ascontiguousarray(rope.reshape(NT, 128, 2, rot_dim // 2).transpose(1, 0, 2, 3))


def _pool_mats(half):
    out = np.zeros((4, NT, 3, 128, 128), np.float32)
    for wi, w in enumerate((2, 4, 8, 16)):
        for j in range(NT):
            n = 2048 if j < 8 else 256
            gt = (half * 8 + j) if j < 8 else half
            t = gt * 128 + np.arange(128)
            lo = np.clip(t - w // 2, 0, n)
            hi = np.clip(t + w - w // 2, 0, n)
            cnt = (hi - lo).astype(np.float32)
            for kk in range(3):
                gi = gt + kk - 1
                if gi < 0 or gi >= n // 128:
                    continue
                tp = gi * 128 + np.arange(128)
                m = ((tp[:, None] >= lo[None, :]) & (tp[:, None] < hi[None, :])).astype(np.float32) / cnt[None, :]
                m -= (tp[:, None] == t[None, :]).astype(np.float32)
                out[wi, j, kk] = m
    return out.astype(NPBF)


def _dft_tabs(half):
    i = np.arange(2048)
    n_in = np.where(i < 1024, half * 1024 + i, (1 - half) * 1024 + (i - 1024)).astype(np.int64)
    n_out = (half * 1024 + np.arange(1024)).astype(np.int64)
    ang = 2 * np.pi * ((n_in[:, None] * n_out[None, :]) % 2048).astype(np.float64) / 2048
    nrm = 1.0 / np.sqrt(2048 * 128)
    d = {"dftC": (np.cos(ang) * nrm).astype(NPBF), "dftS": (np.sin(ang) * nrm).astype(NPBF)}
    i = np.arange(256)
    n_in = np.where(i < 128, half * 128 + i, (1 - half) * 128 + (i - 128)).astype(np.int64)
    n_out = (half * 128 + np.arange(128)).astype(np.int64)
    ang = 2 * np.pi * ((n_in[:, None] * n_out[None, :]) % 256).astype(np.float64) / 256
    nrm = 1.0 / np.sqrt(256 * 128)
    d["dftCc"] = (np.cos(ang) * nrm).astype(NPBF)
    d["dftSc"] = (np.sin(ang) * nrm).astype(NPBF)
    c = np.arange(128)
    ang = 2 * np.pi * ((c[:, None] * c[None, :]) % 128).astype(np.float64) / 128
    d["dftCC"] = np.cos(ang).astype(NPBF)
    d["dftSC"] = np.sin(ang).astype(NPBF)
    return d


def _modl_core(mod, l, b):
    m = np.zeros((128, 2, 96), np.float32)
    for j in range(6):
        m[:, 0, j * 16:(j + 1) * 16] = _fm(mod[l, b, j * 2048:(j + 1) * 2048])
        m[:, 1, j * 16:(j + 1) * 16] = _fm(mod[l, 4, j * 2048:(j + 1) * 2048])
    return m


def _kv_pair(own, partner):
    return np.ascontiguousarray(np.concatenate([own[:TL], partner[:TL], own[TL:], partner[TL:]], 0))


def _moe_host(inp, l):
    wg = inp["w_gu"][l]
    g = wg[:, :, :DEXP].reshape(NEXP, D, 6, 1, 128)
    u = wg[:, :, DEXP:].reshape(NEXP, D, 6, 1, 128)
    w_gu_r = np.ascontiguousarray(np.concatenate([g, u], 3).transpose(0, 2, 1, 3, 4).reshape(NEXP, 6, D, 256))
    b_guT = np.ascontiguousarray(inp["b_gu"][l].reshape(NEXP, 12, 128).transpose(2, 0, 1))
    return {"w_router": np.ascontiguousarray(inp["w_router"][l].reshape(KC, 128, NEXP).transpose(1, 0, 2)),
            "b_router": _bc(inp["b_router"][l]), "w_gu_r": w_gu_r, "b_guT": b_guT,
            "w_down": np.ascontiguousarray(inp["w_down"][l]), "b_down": np.ascontiguousarray(inp["b_down"][l])}


_PROGS = {}


def _prog(key, fn):
    if key not in _PROGS:
        _PROGS[key] = fn()[0]
    return _PROGS[key]


def _run(nc, in_maps):
    res = run_bass_kernel_spmd(nc, in_maps, core_ids=list(range(NCORE)))
    return res.results


def kernel_unfused(**inp):
    inp = {k: np.asarray(v) for k, v in inp.items()}
    x, c, ctx, c_ctx = inp["x"], inp["c"], inp["ctx"], inp["c_ctx"]
    cores = [(cid // 2, cid % 2) for cid in range(NCORE)]
    C5 = np.concatenate([c, c_ctx[None]], 0).astype(np.float32)
    cT5 = np.ascontiguousarray(C5.reshape(5, KC, 128).transpose(2, 1, 0))
    ims = []
    for cid in range(NCORE):
        sl = slice(cid * 1536, (cid + 1) * 1536)
        ims.append({"cT5": cT5, "wmod_sh": np.ascontiguousarray(inp["w_mod"][:, :, sl]),
                    "bmodT": np.ascontiguousarray(inp["b_mod"][:, sl].reshape(DEPTH, 12, 128).transpose(2, 0, 1))})
    rM = _run(_prog("M", build_M), ims)
    mod = np.zeros((DEPTH, 5, 6 * D), np.float32)
    for cid in range(NCORE):
        o = rM[cid]["mod_sh"].reshape(128, DEPTH, 12, 5)
        mod[:, :, cid * 1536:(cid + 1) * 1536] = o.transpose(1, 3, 2, 0).reshape(DEPTH, 5, 1536)
    X = [np.ascontiguousarray(np.concatenate([x[b, h * TL:(h + 1) * TL], ctx[b, h * TCX:(h + 1) * TCX]], 0)) for (b, h) in cores]
    ropes = [_rope_core(64, h) for h in range(2)]
    perm = np.concatenate([np.arange(0, 1024), np.arange(1088, 1600), np.arange(1024, 1088)])
    for l in range(DEPTH):
        i = l // 2
        modls = [_modl_core(mod, l, b) for b in range(4)]
        gmix = _fm(inp["g_mix"][l])
        w_out = np.ascontiguousarray(inp["w_out"][l])
        if l % 2 == 0:
            lam_init = 0.8 - 0.6 * math.exp(-0.3 * l)
            w_in = np.ascontiguousarray(inp["w_in_ab"][i])
            gqk = np.ascontiguousarray(np.stack([_bc(inp["g_aq"][i]), _bc(inp["g_ak"][i])], 1))
            ims = [{"x_in": X[cid], "w_in": w_in, "modl": modls[b], "gmix": gmix, "gqk": gqk, "rope": ropes[h]}
                   for cid, (b, h) in enumerate(cores)]
            rA = _run(_prog("Ae", build_A_even), ims)
            pm = [_pool_mats(h) for h in range(2)]
            w_pool = np.ascontiguousarray(inp["w_pool"][i])
            sp = _fm(inp["s_pool"][i])
            lamr = _bc(inp["lam"][i].reshape(-1))
            gs = _bc(inp["g_subln"][i])
            ims = [{"x_in": X[cid], "q_in": rA[cid]["q_out"], "kvu_in": _kv_pair(rA[cid]["kvu_out"], rA[cid ^ 1]["kvu_out"]),
                    "poolA": pm[h], "w_pool": w_pool, "w_out": w_out, "modl": modls[b], "s_poolT": sp, "lam": lamr, "g_subln": gs}
                   for cid, (b, h) in enumerate(cores)]
            rB = _run(_prog("Be%d" % l, lambda: build_B1_even(lam_init)), ims)
        else:
            w_in = np.ascontiguousarray(inp["w_in_cd"][i][:, perm])
            w_qb = np.ascontiguousarray(inp["w_qb"][i])
            w_kvb = np.ascontiguousarray(inp["w_kvb"][i])
            gq, gk, gmq, gmk = _bc(inp["g_qa"][i]), _bc(inp["g_kva"][i]), _bc(inp["g_mq"][i]), _bc(inp["g_mk"][i])
            ims = [{"x_in": X[cid], "w_in": w_in, "w_qb": w_qb, "w_kvb": w_kvb, "modl": modls[b], "gmix": gmix, "g_qa": gq,
                    "g_kva": gk, "g_mq": gmq, "g_mk": gmk, "rope": ropes[h]} for cid, (b, h) in enumerate(cores)]
            rA = _run(_prog("Ao", build_A_odd), ims)
            dft = [_dft_tabs(h) for h in range(2)]
            w_f = np.ascontiguousarray(inp["w_fourier"][i])
            ims = []
            for cid, (b, h) in enumerate(cores):
                d = {"x_in": X[cid], "q_in": rA[cid]["q_out"], "kvu_in": _kv_pair(rA[cid]["kvu_out"], rA[cid ^ 1]["kvu_out"]),
                     "w_fourier": w_f, "w_out": w_out, "modl": modls[b]}
                d.update(dft[h])
                ims.append(d)
            rB = _run(_prog("Bo", build_B1_odd), ims)
        X = [np.ascontiguousarray(rB[cid]["x_out"]) for cid in range(NCORE)]
        mh = _moe_host(inp, l)
        gffn = _fm(inp["g_ffn"][l])
        ims = []
        for cid, (b, h) in enumerate(cores):
            d = {"x_in": X[cid], "modl": modls[b], "gffn": gffn}
            d.update(mh)
            ims.append(d)
        rC = _run(_prog("C", build_moe_test), ims)
        X = [np.ascontiguousarray(rC[cid]["x_out"]) for cid in range(NCORE)]
    out = np.zeros((4, 2048, D), np.float32)
    for cid, (b, h) in enumerate(cores):
        out[b, h * TL:(h + 1) * TL] = X[cid][:TL]
    return out


I32 = mybir.dt.int32
ARENA_BYTES = 150 * 1024


def emit_mods_fused(P):
    S, M = P.S, P.M
    nc = P.nc
    cT5 = P.din("cT5", [128, KC, 5])
    wm = P.din("wmod_sh", [DEPTH, D, 1536])
    bmT = P.din("bmodT", [128, DEPTH, 12])
    bsel_d = P.din("bsel", [128, 5])
    ct = M.sb("ct", [128, KC, 5], F32)
    st = M.sb("st", [128, KC, 5], F32)
    bt = M.sb("bt", [128, DEPTH, 12], F32)
    bsel = M.sb("bsel", [128, 5], F32)
    res = M.sb("res", [128, DEPTH * 12 * 5], F32)
    wbuf = [M.sb("wm%d" % i, [128, KC, 768], F32) for i in range(2)]
    b_ct, b_st, b_bt, b_res, b_bsel = Buf(), Buf(), Buf(), Buf(), Buf()
    b_w = [Buf(), Buf()]
    S.dma("sp", ct, cT5, writes=[b_ct])
    S.dma("sp", bt, bmT, writes=[b_bt])
    S.dma("sp", bsel, bsel_d, writes=[b_bsel])
    S.op("act", lambda e: e.activation(out=st, in_=ct, func=AF.Silu), reads=[b_ct], writes=[b_st])
    it = 0
    for l in range(DEPTH):
        for hh in range(2):
            wb, bw = wbuf[it % 2], b_w[it % 2]
            it += 1
            S.dma("sp" if hh == 0 else "act", wb,
                  wm[l, :, hh * 768:(hh + 1) * 768].rearrange("(k p) c -> p k c", p=128), writes=[bw])
            for q6 in range(6):
                q = hh * 6 + q6
                bank = P.rr("m", 4)
                ps = P.pb[bank][:, 0:5]
                mm_group(P, ps, [(wb[:, k, q6 * 128:(q6 + 1) * 128], st[:, k, :]) for k in range(KC)],
                         reads=[bw, b_st], writes=[P.pbb[bank]])
                o = (l * 12 + q) * 5
                S.op("dve", lambda e, ps=ps, o=o, l=l, q=q: e.tensor_scalar(
                    out=res[:, o:o + 5], in0=ps, scalar1=bt[:, l, q:q + 1], scalar2=None, op0=ALU.add),
                    reads=[P.pbb[bank], b_bt], writes=[b_res])
    b_own, b_all, b_m5, b_tmp = Buf(), Buf(), Buf(), Buf()
    m5 = M.sb("m5", [128, NCORE, 240], F32)
    for l in range(DEPTH):
        mod_own = nc.dram_tensor("mod_own%d" % l, [128, 60], F32).ap()
        mod_all = nc.dram_tensor("mod_all%d" % l, [NCORE * 128, 60], F32).ap()
        S.dma("sp", mod_own, res[:, l * 60:(l + 1) * 60], reads=[b_res], writes=[b_own])
        if getattr(P, "fake_ag", False):
            mod_all = P.din("fake_mod_all%d" % l, [NCORE * 128, 60])
        else:
            S.custom("pool", lambda e, mod_own=mod_own, mod_all=mod_all: e.collective_compute(
                "AllGather", ALU.bypass, replica_groups=[list(range(NCORE))], ins=[mod_own.opt()], outs=[mod_all.opt()]),
                reads=[b_own], writes=[b_all], inc=1)
        S.dma("sp", m5[:, :, l * 60:(l + 1) * 60], mod_all.rearrange("(r p) x -> p r x", p=128), reads=[b_all], writes=[b_m5])
    tmp = M.sb("mtmp", [128, NCORE * 48, 5], F32)
    sel = M.sb("msel", [128, NCORE * 48], F32)
    m5f = m5.rearrange("p r (x w) -> p (r x) w", w=5)
    S.op("dve", lambda e: e.tensor_tensor(out=tmp, in0=m5f, in1=bsel.unsqueeze(1).to_broadcast([128, NCORE * 48, 5]),
                                          op=ALU.mult), reads=[b_m5, b_bsel], writes=[b_tmp])
    S.op("dve", lambda e: e.tensor_reduce(out=sel, in_=tmp, axis=AX.X, op=ALU.add), reads=[b_tmp], writes=[b_tmp])
    sel4 = sel.rearrange("p (r l q) -> p r l q", r=NCORE, l=DEPTH)
    m54 = m5.rearrange("p r (l q w) -> p r l q w", l=DEPTH, w=5)
    for l in range(DEPTH):
        S.op("dve", lambda e, l=l: e.tensor_copy(out=P.modl_all[:, l, 0, :].rearrange("p (r q) -> p r q", q=12),
                                                in_=sel4[:, :, l, :]), reads=[b_tmp], writes=[P.b_modl_all])
        S.op("dve", lambda e, l=l: e.tensor_copy(out=P.modl_all[:, l, 1, :].rearrange("p (r q) -> p r q", q=12),
                                                in_=m54[:, :, l, :, 4]), reads=[b_m5], writes=[P.b_modl_all])


def emit_exchange(P, W):
    S, M = P.S, P.M
    nc = P.nc
    own = P.bind["kvu_out"]
    shr = nc.dram_tensor(P.tag + "kvu_shr", [2 * T, W], BF16, addr_space="Shared").ap()
    mine = nc.dram_tensor(P.tag + "kvu_mine", [2304, W], BF16).ap()
    bar_in = nc.dram_tensor(P.tag + "bar_in", [128, 16], F32).ap()
    bar_out = nc.dram_tensor(P.tag + "bar_out", [NCORE * 128, 16], F32).ap()
    b_shr, b_bar, b_mine, b_bi = Buf(), Buf(), Buf(), Buf()
    g = [M.sb("xg%d" % i, [128, W], BF16) for i in range(2)]
    bg = [Buf(), Buf()]
    S.dma("sp", bar_in, P.ones128[:, 0:16], reads=[P.b_ones], writes=[b_bi])
    for t in range(NT):
        gi = t % 2
        S.dma("sp", g[gi], own[t * 128:(t + 1) * 128, :], writes=[bg[gi]])
        S.custom("pool", lambda e, gi=gi, t=t: e.indirect_dma_start(
            out=shr, out_offset=bass.IndirectOffsetOnAxis(ap=P.widx[:, t:t + 1], axis=0), in_=g[gi], in_offset=None),
            reads=[bg[gi], P.b_kidx], writes=[b_shr], inc=16)
    S.custom("pool", lambda e: e.collective_compute("AllGather", ALU.bypass, replica_groups=[list(range(NCORE))],
                                                    ins=[bar_in.opt()], outs=[bar_out.opt()]),
             reads=[b_shr, b_bi], writes=[b_bar], inc=1)
    for kt in range(18):
        gi = kt % 2
        S.custom("pool", lambda e, gi=gi, kt=kt: e.indirect_dma_start(
            out=g[gi], out_offset=None, in_=shr,
            in_offset=bass.IndirectOffsetOnAxis(ap=P.kidx[:, kt:kt + 1], axis=0)),
            reads=[b_bar, P.b_kidx], writes=[bg[gi]], inc=16)
        S.dma("sp", mine[kt * 128:(kt + 1) * 128, :], g[gi], reads=[bg[gi]], writes=[b_mine])
    P.bind["kvu_in"] = mine
    P.bind["q_in"] = P.bind["q_out"]


def build_fused(n_layers=DEPTH, n_exp=NEXP, debug=False):
    P = Prog(fused=True, arena_bytes=ARENA_BYTES)
    P.debugA = False
    P.consts()
    S = P.S
    nc = P.nc
    x_in = P.din("x_in", [T, D])
    x_out = nc.dram_tensor("x_out", [TL, D], F32, kind="ExternalOutput").ap()
    load_x(P, x_in)
    kidx_d = P.din("kidx", [128, 18], I32)
    P.kidx = P.MP.sb("kidx_sb", [128, 18], I32)
    P.b_kidx = Buf()
    S.dma("sp", P.kidx[:], kidx_d, writes=[P.b_kidx])
    widx_d = P.din("widx", [128, NT], I32)
    P.widx = P.MP.sb("widx_sb", [128, NT], I32)
    S.dma("sp", P.widx[:], widx_d, writes=[P.b_kidx])
    P.modl_all = P.MP.sb("modl_all", [128, DEPTH, 2, 96], F32)
    P.b_modl_all = Buf()
    P.make_arena()
    emit_mods_fused(P)
    for l in range(n_layers):
        P.new_phase("L%d_" % l)
        P.bind = {"x_in": x_in}
        P.sb_bind = {"modl": (P.modl_all[:, l], P.b_modl_all)}
        if l % 2 == 0:
            emit_A_even(P)
            W = 3072
        else:
            emit_A_odd(P)
            W = 4352
        P.new_phase()
        if debug and l == 0:
            dbo0 = Buf()
            dqa = nc.dram_tensor("dbg_qA", [T, 1024], BF16, kind="ExternalOutput").ap()
            S.dma("sp", dqa, P.bind["q_out"], writes=[dbo0])
            dka = nc.dram_tensor("dbg_kvuA", [T, 3072], BF16, kind="ExternalOutput").ap()
            S.dma("sp", dka, P.bind["kvu_out"], writes=[dbo0])
            P.S.barrier()
        emit_exchange(P, W)
        P.new_phase()
        if l % 2 == 0:
            emit_B1_even(P, 0.8 - 0.6 * math.exp(-0.3 * l))
        else:
            emit_B1_odd(P)
        P.new_phase()
        if debug and l == 0:
            dbo = Buf()
            dx = nc.dram_tensor("dbg_xmid", [T, D], F32, kind="ExternalOutput").ap()
            for t in range(NT):
                S.dma("sp", dx[t * 128:(t + 1) * 128, :], P.X[:, t, :], reads=[P.bX[t]], writes=[dbo])
            dm = nc.dram_tensor("dbg_modl", [128, DEPTH * 2 * 96], F32, kind="ExternalOutput").ap()
            S.dma("sp", dm, P.modl_all[:].rearrange("p a b c -> p (a b c)"), reads=[P.b_modl_all], writes=[dbo])
            dk = nc.dram_tensor("dbg_kvu", [2304, 3072], BF16, kind="ExternalOutput").ap()
            S.dma("sp", dk, P.bind["kvu_in"], writes=[dbo])
            dq = nc.dram_tensor("dbg_q", [T, 1024], BF16, kind="ExternalOutput").ap()
            S.dma("sp", dq, P.bind["q_in"], writes=[dbo])
            P.S.barrier()
        emit_moe_test(P, n_exp)
    S.barrier()
    bo = Buf()
    for t in range(8):
        S.dma("sp" if t % 2 == 0 else "act", x_out[t * 128:(t + 1) * 128, :], P.X[:, t, :], reads=[P.bX[t]], writes=[bo])
    return P.finish([bo]), P


def _fused_inputs(inp):
    x, c, ctx, c_ctx = inp["x"], inp["c"], inp["ctx"], inp["c_ctx"]
    cores = [(cid // 2, cid % 2) for cid in range(NCORE)]
    C5 = np.concatenate([c, c_ctx[None]], 0).astype(np.float32)
    shared = {"cT5": np.ascontiguousarray(C5.reshape(5, KC, 128).transpose(2, 1, 0))}
    per_half = [dict(), dict()]
    ropes = [_rope_core(64, h) for h in range(2)]
    perm = np.concatenate([np.arange(0, 1024), np.arange(1088, 1600), np.arange(1024, 1088)])
    for l in range(DEPTH):
        i = l // 2
        tg = "L%d_" % l
        shared[tg + "gmix"] = _fm(inp["g_mix"][l])
        shared[tg + "gffn"] = _fm(inp["g_ffn"][l])
        shared[tg + "w_out"] = np.ascontiguousarray(inp["w_out"][l])
        for k, v in _moe_host(inp, l).items():
            shared[tg + k] = v
        for h in range(2):
            per_half[h][tg + "rope"] = ropes[h]
        if l % 2 == 0:
            shared[tg + "w_in"] = np.ascontiguousarray(inp["w_in_ab"][i])
            shared[tg + "gqk"] = np.ascontiguousarray(np.stack([_bc(inp["g_aq"][i]), _bc(inp["g_ak"][i])], 1))
            shared[tg + "w_pool"] = np.ascontiguousarray(inp["w_pool"][i])
            shared[tg + "s_poolT"] = _fm(inp["s_pool"][i])
            shared[tg + "lam"] = _bc(inp["lam"][i].reshape(-1))
            shared[tg + "g_subln"] = _bc(inp["g_subln"][i])
            for h in range(2):
                per_half[h][tg + "poolA"] = _pool_mats(h)
        else:
            shared[tg + "w_in"] = np.ascontiguousarray(inp["w_in_cd"][i][:, perm])
            shared[tg + "w_qb"] = np.ascontiguousarray(inp["w_qb"][i])
            shared[tg + "w_kvb"] = np.ascontiguousarray(inp["w_kvb"][i])
            shared[tg + "g_qa"] = _bc(inp["g_qa"][i])
            shared[tg + "g_kva"] = _bc(inp["g_kva"][i])
            shared[tg + "g_mq"] = _bc(inp["g_mq"][i])
            shared[tg + "g_mk"] = _bc(inp["g_mk"][i])
            shared[tg + "w_fourier"] = np.ascontiguousarray(inp["w_fourier"][i])
            for h in range(2):
                for k, v in _dft_tabs(h).items():
                    per_half[h][tg + k] = v
    ims = []
    for cid, (b, h) in enumerate(cores):
        d = dict(shared)
        d.update(per_half[h])
        d["x_in"] = np.ascontiguousarray(np.concatenate([x[b, h * TL:(h + 1) * TL], ctx[b, h * TCX:(h + 1) * TCX]], 0))
        ids = np.concatenate([h * T + np.arange(TL), (1 - h) * T + np.arange(TL), h * T + TL + np.arange(TCX),
                              (1 - h) * T + TL + np.arange(TCX)]).astype(np.int32)
        d["kidx"] = np.ascontiguousarray(ids.reshape(18, 128).T)
        d["widx"] = np.ascontiguousarray((h * T + np.arange(T)).astype(np.int32).reshape(NT, 128).T)
        sl = slice(cid * 1536, (cid + 1) * 1536)
        d["wmod_sh"] = np.ascontiguousarray(inp["w_mod"][:, :, sl])
        d["bmodT"] = np.ascontiguousarray(inp["b_mod"][:, sl].reshape(DEPTH, 12, 128).transpose(2, 0, 1))
        bs = np.zeros((128, 5), np.float32)
        bs[:, b] = 1.0
        d["bsel"] = bs
        ims.append(d)
    return ims


def kernel_fused(**inp):
    inp = {k: np.asarray(v) for k, v in inp.items()}
    if "F" not in _PROGS:
        _PROGS["F"] = build_fused()[0]
    ims = _fused_inputs(inp)
    res = run_bass_kernel_spmd(_PROGS["F"], ims, core_ids=list(range(NCORE))).results
    out = np.zeros((4, 2048, D), np.float32)
    for cid in range(NCORE):
        b, h = cid // 2, cid % 2
        out[b, h * TL:(h + 1) * TL] = res[cid]["x_out"]
    return out


def kernel(**inp):
    return kernel_unfused(**inp)
```
